# Optimizing a Trainium2 kernel written in Bass

```python
import jax, jax.numpy as jnp
from jax import lax
import numpy as np

D_MODEL = 2048
BATCH = 4
SEQ = 4096
DEPTH = 4

HEAD_DIM = 64
SWA_HEADS = 8
SWA_KV_HEADS = 2
WINDOW = 128
FOX_HEADS = 8
FOX_QBLOCK = 128
GLA_HEADS = 4
GLA_DK = 64
GLA_DV = 128
GLA_GATE_RANK = 16
GLA_TAU = 16.0
RET_HEADS = 4
RET_DK = 64
RET_DV = 128
RET_THETA_BASE = 10000.0
CHUNK = 64
BRANCH_WIDTH = SWA_HEADS * HEAD_DIM
N_BRANCH = 4
D_FF = 5632
N_MOD = 9
EPS = 1e-6

IN_SIZES = (
    SWA_HEADS * HEAD_DIM, SWA_KV_HEADS * HEAD_DIM, SWA_KV_HEADS * HEAD_DIM,
    FOX_HEADS * HEAD_DIM, FOX_HEADS * HEAD_DIM, FOX_HEADS * HEAD_DIM, FOX_HEADS,
    GLA_HEADS * GLA_DK, GLA_HEADS * GLA_DK, GLA_HEADS * GLA_DV, GLA_GATE_RANK, GLA_HEADS * GLA_DV,
    RET_HEADS * RET_DK, RET_HEADS * RET_DK, RET_HEADS * RET_DV, RET_HEADS * RET_DV,
)
IN_COLS = sum(IN_SIZES)

kernel_name = 'hybrid_macaron_swa_fox_gla_retnet_adaln'


def _split_cols(p):
    out = []
    start = 0
    for size in IN_SIZES:
        out.append(p[..., start:start + size])
        start += size
    return out


def rmsnorm(x, g):
    xf = x.astype(jnp.float32)
    y = xf * lax.rsqrt(jnp.mean(xf * xf, axis=-1, keepdims=True) + EPS)
    return (y * g.astype(jnp.float32)).astype(x.dtype)


def modulate(x, shift, scale):
    return x * (1.0 + scale) + shift


def swiglu(x, w_gate, w_up, w_down):
    return (jax.nn.silu(x @ w_gate) * (x @ w_up)) @ w_down


def head_group_norm(o, w, bias):
    b, s = o.shape[:2]
    of = o.astype(jnp.float32)
    mu = jnp.mean(of, axis=-1, keepdims=True)
    var = jnp.mean(jnp.square(of - mu), axis=-1, keepdims=True)
    y = ((of - mu) * lax.rsqrt(var + EPS)).reshape(b, s, -1)
    return (y * w.astype(jnp.float32) + bias.astype(jnp.float32)).astype(o.dtype)


def rotary(x, pos):
    d = x.shape[-1]
    half = d // 2
    inv = RET_THETA_BASE ** (-jnp.arange(half, dtype=jnp.float32) / half)
    ang = pos.astype(jnp.float32)[:, None] * inv[None, :]
    cos = jnp.cos(ang)[None, :, None, :]
    sin = jnp.sin(ang)[None, :, None, :]
    x1 = x[..., :half].astype(jnp.float32)
    x2 = x[..., half:].astype(jnp.float32)
    return jnp.concatenate([x1 * cos - x2 * sin, x2 * cos + x1 * sin], axis=-1).astype(x.dtype)


def sliding_window_attention(q, k, v, sinks):
    b, s, h, d = q.shape
    kvh = k.shape[2]
    g = h // kvh
    nb = s // WINDOW
    qb = q.reshape(b, nb, WINDOW, kvh, g, d)

    def with_prev(t):
        t = t.reshape(b, nb, WINDOW, kvh, d)
        prev = jnp.concatenate([jnp.zeros_like(t[:, :1]), t[:, :-1]], axis=1)
        return jnp.concatenate([prev, t], axis=2)

    kb, vb = with_prev(k), with_prev(v)
    logits = jnp.einsum('bnqkgd,bnskd->bnkgqs', qb, kb).astype(jnp.float32) * (d ** -0.5)
    qpos = jnp.arange(WINDOW)[:, None] + WINDOW
    kpos = jnp.arange(2 * WINDOW)[None, :]
    rel = qpos - kpos
    band = (rel >= 0) & (rel < WINDOW)
    has_prev = (jnp.arange(nb)[:, None, None] > 0) | (kpos[None] >= WINDOW)
    mask = band[None] & has_prev
    logits = jnp.where(mask[None, :, None, None], logits, -jnp.inf)
    sink = sinks.astype(jnp.float32).reshape(1, 1, kvh, g, 1, 1)
    m = jnp.maximum(jnp.max(logits, axis=-1, keepdims=True), sink)
    p = jnp.exp(logits - m)
    denom = jnp.sum(p, axis=-1, keepdims=True) + jnp.exp(sink - m)
    w = (p / denom).astype(v.dtype)
    o = jnp.einsum('bnkgqs,bnskd->bnqkgd', w, vb)
    return o.reshape(b, s, h * d)


def forgetting_attention(q, k, v, f_logit):
    b, s, h, d = q.shape
    nb = s // FOX_QBLOCK
    logf_cum = jnp.cumsum(jax.nn.log_sigmoid(f_logit.astype(jnp.float32)), axis=1).transpose(0, 2, 1)
    q_blocks = q.reshape(b, nb, FOX_QBLOCK, h, d).transpose(1, 0, 2, 3, 4)
    f_blocks = logf_cum.reshape(b, h, nb, FOX_QBLOCK).transpose(2, 0, 1, 3)
    kpos = jnp.arange(s)
    scale = d ** -0.5

    def block(args):
        qi, fi, i = args
        logits = (jnp.einsum('bqhd,bshd->bhqs', qi, k).astype(jnp.float32) * scale
                  + fi[..., :, None] - logf_cum[:, :, None, :])
        qpos = i * FOX_QBLOCK + jnp.arange(FOX_QBLOCK)
        logits = jnp.where(kpos[None, :] <= qpos[:, None], logits, -jnp.inf)
        p = jax.nn.softmax(logits, axis=-1).astype(v.dtype)
        return jnp.einsum('bhqs,bshd->bqhd', p, v)

    o = lax.map(block, (q_blocks, f_blocks, jnp.arange(nb)))
    return o.transpose(1, 0, 2, 3, 4).reshape(b, s, h * d)


def chunked_gated_linear_attention(q, k, v, log_decay):
    b, s, h, dk = q.shape
    dv = v.shape[-1]
    nc = s // CHUNK
    f32 = jnp.float32
    qc = q.astype(f32).reshape(b, nc, CHUNK, h, dk) * (dk ** -0.5)
    kc = k.astype(f32).reshape(b, nc, CHUNK, h, dk)
    vc = v.astype(f32).reshape(b, nc, CHUNK, h, dv)
    ld = jnp.broadcast_to(log_decay.astype(f32), (b, s, h, dk)).reshape(b, nc, CHUNK, h, dk)
    cum = jnp.cumsum(ld, axis=2)
    last = cum[:, :, -1:]
    q_in = qc * jnp.exp(cum)
    k_in = kc * jnp.exp(-cum)
    k_state = kc * jnp.exp(last - cum)
    causal = jnp.tril(jnp.ones((CHUNK, CHUNK), dtype=bool))
    scores = jnp.where(causal, jnp.einsum('bnihk,bnjhk->bnhij', q_in, k_in), 0.0)
    o_intra = jnp.einsum('bnhij,bnjhv->bnihv', scores, vc)
    updates = jnp.einsum('bnjhk,bnjhv->bnhkv', k_state, vc)
    chunk_decay = jnp.exp(last[:, :, 0])

    def step(state, inp):
        dec, upd = inp
        return dec[..., None] * state + upd, state

    init = jnp.zeros((b, h, dk, dv), f32)
    _, prev_states = lax.scan(step, init, (chunk_decay.transpose(1, 0, 2, 3), updates.transpose(1, 0, 2, 3, 4)))
    prev_states = prev_states.transpose(1, 0, 2, 3, 4)
    o_inter = jnp.einsum('bnihk,bnhkv->bnihv', q_in, prev_states)
    return (o_intra + o_inter).reshape(b, s, h, dv).astype(v.dtype)


def hybrid_token_mixer(n, w_in, fox_b_forget, attn_sinks, gla_w_gate, gla_b_gate, gla_norm_g,
                       ret_gn_w, ret_gn_b, w_branch, w_merge, b_merge, w_out):
    b, s, _ = n.shape
    (a_q, a_k, a_v, f_q, f_k, f_v, f_f, g_q, g_k, g_v, g_lr, g_r,
     r_q, r_k, r_v, r_g) = _split_cols(n @ w_in)

    def heads(t, nh):
        return t.reshape(b, s, nh, -1)

    o_a = sliding_window_attention(heads(a_q, SWA_HEADS), heads(a_k, SWA_KV_HEADS),
                                   heads(a_v, SWA_KV_HEADS), attn_sinks)
    o_b = forgetting_attention(heads(f_q, FOX_HEADS), heads(f_k, FOX_HEADS), heads(f_v, FOX_HEADS),
                               f_f + fox_b_forget)
    log_alpha = jax.nn.log_sigmoid((g_lr @ gla_w_gate + gla_b_gate).astype(jnp.float32)) / GLA_TAU
    o_c = chunked_gated_linear_attention(heads(g_q, GLA_HEADS), heads(g_k, GLA_HEADS),
                                         heads(g_v, GLA_HEADS), heads(log_alpha, GLA_HEADS))
    o_c = rmsnorm(o_c, gla_norm_g).reshape(b, s, -1) * jax.nn.silu(g_r)
    pos = jnp.arange(s)
    log_gamma = jnp.log1p(-jnp.exp2(-5.0 - jnp.arange(RET_HEADS, dtype=jnp.float32)))
    o_d = chunked_gated_linear_attention(rotary(heads(r_q, RET_HEADS), pos), rotary(heads(r_k, RET_HEADS), pos),
                                         heads(r_v, RET_HEADS), log_gamma[None, None, :, None])
    o_d = head_group_norm(o_d, ret_gn_w, ret_gn_b) * jax.nn.silu(r_g)
    merged = jnp.zeros((b, s, D_MODEL), n.dtype)
    for i, o in enumerate((o_a, o_b, o_c, o_d)):
        gate = jax.nn.sigmoid((n @ w_merge[i] + b_merge[i]).astype(jnp.float32)).astype(n.dtype)
        merged = merged + gate * (o @ w_branch[i])
    return merged @ w_out


def setup_inputs(seed: int = 0) -> dict:
    key = jax.random.key(seed)
    ks = jax.random.split(key, 24)
    L, D, F = DEPTH, D_MODEL, D_FF
    nrm = jax.random.normal
    f32 = jnp.float32
    return {
        'x': nrm(ks[0], (BATCH, SEQ, D), f32),
        'c': nrm(ks[1], (BATCH, D), f32),
        'w_ada': nrm(ks[2], (L, D, N_MOD * D), f32) * (0.5 * D ** -0.5),
        'b_ada': nrm(ks[3], (L, N_MOD * D), f32) * 0.02,
        'norm_g': 1.0 + 0.02 * nrm(ks[4], (L, 3, D), f32),
        'ffn_w_gate': nrm(ks[5], (L, 2, D, F), f32) * D ** -0.5,
        'ffn_w_up': nrm(ks[6], (L, 2, D, F), f32) * D ** -0.5,
        'ffn_w_down': nrm(ks[7], (L, 2, F, D), f32) * F ** -0.5,
        'w_in': nrm(ks[8], (L, D, IN_COLS), f32) * D ** -0.5,
        'fox_b_forget': 2.0 + 0.5 * nrm(ks[9], (L, FOX_HEADS), f32),
        'attn_sinks': 0.5 * nrm(ks[10], (L, SWA_HEADS), f32),
        'gla_w_gate': nrm(ks[11], (L, GLA_GATE_RANK, GLA_HEADS * GLA_DK), f32) * GLA_GATE_RANK ** -0.5,
        'gla_b_gate': 0.02 * nrm(ks[12], (L, GLA_HEADS * GLA_DK), f32),
        'gla_norm_g': 1.0 + 0.02 * nrm(ks[13], (L, GLA_DV), f32),
        'ret_gn_w': 1.0 + 0.02 * nrm(ks[14], (L, RET_HEADS * RET_DV), f32),
        'ret_gn_b': 0.02 * nrm(ks[15], (L, RET_HEADS * RET_DV), f32),
        'w_branch': nrm(ks[16], (L, N_BRANCH, BRANCH_WIDTH, D), f32) * BRANCH_WIDTH ** -0.5,
        'w_merge': nrm(ks[17], (L, N_BRANCH, D, D), f32) * D ** -0.5,
        'b_merge': 0.02 * nrm(ks[18], (L, N_BRANCH, D), f32),
        'w_out': nrm(ks[19], (L, D, D), f32) * D ** -0.5,
        'final_norm_g': 1.0 + 0.02 * nrm(ks[20], (D,), f32),
    }


def reference(x, c, w_ada, b_ada, norm_g, ffn_w_gate, ffn_w_up, ffn_w_down, w_in, fox_b_forget,
              attn_sinks, gla_w_gate, gla_b_gate, gla_norm_g, ret_gn_w, ret_gn_b, w_branch,
              w_merge, b_merge, w_out, final_norm_g):
    cond = jax.nn.silu(c)
    h = x
    for l in range(DEPTH):
        mod = cond @ w_ada[l] + b_ada[l]
        sh1, sc1, g1, sh2, sc2, g2, sh3, sc3, g3 = [m[:, None, :] for m in jnp.split(mod, N_MOD, axis=-1)]
        u = modulate(rmsnorm(h, norm_g[l, 0]), sh1, sc1)
        h = h + 0.5 * g1 * swiglu(u, ffn_w_gate[l, 0], ffn_w_up[l, 0], ffn_w_down[l, 0])
        u = modulate(rmsnorm(h, norm_g[l, 1]), sh2, sc2)
        h = h + g2 * hybrid_token_mixer(u, w_in[l], fox_b_forget[l], attn_sinks[l], gla_w_gate[l],
                                        gla_b_gate[l], gla_norm_g[l], ret_gn_w[l], ret_gn_b[l],
                                        w_branch[l], w_merge[l], b_merge[l], w_out[l])
        u = modulate(rmsnorm(h, norm_g[l, 2]), sh3, sc3)
        h = h + 0.5 * g3 * swiglu(u, ffn_w_gate[l, 1], ffn_w_up[l, 1], ffn_w_down[l, 1])
    return rmsnorm(h, final_norm_g)
```

```python
import numpy as np
import ml_dtypes
from contextlib import ExitStack
import concourse.bass as bass
import concourse.mybir as mybir
from concourse.bass_utils import run_bass_kernel_spmd

F32 = mybir.dt.float32
BF16 = mybir.dt.bfloat16
AF = mybir.ActivationFunctionType
ALU = mybir.AluOpType

D = 2048
DC = 16
DFF = 5632
FC = 44
NMOD = 9
EPS = 1e-6
DEPTH = 4
SEQ = 4096
BATCH = 4
INCOLS = 5400
A_Q, A_K, A_V = 0, 512, 640
B_Q, B_K, B_V, B_F = 768, 1280, 1792, 2304
C_Q, C_K, C_V, C_LR, C_R = 2312, 2568, 2824, 3336, 3352
D_Q, D_K, D_V, D_G = 3864, 4120, 4376, 4888


import os
LA_CUT = int(os.environ.get('LA_CUT', '0'))


class Buf:
    __slots__ = ("w", "r")

    def __init__(self):
        self.w = None
        self.r = {}


class KB:
    def __init__(self, nc, es):
        self.nc = nc
        self.eng = {"pe": nc.tensor, "act": nc.scalar, "dve": nc.vector, "pool": nc.gpsimd, "sp": nc.sync}
        self.psem = {e: es.enter_context(nc.semaphore("p_" + e)) for e in self.eng}
        self.cnt = {e: 0 for e in self.eng}
        self.seen = {e: {} for e in self.eng}
        self.semof = {("e", e): self.psem[e] for e in self.eng}
        self.dq = {}
        self.dnext = {}
        for q, n in (("sp", 24), ("pool", 12)):
            self.dq[q] = []
            for i in range(n):
                sem = es.enter_context(nc.semaphore("d_%s%d" % (q, i)))
                self.dq[q].append([sem, 0])
                self.semof[("d", q, i)] = sem
            self.dnext[q] = 0
        self.ninst = 0

    def _deps(self, reads, writes):
        d = {}
        for b in reads:
            if b.w is not None:
                k, v = b.w
                if d.get(k, 0) < v:
                    d[k] = v
        for b in writes:
            if b.w is not None:
                k, v = b.w
                if d.get(k, 0) < v:
                    d[k] = v
            for k, v in b.r.items():
                if d.get(k, 0) < v:
                    d[k] = v
        return d

    def _wait(self, e, d):
        seen = self.seen[e]
        for k, v in d.items():
            if e == "pe" and k == ("e", "pe"):
                continue
            if seen.get(k, 0) < v:
                self.eng[e].wait_ge(self.semof[k], v)
                seen[k] = v
                self.ninst += 1

    def _mark(self, tok, reads, writes):
        k, v = tok
        for b in reads:
            if b.r.get(k, 0) < v:
                b.r[k] = v
        for b in writes:
            b.w = tok
            b.r = {}

    def op(self, e, fn, reads=(), writes=()):
        self._wait(e, self._deps(reads, writes))
        ins = fn(self.eng[e])
        self.cnt[e] += 1
        ins.then_inc(self.psem[e], 1)
        self.ninst += 1
        self._mark((("e", e), self.cnt[e]), reads, writes)

    def dma(self, q, out, in_, reads=(), writes=()):
        slots = self.dq[q]
        i = self.dnext[q]
        self.dnext[q] = (i + 1) % len(slots)
        sl = slots[i]
        d = self._deps(reads, writes)
        key = ("d", q, i)
        if sl[1] > 0 and d.get(key, 0) < sl[1]:
            d[key] = sl[1]
        self._wait(q, d)
        ins = self.eng[q].dma_start(out=out, in_=in_)
        sl[1] += 16
        ins.then_inc(sl[0], 16)
        self.ninst += 1
        self._mark((key, sl[1]), reads, writes)

    def barrier(self):
        tgt = {("e", e): c for e, c in self.cnt.items() if c > 0}
        for q, slots in self.dq.items():
            for i, sl in enumerate(slots):
                if sl[1] > 0:
                    tgt[("d", q, i)] = sl[1]
        for e in self.eng:
            seen = self.seen[e]
            for k, v in tgt.items():
                if seen.get(k, 0) < v:
                    self.eng[e].wait_ge(self.semof[k], v)
                    seen[k] = v
                    self.ninst += 1


def host_consts(S):
    c = {}
    p = np.arange(128)
    c["ident_f"] = np.eye(128, dtype=np.float32)
    c["ident_b"] = np.eye(128, dtype=np.float32).astype(ml_dtypes.bfloat16)
    le = (p[:, None] <= p[None, :]).astype(np.float32)
    c["mask_le"] = le.astype(ml_dtypes.bfloat16)
    c["mask_gt"] = (1.0 - le).astype(ml_dtypes.bfloat16)
    c["ones_b"] = np.ones((128, 128), dtype=ml_dtypes.bfloat16)
    c["ones_f"] = np.ones((128, 128), dtype=np.float32)
    op = np.zeros((128, 192), np.float32)
    op[:, 0:64] = 1.0
    op[:, 128:192] = 1.0
    c["onespad"] = op.astype(ml_dtypes.bfloat16)
    sel = np.zeros((128, 128), np.float32)
    sel[63, :] = 1.0
    c["sel63"] = sel
    q = np.arange(64)
    c["tri64n"] = (-(q[:, None] <= q[None, :]).astype(np.float32) / 16.0)
    c["d64n"] = (-(q[:, None] > q[None, :]).astype(np.float32) / 16.0)
    half = 32
    inv = (10000.0 ** (-np.arange(half, dtype=np.float32) / half)).astype(np.float32)
    pos = np.arange(S, dtype=np.float32)
    ang = (pos[:, None] * inv[None, :]).astype(np.float32)
    cos = np.cos(ang).astype(np.float32)
    sin = np.sin(ang).astype(np.float32)
    fidx = (p % 64) % 32
    sgn = np.where((p % 64) < 32, -1.0, 1.0).astype(np.float32)
    c["cosT"] = np.ascontiguousarray(cos.T[fidx, :])
    c["sinT"] = np.ascontiguousarray(sin.T[fidx, :] * sgn[:, None])
    c["cos_tok"] = np.ascontiguousarray(cos.reshape(S // 64, 64, 32).transpose(1, 0, 2))
    c["sin_tok"] = np.ascontiguousarray(sin.reshape(S // 64, 64, 32).transpose(1, 0, 2))
    lg = np.log1p(-np.exp2(-5.0 - np.arange(4, dtype=np.float64)))
    j = np.arange(64, dtype=np.float64)
    dq = np.zeros((64, 4, 64), np.float64)
    dk = np.zeros((64, 4, 64), np.float64)
    dec = np.zeros((64, 4), np.float64)
    for h in range(4):
        dq[:, h, :] = 0.125 * np.exp((j + 1) * lg[h])[None, :]
        dk[:, h, :] = np.exp(-(j + 1) * lg[h])[None, :]
        dec[:, h] = np.exp(64 * lg[h])
    c["ret_dq"] = dq.astype(np.float32)
    c["ret_dk"] = dk.astype(np.float32)
    c["ret_dec"] = dec.astype(np.float32)
    ks = np.zeros((64, 4), np.float64)
    for h in range(4):
        ks[:, h] = np.exp((63 - j) * lg[h])
    c["ret_ks"] = ks.astype(np.float32)
    return c


CONST_DT = {"ident_b": BF16, "mask_le": BF16, "mask_gt": BF16, "ones_b": BF16, "onespad": BF16}


def build_program(S, L, debug=()):
    NT = S // 128
    NCH = S // 64
    nc = bass.Bass("TRN2", target_bir_lowering=False)
    es = ExitStack()
    kb = KB(nc, es)
    dbg = set(debug)

    def din(name, shape, dt=F32):
        return nc.dram_tensor(name, list(shape), dt, kind="ExternalInput").ap()

    def dscr(name, shape, dt=F32):
        kind = "ExternalOutput" if name in dbg else "Internal"
        return nc.dram_tensor(name, list(shape), dt, kind=kind).ap()

    x = din("x", [S, D])
    cvec = din("c", [1, D])
    w_ada = din("w_ada", [L, D, NMOD * D])
    b_ada = din("b_ada", [L, NMOD * D])
    norm_g = din("norm_g", [L, 3 * D])
    w_gate = din("ffn_w_gate", [L, 2, D, DFF])
    w_up = din("ffn_w_up", [L, 2, D, DFF])
    w_down = din("ffn_w_down", [L, 2, DFF, D])
    w_in = din("w_in", [L, D, INCOLS])
    fox_b = din("fox_b_forget", [L, 8])
    sinks = din("attn_sinks", [L, 8])
    gla_wg = din("gla_w_gate", [L, 16, 256])
    gla_bg = din("gla_b_gate", [L, 256])
    gla_ng = din("gla_norm_g", [L, 128])
    ret_w = din("ret_gn_w", [L, 512])
    ret_b = din("ret_gn_b", [L, 512])
    w_branch = din("w_branch", [L, 4, 512, D])
    w_merge = din("w_merge", [L, 4, D, D])
    b_merge = din("b_merge", [L, 4 * D])
    w_out = din("w_out", [L, D, D])
    fin_g = din("final_norm_g", [1, D])
    hc = host_consts(S)
    cin = {k: din("k_" + k, v.shape, CONST_DT.get(k, F32)) for k, v in hc.items()}
    out = nc.dram_tensor("out", [S, D], F32, kind="ExternalOutput").ap()

    hT = dscr("hT", [D, S])
    uT = dscr("uT", [D, S], BF16)
    qTa = dscr("qTa", [512, S], BF16)
    kTa = dscr("kTa", [128, S], BF16)
    va = dscr("va", [S, 128], BF16)
    qTb = dscr("qTb", [512, S], BF16)
    kTb = dscr("kTb", [512, S], BF16)
    vb = dscr("vb", [S, 512], BF16)
    fb = dscr("fb", [S, 8])
    qTc = dscr("qTc", [256, S])
    kTc = dscr("kTc", [256, S])
    kc_tok = dscr("kc_tok", [S, 256])
    vc = dscr("vc", [S, 512], BF16)
    lrT = dscr("lrT", [16, S])
    rc = dscr("rc", [S, 512])
    qTd = dscr("qTd", [256, S])
    qrTd = dscr("qrTd", [256, S])
    kTd = dscr("kTd", [256, S])
    krTd = dscr("krTd", [256, S])
    kd_tok = dscr("kd_tok", [S, 256])
    vd = dscr("vd", [S, 512], BF16)
    gd = dscr("gd", [S, 512])
    oT = [dscr("oT%d" % i, [512, S], BF16) for i in range(4)]

    uid = [0]

    def sb(stack, name, shape, dt=F32):
        uid[0] += 1
        return stack.enter_context(nc.sbuf_tensor("%s_%d" % (name, uid[0]), list(shape), dt))

    K = {}
    KBUF = Buf()
    for k, v in hc.items():
        if k in ("cosT", "sinT", "cos_tok", "sin_tok"):
            continue
        K[k] = sb(es, "K_" + k, v.shape, CONST_DT.get(k, F32))
        kb.dma("sp", K[k][:], cin[k], writes=[KBUF])
    condT = sb(es, "condT", [128, DC], BF16)
    modS = sb(es, "modS", [128, NMOD * DC])
    ngS = sb(es, "ngS", [128, 3 * DC])
    Avec = sb(es, "Avec", [128, 3 * DC])
    Gvec = sb(es, "Gvec", [128, 3 * DC])
    finA = sb(es, "finA", [128, DC])
    zeroB = sb(es, "zeroB", [128, 1])
    VB = Buf()
    kb.op("dve", lambda e: e.memset(zeroB[:], 0.0), writes=[VB])
    epsD = sb(es, "epsD", [128, 2])
    kb.op("dve", lambda e: e.memset(epsD[:, 0:1], float(D * EPS)), writes=[VB])
    kb.op("dve", lambda e: e.memset(epsD[:, 1:2], float(EPS)), writes=[VB])

    NB = 7
    banks = [es.enter_context(nc.psum_tensor("bank%d" % i, [128, 512], F32)) for i in range(NB)]
    bbuf = [Buf() for _ in range(NB)]
    trps = es.enter_context(nc.psum_tensor("trps", [128, 4, 64], BF16))
    trb = Buf()

    SQD = float(np.sqrt(D))

    def stage_transpose_in():
        with ExitStack() as st:
            xs = [sb(st, "ti_x%d" % i, [128, D]) for i in range(2)]
            xb = [Buf() for _ in range(2)]
            ys = [sb(st, "ti_y%d" % i, [128, DC, 128]) for i in range(2)]
            yb = [Buf() for _ in range(2)]
            hTv = hT.rearrange("(dc p) s -> p dc s", p=128)
            for nt in range(NT):
                b = nt % 2
                kb.dma("sp", xs[b][:], x[nt * 128:(nt + 1) * 128, :], writes=[xb[b]])
                for g in range(4):
                    bk = (nt * 4 + g) % NB
                    for i in range(4):
                        dc = g * 4 + i
                        kb.op("pe", lambda e, dc=dc, i=i, bk=bk, b=b: e.transpose(
                            banks[bk][:, i * 128:(i + 1) * 128], xs[b][:, dc * 128:(dc + 1) * 128], K["ident_f"][:]),
                            reads=[xb[b], KBUF], writes=[bbuf[bk]])
                    eng = "act" if g % 2 == 0 else "dve"
                    if eng == "act":
                        kb.op("act", lambda e, g=g, bk=bk, b=b: e.activation(
                            out=ys[b][:, g * 4:(g + 1) * 4, :], in_=banks[bk][:].rearrange("p (a t) -> p a t", t=128),
                            func=AF.Copy), reads=[bbuf[bk]], writes=[yb[b]])
                    else:
                        kb.op("dve", lambda e, g=g, bk=bk, b=b: e.tensor_copy(
                            out=ys[b][:, g * 4:(g + 1) * 4, :], in_=banks[bk][:].rearrange("p (a t) -> p a t", t=128)),
                            reads=[bbuf[bk]], writes=[yb[b]])
                kb.dma("sp", hTv[:, :, nt * 128:(nt + 1) * 128], ys[b][:], reads=[yb[b]])
            kb.barrier()

    def row_to_cols(ps_ap_fn, row_sb, ncols_chunks, bk, rbuf, first=True):
        for j in range(ncols_chunks):
            kb.op("pe", lambda e, j=j: e.matmul(ps_ap_fn(j), row_sb[0:1, j * 128:(j + 1) * 128], K["ones_f"][0:1, 0:1],
                                                start=(first and j == 0), stop=True, skip_group_check=True),
                  reads=[rbuf, KBUF], writes=[bbuf[bk]])

    def stage_cond():
        with ExitStack() as st:
            crow = sb(st, "crow", [1, D])
            frow = sb(st, "frow", [1, D])
            rb = Buf()
            kb.dma("sp", crow[:], cvec[:, :], writes=[rb])
            kb.dma("sp", frow[:], fin_g[:, :], writes=[rb])
            row_to_cols(lambda j: banks[0][:, j:j + 1], crow, DC, 0, rb)
            kb.op("act", lambda e: e.activation(out=condT[:], in_=banks[0][:, 0:DC], func=AF.Silu),
                  reads=[bbuf[0]], writes=[VB])
            row_to_cols(lambda j: banks[1][:, j:j + 1], frow, DC, 1, rb)
            kb.op("dve", lambda e: e.tensor_scalar(out=finA[:], in0=banks[1][:, 0:DC], scalar1=SQD, scalar2=None,
                                                   op0=ALU.mult), reads=[bbuf[1]], writes=[VB])
            kb.barrier()

    def stage_mod(l):
        with ExitStack() as st:
            NCc = NMOD * DC
            wa = [sb(st, "wa%d" % i, [128, NMOD * D], BF16) for i in range(2)]
            wab = [Buf() for _ in range(2)]
            brow = sb(st, "brow", [1, NMOD * D])
            grow = sb(st, "grow", [1, 3 * D])
            rb = Buf()
            kb.dma("sp", brow[:], b_ada[l:l + 1, :], writes=[rb])
            kb.dma("sp", grow[:], norm_g[l:l + 1, :], writes=[rb])
            for kc in range(DC):
                b = kc % 2
                kb.dma("pool", wa[b][:], w_ada[l, kc * 128:(kc + 1) * 128, :], writes=[wab[b]])
                for j in range(NCc):
                    kb.op("pe", lambda e, j=j, b=b, kc=kc: e.matmul(
                        banks[0][:, j:j + 1], wa[b][:, j * 128:(j + 1) * 128], condT[:, kc:kc + 1],
                        start=(kc == 0 and j == 0), stop=False, skip_group_check=True),
                        reads=[wab[b], VB], writes=[bbuf[0]])
            row_to_cols(lambda j: banks[0][:, j:j + 1], brow, NCc, 0, rb, first=False)
            row_to_cols(lambda j: banks[1][:, j:j + 1], grow, 3 * DC, 1, rb)
            kb.op("act", lambda e: e.activation(out=modS[:], in_=banks[0][:, 0:NCc], func=AF.Copy),
                  reads=[bbuf[0]], writes=[VB])
            kb.op("act", lambda e: e.activation(out=ngS[:], in_=banks[1][:, 0:3 * DC], func=AF.Copy),
                  reads=[bbuf[1]], writes=[VB])
            for k in range(3):
                sc = modS[:, (3 * k + 1) * DC:(3 * k + 2) * DC]
                g = modS[:, (3 * k + 2) * DC:(3 * k + 3) * DC]
                kb.op("dve", lambda e, k=k, sc=sc: e.scalar_tensor_tensor(
                    out=Avec[:, k * DC:(k + 1) * DC], in0=sc, scalar=1.0, in1=ngS[:, k * DC:(k + 1) * DC],
                    op0=ALU.add, op1=ALU.mult), reads=[VB], writes=[VB])
                kb.op("dve", lambda e, k=k: e.tensor_scalar(
                    out=Avec[:, k * DC:(k + 1) * DC], in0=Avec[:, k * DC:(k + 1) * DC], scalar1=SQD, scalar2=None,
                    op0=ALU.mult), reads=[VB], writes=[VB])
                kb.op("dve", lambda e, k=k, g=g: e.tensor_scalar(
                    out=Gvec[:, k * DC:(k + 1) * DC], in0=g, scalar1=(1.0 if k == 1 else 0.5), scalar2=None,
                    op0=ALU.mult), reads=[VB], writes=[VB])
            kb.barrier()

    def stage_norm(Aap, Bap, final=False):
        T = min(512, S)
        with ExitStack() as st:
            hs = [sb(st, "nm_h%d" % i, [128, DC, T]) for i in range(2)]
            hb = [Buf() for _ in range(2)]
            sq = sb(st, "nm_sq", [128, DC, T], BF16)
            sqb = Buf()
            rstd = sb(st, "nm_rstd", [128, T])
            rb = Buf()
            tmp = [sb(st, "nm_tmp%d" % i, [128, T]) for i in range(2)]
            tb = [Buf() for _ in range(2)]
            if not final:
                ub = [sb(st, "nm_u%d" % i, [128, DC, T], BF16) for i in range(2)]
            else:
                ub = [sb(st, "nm_u%d" % i, [128, DC, T]) for i in range(1)]
                ot = [sb(st, "nm_o%d" % i, [128, D]) for i in range(2)]
                otb = [Buf() for _ in range(2)]
            ubb = [Buf() for _ in range(2)]
            hTv = hT.rearrange("(dc p) s -> p dc s", p=128)
            uTv = uT.rearrange("(dc p) s -> p dc s", p=128)
            for ti in range(S // T):
                b = ti % 2
                ts = slice(ti * T, (ti + 1) * T)
                kb.dma("sp", hs[b][:], hTv[:, :, ts], writes=[hb[b]])
                for dc in range(DC):
                    eng = "act" if dc % 2 == 0 else "pool"
                    if eng == "act":
                        kb.op("act", lambda e, dc=dc, b=b: e.activation(out=sq[:, dc, :], in_=hs[b][:, dc, :], func=AF.Square),
                              reads=[hb[b]], writes=[sqb])
                    else:
                        kb.op("pool", lambda e, dc=dc, b=b: e.tensor_tensor(out=sq[:, dc, :], in0=hs[b][:, dc, :],
                                                                            in1=hs[b][:, dc, :], op=ALU.mult),
                              reads=[hb[b]], writes=[sqb])
                bk = ti % 2
                for dc in range(DC):
                    kb.op("pe", lambda e, dc=dc, bk=bk: e.matmul(banks[bk][:, 0:T], K["ones_b"][:], sq[:, dc, :],
                                                                 start=(dc == 0), stop=(dc == DC - 1)),
                          reads=[sqb, KBUF], writes=[bbuf[bk]])
                kb.op("act", lambda e, bk=bk: e.activation(out=rstd[:], in_=banks[bk][:, 0:T], func=AF.Ln, bias=epsD[:, 0:1], scale=1.0),
                      reads=[bbuf[bk], VB], writes=[rb])
                kb.op("act", lambda e: e.activation(out=rstd[:], in_=rstd[:], func=AF.Exp, scale=-0.5), reads=[rb], writes=[rb])
                ui = b if not final else 0
                u = ub[ui]
                for dc in range(DC):
                    tb_i = dc % 2
                    if Bap is not None:
                        kb.op("dve", lambda e, dc=dc, b=b, tb_i=tb_i: e.scalar_tensor_tensor(
                            out=tmp[tb_i][:], in0=hs[b][:, dc, :], scalar=Aap[:, dc:dc + 1], in1=rstd[:],
                            op0=ALU.mult, op1=ALU.mult), reads=[hb[b], rb, VB], writes=[tb[tb_i]])
                        kb.op("act", lambda e, dc=dc, tb_i=tb_i, u=u: e.activation(
                            out=u[:, dc, :], in_=tmp[tb_i][:], func=AF.Identity, bias=Bap[:, dc:dc + 1], scale=1.0),
                            reads=[tb[tb_i], VB], writes=[ubb[ui]])
                    else:
                        kb.op("dve", lambda e, dc=dc, b=b, u=u: e.scalar_tensor_tensor(
                            out=u[:, dc, :], in0=hs[b][:, dc, :], scalar=Aap[:, dc:dc + 1], in1=rstd[:],
                            op0=ALU.mult, op1=ALU.mult), reads=[hb[b], rb, VB], writes=[ubb[ui]])
                if not final:
                    kb.dma("sp", uTv[:, :, ts], u[:], reads=[ubb[ui]])
                else:
                    for sub in range(T // 128):
                        ob = (ti * (T // 128) + sub) % 2
                        for g in range(4):
                            bk2 = 2 + (sub * 4 + g) % 5
                            for i in range(4):
                                dc = g * 4 + i
                                kb.op("pe", lambda e, dc=dc, i=i, bk2=bk2, sub=sub, u=u: e.transpose(
                                    banks[bk2][:, i * 128:(i + 1) * 128], u[:, dc, sub * 128:(sub + 1) * 128],
                                    K["ident_f"][:]), reads=[ubb[ui], KBUF], writes=[bbuf[bk2]])
                            if g % 2 == 0:
                                kb.op("act", lambda e, g=g, bk2=bk2, ob=ob: e.activation(
                                    out=ot[ob][:, g * 512:(g + 1) * 512], in_=banks[bk2][:], func=AF.Copy),
                                    reads=[bbuf[bk2]], writes=[otb[ob]])
                            else:
                                kb.op("dve", lambda e, g=g, bk2=bk2, ob=ob: e.tensor_copy(
                                    out=ot[ob][:, g * 512:(g + 1) * 512], in_=banks[bk2][:]),
                                    reads=[bbuf[bk2]], writes=[otb[ob]])
                        r0 = ti * T + sub * 128
                        kb.dma("sp", out[r0:r0 + 128, :], ot[ob][:], reads=[otb[ob]])
            kb.barrier()

    def stage_ffn(l, which, Gap):
        T = min(1024, S)
        NH = T // 512 if T >= 512 else 1
        HW = min(512, T)
        with ExitStack() as st:
            ut = sb(st, "ff_u", [128, DC, T], BF16)
            utb = Buf()
            act = sb(st, "ff_act", [128, FC, T], BF16)
            actb = Buf()
            wbuf = [sb(st, "ff_w%d" % i, [128, 11264], BF16) for i in range(2)]
            wb = [Buf() for _ in range(2)]
            sg = [sb(st, "ff_sg%d" % i, [128, HW]) for i in range(2)]
            sgb = [Buf() for _ in range(2)]
            hr = [sb(st, "ff_hr%d" % i, [128, T]) for i in range(2)]
            hrb = [Buf() for _ in range(2)]
            uTv = uT.rearrange("(dc p) s -> p dc s", p=128)
            wgv = w_gate[l, which].rearrange("(kc p) f -> p kc f", p=128)
            wuv = w_up[l, which].rearrange("(kc p) f -> p kc f", p=128)
            wdv = w_down[l, which].rearrange("(j p) d -> p j d", p=128)
            wi = 0
            pi = 0
            for ti in range(S // T):
                ts = slice(ti * T, (ti + 1) * T)
                kb.dma("sp", ut[:], uTv[:, :, ts], writes=[utb])
                for jj in range(FC // 2):
                    b = wi % 2
                    wi += 1
                    wg = wbuf[b][:, 0:4096].rearrange("p (k f) -> p k f", f=256)
                    wu = wbuf[b][:, 4096:8192].rearrange("p (k f) -> p k f", f=256)
                    kb.dma("pool", wg, wgv[:, :, jj * 256:(jj + 1) * 256], writes=[wb[b]])
                    kb.dma("pool", wu, wuv[:, :, jj * 256:(jj + 1) * 256], writes=[wb[b]])
                    for j2 in range(2):
                        j = jj * 2 + j2
                        for hf in range(NH):
                            bg = 2 * (pi % 3)
                            bu = bg + 1
                            si = pi % 2
                            pi += 1
                            hs_ = slice(hf * HW, (hf + 1) * HW)
                            for kc in range(DC):
                                kb.op("pe", lambda e, kc=kc, bg=bg, wg=wg, j2=j2, hs_=hs_: e.matmul(
                                    banks[bg][:, 0:HW], wg[:, kc, j2 * 128:(j2 + 1) * 128], ut[:, kc, hs_],
                                    start=(kc == 0), stop=(kc == DC - 1)), reads=[wb[b], utb], writes=[bbuf[bg]])
                            for kc in range(DC):
                                kb.op("pe", lambda e, kc=kc, bu=bu, wu=wu, j2=j2, hs_=hs_: e.matmul(
                                    banks[bu][:, 0:HW], wu[:, kc, j2 * 128:(j2 + 1) * 128], ut[:, kc, hs_],
                                    start=(kc == 0), stop=(kc == DC - 1)), reads=[wb[b], utb], writes=[bbuf[bu]])
                            kb.op("act", lambda e, bg=bg, si=si: e.activation(out=sg[si][:], in_=banks[bg][:, 0:HW], func=AF.Silu),
                                  reads=[bbuf[bg]], writes=[sgb[si]])
                            kb.op("dve", lambda e, bu=bu, si=si, j=j, hs_=hs_: e.tensor_tensor(
                                out=act[:, j, hs_], in0=banks[bu][:, 0:HW], in1=sg[si][:], op=ALU.mult),
                                reads=[bbuf[bu], sgb[si]], writes=[actb])
                for g in range(DC // 2):
                    b = wi % 2
                    wi += 1
                    wd = wbuf[b][:, 0:11264].rearrange("p (j d) -> p j d", d=256)
                    kb.dma("pool", wd, wdv[:, :, g * 256:(g + 1) * 256], writes=[wb[b]])
                    for d2 in range(2):
                        dc = g * 2 + d2
                        hb_i = dc % 2
                        kb.dma("sp", hr[hb_i][:], hT[dc * 128:(dc + 1) * 128, ts], writes=[hrb[hb_i]])
                        for hf in range(NH):
                            bk = pi % NB
                            pi += 1
                            hs_ = slice(hf * HW, (hf + 1) * HW)
                            for j in range(FC):
                                kb.op("pe", lambda e, j=j, bk=bk, wd=wd, d2=d2, hs_=hs_: e.matmul(
                                    banks[bk][:, 0:HW], wd[:, j, d2 * 128:(d2 + 1) * 128], act[:, j, hs_],
                                    start=(j == 0), stop=(j == FC - 1)), reads=[wb[b], actb], writes=[bbuf[bk]])
                            kb.op("dve", lambda e, bk=bk, dc=dc, hb_i=hb_i, hs_=hs_: e.scalar_tensor_tensor(
                                out=hr[hb_i][:, hs_], in0=banks[bk][:, 0:HW], scalar=Gap[:, dc:dc + 1], in1=hr[hb_i][:, hs_],
                                op0=ALU.mult, op1=ALU.add), reads=[bbuf[bk], VB, hrb[hb_i]], writes=[hrb[hb_i]])
                        kb.dma("sp", hT[dc * 128:(dc + 1) * 128, ts], hr[hb_i][:], reads=[hrb[hb_i]])
            kb.barrier()

    def stage_inproj(l):
        T = min(1024, S)
        HW = min(512, T)
        NH = T // HW
        W = w_in[l].rearrange("(kc p) c -> p kc c", p=128)
        groups = []
        for c in range(4):
            groups.append(([(A_Q + c * 64, 64), (A_Q + (4 + c) * 64, 64)], [(0, qTa[c * 128:(c + 1) * 128, :], 128, BF16)]))
        groups.append(([(A_K, 128)], [(0, kTa[:, :], 128, BF16)]))
        groups.append(([(B_Q, 512)], [(c * 128, qTb[c * 128:(c + 1) * 128, :], 128, BF16) for c in range(4)]))
        groups.append(([(B_K, 512)], [(c * 128, kTb[c * 128:(c + 1) * 128, :], 128, BF16) for c in range(4)]))
        groups.append(([(C_Q, 512)], [(c * 128, qTc[c * 128:(c + 1) * 128, :], 128, F32) for c in range(2)] +
                       [(256 + c * 128, kTc[c * 128:(c + 1) * 128, :], 128, F32) for c in range(2)]))
        groups.append(([(D_Q, 512)], [(c * 128, qTd[c * 128:(c + 1) * 128, :], 128, F32) for c in range(2)] +
                       [(256 + c * 128, kTd[c * 128:(c + 1) * 128, :], 128, F32) for c in range(2)]))
        for (base, dst) in ((D_Q, qrTd), (D_K, krTd)):
            segs = []
            for h in range(4):
                h0 = base + h * 64
                segs += [(h0 + 32, 32), (h0, 32)]
            groups.append((segs, [(c * 128, dst[c * 128:(c + 1) * 128, :], 128, F32) for c in range(2)]))
        groups.append(([(C_LR, 16)], [(0, lrT[:, :], 16, F32)]))
        tm = [(A_V, 128, va, BF16, False), (B_V, 512, vb, BF16, False), (B_F, 8, fb, F32, False),
              (C_K, 256, kc_tok, F32, False), (C_V, 512, vc, BF16, False), (C_R, 512, rc, F32, True),
              (D_K, 256, kd_tok, F32, False), (D_V, 512, vd, BF16, False), (D_G, 512, gd, F32, True)]
        with ExitStack() as st:
            ut = [sb(st, "ip_u%d" % i, [128, DC, T], BF16) for i in range(2)]
            utb = [Buf() for _ in range(2)]
            wl = [sb(st, "ip_wl%d" % i, [128, DC, 512], BF16) for i in range(2)]
            wlb = [Buf() for _ in range(2)]
            wr = [sb(st, "ip_wr%d" % i, [128, DC, 512], BF16) for i in range(2)]
            wrb = [Buf() for _ in range(2)]
            of = [sb(st, "ip_of%d" % i, [128, T]) for i in range(3)]
            ofb = [Buf() for _ in range(3)]
            obf = [sb(st, "ip_ob%d" % i, [128, T], BF16) for i in range(3)]
            obb = [Buf() for _ in range(3)]
            tf = [sb(st, "ip_tf%d" % i, [128, 512]) for i in range(3)]
            tfb = [Buf() for _ in range(3)]
            tbf = [sb(st, "ip_tb%d" % i, [128, 512], BF16) for i in range(3)]
            tbb = [Buf() for _ in range(3)]
            uTv = uT.rearrange("(dc p) s -> p dc s", p=128)
            wi = 0
            oi = 0
            pi = 0
            for ti in range(S // T):
                ub_ = ti % 2
                ts = slice(ti * T, (ti + 1) * T)
                kb.dma("sp", ut[ub_][:], uTv[:, :, ts], writes=[utb[ub_]])
                for (segs, jobs) in groups:
                    b = wi % 2
                    wi += 1
                    c0 = 0
                    for (col, n) in segs:
                        kb.dma("pool", wl[b][:, :, c0:c0 + n], W[:, :, col:col + n], writes=[wlb[b]])
                        c0 += n
                    for (off, dst, M, dt) in jobs:
                        o = oi % 3
                        oi += 1
                        if dt == BF16:
                            dstt, dstb = obf[o], obb[o]
                        else:
                            dstt, dstb = of[o], ofb[o]
                        for hf in range(NH):
                            hs_ = slice(hf * HW, (hf + 1) * HW)
                            bk = pi % NB
                            pi += 1
                            for kc in range(DC):
                                kb.op("pe", lambda e: e.matmul(banks[bk][0:M, 0:HW], wl[b][:, kc, off:off + M], ut[ub_][:, kc, hs_],
                                                               start=(kc == 0), stop=(kc == DC - 1)),
                                      reads=[wlb[b], utb[ub_]], writes=[bbuf[bk]])
                            if pi % 2 == 0:
                                kb.op("act", lambda e: e.activation(out=dstt[0:M, hs_], in_=banks[bk][0:M, 0:HW], func=AF.Copy),
                                      reads=[bbuf[bk]], writes=[dstb])
                            else:
                                kb.op("dve", lambda e: e.tensor_copy(out=dstt[0:M, hs_], in_=banks[bk][0:M, 0:HW]),
                                      reads=[bbuf[bk]], writes=[dstb])
                        kb.dma("sp", dst[:, ts], dstt[0:M, :], reads=[dstb])
                for (col, n, dst, dt, silu) in tm:
                    b = wi % 2
                    wi += 1
                    kb.dma("pool", wr[b][:, :, 0:n], W[:, :, col:col + n], writes=[wrb[b]])
                    for sub in range(T // 128):
                        bk = pi % NB
                        pi += 1
                        for kc in range(DC):
                            kb.op("pe", lambda e: e.matmul(banks[bk][:, 0:n], ut[ub_][:, kc, sub * 128:(sub + 1) * 128], wr[b][:, kc, 0:n],
                                                           start=(kc == 0), stop=(kc == DC - 1)), reads=[wrb[b], utb[ub_]], writes=[bbuf[bk]])
                        o = oi % 3
                        oi += 1
                        if dt == BF16:
                            dstt, dstb = tbf[o], tbb[o]
                        else:
                            dstt, dstb = tf[o], tfb[o]
                        if silu:
                            kb.op("act", lambda e: e.activation(out=dstt[:, 0:n], in_=banks[bk][:, 0:n], func=AF.Silu),
                                  reads=[bbuf[bk]], writes=[dstb])
                        elif oi % 2 == 0:
                            kb.op("act", lambda e: e.activation(out=dstt[:, 0:n], in_=banks[bk][:, 0:n], func=AF.Copy),
                                  reads=[bbuf[bk]], writes=[dstb])
                        else:
                            kb.op("dve", lambda e: e.tensor_copy(out=dstt[:, 0:n], in_=banks[bk][:, 0:n]),
                                  reads=[bbuf[bk]], writes=[dstb])
                        r0 = ti * T + sub * 128
                        kb.dma("sp", dst[r0:r0 + 128, :], dstt[:, 0:n], reads=[dstb])
            kb.barrier()

    def stage_attn(l, fox):
        npair = 4
        nkc = 4 if fox else 1
        with ExitStack() as st:
            QT = sb(st, "at_q", [128, 4, S], BF16)
            KT = sb(st, "at_k", [128, nkc, S], BF16)
            VP = sb(st, "at_v", [128, NT, nkc, 192], BF16)
            OT = sb(st, "at_o", [128, 4, S], BF16)
            LB = Buf()
            OB = Buf()
            NPT = 6
            PT = [sb(st, "at_p%d" % i, [128, 128], BF16) for i in range(NPT)]
            PB = [Buf() for _ in range(NPT)]
            rd = [sb(st, "at_rd%d" % i, [128, 128]) for i in range(2)]
            rdb = [Buf() for _ in range(2)]
            kb.op("pool", lambda e: e.memset(VP[:], 0.0), writes=[LB])
            qsrc = qTb if fox else qTa
            ksrc = kTb if fox else kTa
            vsrc = vb if fox else va
            kb.dma("sp", QT[:], qsrc.rearrange("(c p) s -> p c s", p=128), writes=[LB])
            kb.dma("sp", KT[:], ksrc.rearrange("(c p) s -> p c s", p=128), writes=[LB])
            vv = vsrc.rearrange("(n p) (c h d) -> p n c h d", p=128, h=2, d=64)
            for c in range(nkc):
                kb.dma("sp", VP[:, :, c, 0:64], vv[:, :, c, 0, :], writes=[LB])
                kb.dma("sp", VP[:, :, c, 128:192], vv[:, :, c, 1, :], writes=[LB])
            if fox:
                fz = sb(st, "at_fz", [128, NT, 8])
                fl = sb(st, "at_fl", [128, NT, 8])
                fP = sb(st, "at_fP", [128, NT, 8])
                Lc = sb(st, "at_Lc", [128, NT, 8])
                Lr = sb(st, "at_Lr", [128, NT, 8])
                bfo = sb(st, "at_bf", [128, 8])
                bias = sb(st, "at_bias", [128, 8, NT, NT])
                FB = Buf()
                kb.dma("sp", fz[:], fb.rearrange("(n p) h -> p n h", p=128), writes=[FB])
                kb.dma("sp", bfo[:], fox_b[l, :].partition_broadcast(128), writes=[FB])
                kb.op("dve", lambda e: e.tensor_tensor(out=fz[:], in0=fz[:], in1=bfo[:].unsqueeze(1).broadcast_to([128, NT, 8]),
                                                       op=ALU.add), reads=[FB], writes=[FB])
                kb.op("act", lambda e: e.activation(out=fl[:], in_=fz[:], func=AF.Exp, scale=-1.0), reads=[FB], writes=[FB])
                kb.op("act", lambda e: e.activation(out=fl[:], in_=fl[:], func=AF.Ln, bias=1.0, scale=1.0), reads=[FB], writes=[FB])
                kb.op("dve", lambda e: e.memset(fP[:, 0, :], 0.0), reads=[FB], writes=[FB])
                for n in range(1, NT):
                    kb.op("dve", lambda e, n=n: e.tensor_tensor(out=fP[:, n, :], in0=fP[:, n - 1, :], in1=fl[:, n - 1, :], op=ALU.add),
                          reads=[FB], writes=[FB])
                for n in range(NT):
                    kb.op("pe", lambda e, n=n: e.matmul(banks[0][:, n * 8:(n + 1) * 8], K["mask_le_f"][:], fl[:, n, :],
                                                        start=(n == 0), stop=False, skip_group_check=True),
                          reads=[FB, KBUF], writes=[bbuf[0]])
                    kb.op("pe", lambda e, n=n: e.matmul(banks[0][:, n * 8:(n + 1) * 8], K["ones_f"][:], fP[:, n, :],
                                                        start=False, stop=True, skip_group_check=True),
                          reads=[FB, KBUF], writes=[bbuf[0]])
                kb.op("act", lambda e: e.activation(out=Lc[:].rearrange("p n h -> p (n h)"), in_=banks[0][:, 0:NT * 8], func=AF.Copy),
                      reads=[bbuf[0]], writes=[FB])
                for n0 in range(0, NT * 8, 512):
                    n1 = min(NT * 8, n0 + 512)
                    kb.op("pe", lambda e, n0=n0, n1=n1: e.matmul(banks[1][:, 0:n1 - n0], K["sel63"][:],
                                                                 Lc[:].rearrange("p n h -> p (n h)")[:, n0:n1], start=True, stop=True),
                          reads=[FB, KBUF], writes=[bbuf[1]])
                    kb.op("act", lambda e, n0=n0, n1=n1: e.activation(out=Lr[:].rearrange("p n h -> p (n h)")[:, n0:n1],
                                                                      in_=banks[1][:, 0:n1 - n0], func=AF.Copy),
                          reads=[bbuf[1]], writes=[FB])
                for h in range(8):
                    kb.op("dve", lambda e, h=h: e.tensor_tensor(
                        out=bias[:, h, :, :], in0=Lc[:, :, h].unsqueeze(1).broadcast_to([128, NT, NT]),
                        in1=Lr[:, :, h].unsqueeze(2).broadcast_to([128, NT, NT]), op=ALU.subtract),
                        reads=[FB], writes=[FB])
            else:
                es_ = sb(st, "at_es", [128, 8])
                esp = sb(st, "at_esp", [128, 4])
                FB = Buf()
                kb.dma("sp", es_[:], sinks[l, :].partition_broadcast(128), writes=[FB])
                kb.op("act", lambda e: e.activation(out=es_[:], in_=es_[:], func=AF.Exp), reads=[FB], writes=[FB])
                kb.op("dve", lambda e: e.tensor_copy(out=esp[0:64, :], in_=es_[0:64, 0:4]), reads=[FB], writes=[FB])
                kb.op("dve", lambda e: e.tensor_copy(out=esp[64:128, :], in_=es_[64:128, 4:8]), reads=[FB], writes=[FB])

            gi = 0
            si = 0
            for c in range(npair):
                kc_ = c if fox else 0
                for i in range(NT):
                    bo = gi % 2
                    bd = bo
                    gi += 1
                    steps = []
                    for hf in range(2):
                        js = list(range(i + 1)) if fox else ([i - 1, i] if i > 0 else [i])
                        for j in js:
                            steps.append((hf, j))
                    isl = slice(i * 128, (i + 1) * 128)
                    pend = []

                    def emit_score(k):
                        hf, j = steps[k]
                        bs = 2 + (si + k) % 5
                        rs = slice(hf * 64, (hf + 1) * 64)
                        kb.op("pe", lambda e: e.matmul(banks[bs][:, 0:128], KT[rs, kc_, j * 128:(j + 1) * 128], QT[rs, c, isl],
                                                       start=True, stop=True), reads=[LB], writes=[bbuf[bs]])
                        pb = (si + k) % NPT
                        h = (2 * c + hf) if fox else None
                        if fox:
                            kb.op("act", lambda e: e.activation(out=PT[pb][:], in_=banks[bs][:, 0:128], func=AF.Exp,
                                                                bias=bias[:, h, i, j:j + 1], scale=0.125),
                                  reads=[bbuf[bs], FB], writes=[PB[pb]])
                        else:
                            kb.op("act", lambda e: e.activation(out=PT[pb][:], in_=banks[bs][:, 0:128], func=AF.Exp, scale=0.125),
                                  reads=[bbuf[bs]], writes=[PB[pb]])
                        if j == i:
                            kb.op("pool", lambda e: e.tensor_tensor(out=PT[pb][:], in0=PT[pb][:], in1=K["mask_le"][:], op=ALU.mult),
                                  reads=[PB[pb], KBUF], writes=[PB[pb]])
                        elif not fox:
                            kb.op("pool", lambda e: e.tensor_tensor(out=PT[pb][:], in0=PT[pb][:], in1=K["mask_gt"][:], op=ALU.mult),
                                  reads=[PB[pb], KBUF], writes=[PB[pb]])

                    def emit_pv(k):
                        hf, j = steps[k]
                        pb = (si + k) % NPT
                        first = (k == 0)
                        last = (k == len(steps) - 1)
                        kb.op("pe", lambda e: e.matmul(banks[bo][:, 0:128], VP[:, j, kc_, hf * 64:hf * 64 + 128], PT[pb][:],
                                                       start=first, stop=last, skip_group_check=True), reads=[LB, PB[pb]], writes=[bbuf[bo]])
                        kb.op("pe", lambda e: e.matmul(banks[bd][:, 128:256], K["onespad"][:, hf * 64:hf * 64 + 128], PT[pb][:],
                                                       start=False, stop=last, skip_group_check=True), reads=[KBUF, PB[pb]], writes=[bbuf[bd]])

                    LOOK = 4
                    for k in range(len(steps) + LOOK):
                        if k < len(steps):
                            emit_score(k)
                        if k - LOOK >= 0:
                            emit_pv(k - LOOK)
                    si += len(steps)
                    r = gi % 2
                    if fox:
                        kb.op("dve", lambda e, r=r, bd=bd: e.reciprocal(out=rd[r][:], in_=banks[bd][:, 128:256]),
                              reads=[bbuf[bd]], writes=[rdb[r]])
                    else:
                        kb.op("dve", lambda e, r=r, bd=bd, c=c: e.tensor_scalar(out=rd[r][:], in0=banks[bd][:, 128:256],
                                                                               scalar1=esp[:, c:c + 1], scalar2=None, op0=ALU.add),
                              reads=[bbuf[bd], FB], writes=[rdb[r]])
                        kb.op("dve", lambda e, r=r: e.reciprocal(out=rd[r][:], in_=rd[r][:]), reads=[rdb[r]], writes=[rdb[r]])
                    kb.op("dve", lambda e, r=r, bo=bo, c=c, isl=isl: e.tensor_tensor(out=OT[:, c, isl], in0=banks[bo][:, 0:128],
                                                                                    in1=rd[r][:], op=ALU.mult),
                          reads=[bbuf[bo], rdb[r]], writes=[OB])
            dst = oT[1] if fox else oT[0]
            kb.dma("sp", dst.rearrange("(c p) s -> p c s", p=128), OT[:], reads=[OB])
            kb.barrier()

    def stage_linattn(l, ret):
        with ExitStack() as st:
            qin = sb(st, "la_qin", [64, 4, S], BF16)
            kin = sb(st, "la_kin", [64, 4, S], BF16)
            OTs = sb(st, "la_o", [128, 4, S], BF16)
            LB = Buf()
            QB = Buf()
            OB = Buf()
            dec = sb(st, "la_dec", [64, 4, NCH])
            DB = Buf()
            Sst = sb(st, "la_S", [64, 4, 128])
            Sbf = sb(st, "la_Sb", [64, 4, 128], BF16)
            SB_ = Buf()
            SBb = Buf()
            ksr = [sb(st, "la_ks%d" % i, [64, 256], BF16) for i in range(3)]
            ksb = [Buf() for _ in range(3)]
            vt = [sb(st, "la_v%d" % i, [64, 512], BF16) for i in range(3)]
            vtb = [Buf() for _ in range(3)]
            gt = [sb(st, "la_g%d" % i, [64, 512]) for i in range(3)]
            gtb = [Buf() for _ in range(3)]
            kt = [sb(st, "la_kt%d" % i, [64, 256]) for i in range(3)]
            ktb = [Buf() for _ in range(3)]
            sm = [sb(st, "la_sm%d" % i, [64, 4, 64], BF16) for i in range(3)]
            smb = [Buf() for _ in range(3)]
            ss = sb(st, "la_ss", [64, 8])
            st2 = sb(st, "la_st2", [64, 8])
            junk = sb(st, "la_junk", [64, 128])
            t1 = [sb(st, "la_t1%d" % i, [64, 512]) for i in range(3)]
            t1b = [Buf() for _ in range(3)]
            ob16 = [sb(st, "la_ob%d" % i, [64, 512], BF16) for i in range(3)]
            obb = [Buf() for _ in range(3)]
            SS = Buf()
            gtab = sb(st, "la_gtab", [64, 512])
            btab = sb(st, "la_btab", [64, 512])
            TB = Buf()
            kb.op("dve", lambda e: e.memset(Sst[:], 0.0), writes=[SB_])
            kb.op("pool", lambda e: e.memset(Sbf[:], 0.0), writes=[SBb])
            qsrc, ksrc, ktok, vsrc, gsrc, odst = (qTd, kTd, kd_tok, vd, gd, oT[3]) if ret else (qTc, kTc, kc_tok, vc, rc, oT[2])
            qsv = qsrc.rearrange("(h p) s -> p h s", p=64)
            ksv = ksrc.rearrange("(h p) s -> p h s", p=64)
            if ret:
                ctk = sb(st, "la_ctk", [64, NCH, 32])
                stk = sb(st, "la_stk", [64, NCH, 32])
                kb.dma("sp", ctk[:], cin["cos_tok"], writes=[LB])
                kb.dma("sp", stk[:], cin["sin_tok"], writes=[LB])
                kb.dma("sp", gtab[:], ret_w[l, :].partition_broadcast(64), writes=[TB])
                kb.dma("sp", btab[:], ret_b[l, :].partition_broadcast(64), writes=[TB])
                with ExitStack() as st_pre:
                    cosT = sb(st_pre, "la_cos", [64, S])
                    sinT = sb(st_pre, "la_sin", [64, S])
                    SH = S // 2
                    qx = sb(st_pre, "la_qx", [64, SH])
                    qr = sb(st_pre, "la_qr", [64, SH])
                    kb.dma("sp", cosT[:], cin["cosT"][0:64, :], writes=[LB])
                    kb.dma("sp", sinT[:], cin["sinT"][0:64, :], writes=[LB])
                    for (srcv, rsrc, dstt, tab) in ((qsv, qrTd, qin, "ret_dq"), (ksv, krTd, kin, "ret_dk")):
                        rsv = rsrc.rearrange("(h p) s -> p h s", p=64)
                        for h in range(4):
                            for hh in range(2):
                                hsl = slice(hh * SH, (hh + 1) * SH)
                                kb.dma("sp", qx[:], srcv[:, h, hsl], reads=[LB], writes=[LB])
                                kb.dma("sp", qr[:], rsv[:, h, hsl], reads=[LB], writes=[LB])
                                kb.op("dve", lambda e: e.tensor_tensor(out=qx[:], in0=qx[:], in1=cosT[:, hsl], op=ALU.mult),
                                      reads=[LB], writes=[LB])
                                kb.op("pool", lambda e: e.tensor_tensor(out=qr[:], in0=qr[:], in1=sinT[:, hsl], op=ALU.mult),
                                      reads=[LB], writes=[LB])
                                kb.op("dve", lambda e: e.tensor_tensor(out=qx[:], in0=qx[:], in1=qr[:], op=ALU.add),
                                      reads=[LB], writes=[LB])
                                kb.op("pool", lambda e: e.tensor_tensor(
                                    out=dstt[:, h, hsl].rearrange("p (n j) -> p n j", j=64),
                                    in0=qx[:].rearrange("p (n j) -> p n j", j=64),
                                    in1=K[tab][:, h, :].unsqueeze(1).broadcast_to([64, NCH // 2, 64]), op=ALU.mult),
                                    reads=[LB, KBUF], writes=[QB])
                    kb.barrier()
                kb.op("dve", lambda e: e.tensor_copy(out=dec[:], in_=K["ret_dec"][:].unsqueeze(2).broadcast_to([64, 4, NCH])),
                      reads=[KBUF], writes=[DB])
            else:
                lr = sb(st, "la_lr", [33, S])
                wga = sb(st, "la_wga", [33, 256])
                kb.op("dve", lambda e: e.memset(lr[:], 1.0), writes=[LB])
                kb.op("dve", lambda e: e.memset(wga[:], 0.0), writes=[TB])
                kb.dma("sp", lr[0:16, :], lrT[:, :], reads=[LB], writes=[LB])
                kb.dma("sp", wga[0:16, :], gla_wg[l], reads=[TB], writes=[TB])
                kb.dma("sp", wga[32:33, :], gla_bg[l:l + 1, :], reads=[TB], writes=[TB])
                kb.dma("sp", gtab[:, 0:128], gla_ng[l, :].partition_broadcast(64), writes=[TB])
                lsb = [sb(st, "la_l%d" % i, [64, 256]) for i in range(3)]
                lbb = [Buf() for _ in range(3)]
                eq = [sb(st, "la_eq%d" % i, [64, 4, 64]) for i in range(3)]
                eqb = [Buf() for _ in range(3)]
                ek = [sb(st, "la_ek%d" % i, [64, 4, 64]) for i in range(3)]
                ekb = [Buf() for _ in range(3)]
                ed = [sb(st, "la_ed%d" % i, [64, 256]) for i in range(3)]
                edb = [Buf() for _ in range(3)]
                qch = [sb(st, "la_qch%d" % i, [64, 4, 64]) for i in range(3)]
                kch = [sb(st, "la_kch%d" % i, [64, 4, 64]) for i in range(3)]
                qcb = [Buf() for _ in range(3)]

            def phase(n, ph):
                b = n % 3
                cs = slice(n * 64, (n + 1) * 64)
                bo = 4 + n % 2
                o3 = banks[bo][0:64, :].rearrange("p (h d) -> p h d", h=4)
                if ph == 0:
                    kb.dma("sp", vt[b][:], vsrc[n * 64:(n + 1) * 64, :], writes=[vtb[b]])
                    kb.dma("sp", gt[b][:], gsrc[n * 64:(n + 1) * 64, :], writes=[gtb[b]])
                    kb.dma("sp", kt[b][:], ktok[n * 64:(n + 1) * 64, :], writes=[ktb[b]])
                    if not ret:
                        kb.dma("sp", qch[b][:], qsv[:, :, cs], writes=[qcb[b]])
                        kb.dma("sp", kch[b][:], ksv[:, :, cs], writes=[qcb[b]])
                        kb.op("pe", lambda e: e.matmul(banks[0][0:64, 0:256], lr[:, cs], wga[:], start=True, stop=True),
                              reads=[LB, TB], writes=[bbuf[0]])
                        kb.op("act", lambda e: e.activation(out=lsb[b][:], in_=banks[0][0:64, 0:256], func=AF.Exp, scale=-1.0),
                              reads=[bbuf[0]], writes=[lbb[b]])
                        kb.op("act", lambda e: e.activation(out=lsb[b][:], in_=lsb[b][:], func=AF.Ln, bias=1.0, scale=1.0),
                              reads=[lbb[b]], writes=[lbb[b]])
                        for h in range(4):
                            kb.op("pe", lambda e: e.matmul(banks[1][0:64, h * 64:(h + 1) * 64], lsb[b][:, h * 64:(h + 1) * 64],
                                                           K["tri64n"][:], start=(h == 0), stop=True, skip_group_check=True),
                                  reads=[lbb[b], KBUF], writes=[bbuf[1]])
                        kb.op("pe", lambda e: e.matmul(banks[2][0:64, 0:256], K["d64n"][:], lsb[b][:], start=True, stop=True),
                              reads=[lbb[b], KBUF], writes=[bbuf[2]])
                        kb.op("act", lambda e: e.activation(out=eq[b][:].rearrange("p c j -> p (c j)"), in_=banks[1][0:64, 0:256], func=AF.Exp),
                              reads=[bbuf[1]], writes=[eqb[b]])
                        kb.op("act", lambda e: e.activation(out=ek[b][:].rearrange("p c j -> p (c j)"), in_=banks[1][0:64, 0:256], func=AF.Exp, scale=-1.0),
                              reads=[bbuf[1]], writes=[ekb[b]])
                        kb.op("act", lambda e: e.activation(out=ed[b][:], in_=banks[2][0:64, 0:256], func=AF.Exp),
                              reads=[bbuf[2]], writes=[edb[b]])
                        kb.op("dve", lambda e: e.scalar_tensor_tensor(out=qin[:, :, cs], in0=qch[b][:], scalar=0.125, in1=eq[b][:],
                                                                      op0=ALU.mult, op1=ALU.mult), reads=[qcb[b], eqb[b]], writes=[QB])
                        kb.op("pool", lambda e: e.tensor_tensor(out=kin[:, :, cs], in0=kch[b][:], in1=ek[b][:], op=ALU.mult),
                              reads=[qcb[b], ekb[b]], writes=[QB])
                        kb.op("dve", lambda e: e.tensor_copy(out=dec[:, :, n:n + 1], in_=eq[b][:, :, 63:64]),
                              reads=[eqb[b]], writes=[DB])
                        kb.op("dve", lambda e: e.tensor_tensor(out=ksr[b][:], in0=kt[b][:], in1=ed[b][:], op=ALU.mult),
                              reads=[ktb[b], edb[b]], writes=[ksb[b]])
                    else:
                        k4 = kt[b][:].rearrange("p (h two f) -> p h two f", h=4, two=2)
                        o4 = t1[b][:, 0:256].rearrange("p (h two f) -> p h two f", h=4, two=2)
                        j4 = t1[b][:, 256:512].rearrange("p (h two f) -> p h two f", h=4, two=2)
                        cb = ctk[:, n, :].unsqueeze(1).broadcast_to([64, 4, 32])
                        sbn = stk[:, n, :].unsqueeze(1).broadcast_to([64, 4, 32])
                        rd_ = [ktb[b], LB]
                        kb.op("dve", lambda e: e.tensor_tensor(out=o4[:, :, 0, :], in0=k4[:, :, 0, :], in1=cb, op=ALU.mult), reads=rd_, writes=[t1b[b]])
                        kb.op("pool", lambda e: e.tensor_tensor(out=j4[:, :, 0, :], in0=k4[:, :, 1, :], in1=sbn, op=ALU.mult), reads=rd_, writes=[t1b[b]])
                        kb.op("dve", lambda e: e.tensor_tensor(out=o4[:, :, 0, :], in0=o4[:, :, 0, :], in1=j4[:, :, 0, :], op=ALU.subtract),
                              reads=[t1b[b]], writes=[t1b[b]])
                        kb.op("dve", lambda e: e.tensor_tensor(out=o4[:, :, 1, :], in0=k4[:, :, 1, :], in1=cb, op=ALU.mult), reads=rd_, writes=[t1b[b]])
                        kb.op("pool", lambda e: e.tensor_tensor(out=j4[:, :, 1, :], in0=k4[:, :, 0, :], in1=sbn, op=ALU.mult), reads=rd_, writes=[t1b[b]])
                        kb.op("dve", lambda e: e.tensor_tensor(out=o4[:, :, 1, :], in0=o4[:, :, 1, :], in1=j4[:, :, 1, :], op=ALU.add),
                              reads=[t1b[b]], writes=[t1b[b]])
                        kb.op("dve", lambda e: e.tensor_tensor(
                            out=ksr[b][:].rearrange("p (h d) -> p h d", h=4), in0=t1[b][:, 0:256].rearrange("p (h d) -> p h d", h=4),
                            in1=K["ret_ks"][:].unsqueeze(2).broadcast_to([64, 4, 64]), op=ALU.mult),
                            reads=[t1b[b], KBUF], writes=[ksb[b]])
                if ph == 1:
                    bs = 3
                    for h in range(4):
                        kb.op("pe", lambda e: e.matmul(banks[bs][0:64, h * 64:(h + 1) * 64], kin[:, h, cs], qin[:, h, cs],
                                                       start=(h == 0), stop=True, skip_group_check=True),
                              reads=[QB], writes=[bbuf[bs]])
                    kb.op("dve", lambda e: e.tensor_tensor(out=sm[b][:], in0=banks[bs][0:64, 0:256].rearrange("p (h i) -> p h i", h=4),
                                                           in1=K["mask_le"][0:64, 0:64].unsqueeze(1).broadcast_to([64, 4, 64]), op=ALU.mult),
                          reads=[bbuf[bs], KBUF], writes=[smb[b]])
                    for h in range(4):
                        kb.op("pe", lambda e: e.matmul(banks[bo][0:64, h * 128:(h + 1) * 128], qin[:, h, cs], Sbf[:, h, :],
                                                       start=(h == 0), stop=False, skip_group_check=True),
                              reads=[QB, SBb], writes=[bbuf[bo]])
                        kb.op("pe", lambda e: e.matmul(banks[bo][0:64, h * 128:(h + 1) * 128], sm[b][:, h, :], vt[b][:, h * 128:(h + 1) * 128],
                                                       start=False, stop=True, skip_group_check=True),
                              reads=[smb[b], vtb[b]], writes=[bbuf[bo]])
                    bu = 6
                    for h in range(4):
                        kb.op("pe", lambda e: e.matmul(banks[bu][0:64, h * 128:(h + 1) * 128], ksr[b][:, h * 64:(h + 1) * 64], vt[b][:, h * 128:(h + 1) * 128],
                                                       start=(h == 0), stop=True, skip_group_check=True), reads=[ksb[b], vtb[b]], writes=[bbuf[bu]])
                    kb.op("dve", lambda e: e.tensor_tensor(out=Sst[:], in0=Sst[:], in1=dec[:, :, n:n + 1].broadcast_to([64, 4, 128]), op=ALU.mult),
                          reads=[SB_, DB], writes=[SB_])
                    kb.op("dve", lambda e: e.tensor_tensor(out=Sst[:], in0=banks[bu][0:64, :].rearrange("p (h d) -> p h d", h=4), in1=Sst[:], op=ALU.add),
                          reads=[SB_, bbuf[bu]], writes=[SB_])
                    kb.op("act", lambda e: e.activation(out=Sbf[:], in_=Sst[:], func=AF.Copy), reads=[SB_], writes=[SBb])
                    kb.op("dve", lambda e: e.memset(ss[:], 0.0), reads=[SS], writes=[SS])
                if ph == 2:
                    for h in range(4):
                        kb.op("act", lambda e: e.activation(out=junk[:], in_=banks[bo][0:64, h * 128:(h + 1) * 128], func=AF.Square,
                                                            accum_out=ss[:, h:h + 1]), reads=[bbuf[bo]], writes=[SS])
                        if ret:
                            kb.op("act", lambda e: e.activation(out=junk[:], in_=banks[bo][0:64, h * 128:(h + 1) * 128], func=AF.Identity,
                                                                accum_out=ss[:, 4 + h:5 + h]), reads=[bbuf[bo]], writes=[SS])
                    if not ret:
                        kb.op("dve", lambda e: e.tensor_scalar(out=st2[:, 0:4], in0=ss[:, 0:4], scalar1=1.0 / 128, scalar2=EPS, op0=ALU.mult, op1=ALU.add),
                              reads=[SS], writes=[SS])
                        kb.op("act", lambda e: e.activation(out=st2[:, 0:4], in_=st2[:, 0:4], func=AF.Ln), reads=[SS], writes=[SS])
                        kb.op("act", lambda e: e.activation(out=st2[:, 0:4], in_=st2[:, 0:4], func=AF.Exp, scale=-0.5), reads=[SS], writes=[SS])
                        kb.op("dve", lambda e: e.tensor_tensor(out=t1[b][:].rearrange("p (h d) -> p h d", h=4), in0=o3,
                                                               in1=st2[:, 0:4].unsqueeze(2).broadcast_to([64, 4, 128]), op=ALU.mult),
                              reads=[bbuf[bo], SS], writes=[t1b[b]])
                        kb.op("pool", lambda e: e.tensor_tensor(out=t1[b][:].rearrange("p (h d) -> p h d", h=4),
                                                                in0=t1[b][:].rearrange("p (h d) -> p h d", h=4),
                                                                in1=gtab[:, 0:128].unsqueeze(1).broadcast_to([64, 4, 128]), op=ALU.mult),
                              reads=[t1b[b], TB], writes=[t1b[b]])
                    else:
                        kb.op("dve", lambda e: e.tensor_scalar(out=st2[:, 4:8], in0=ss[:, 4:8], scalar1=1.0 / 128, scalar2=None, op0=ALU.mult),
                              reads=[SS], writes=[SS])
                        kb.op("dve", lambda e: e.tensor_tensor(out=st2[:, 0:4], in0=st2[:, 4:8], in1=st2[:, 4:8], op=ALU.mult), reads=[SS], writes=[SS])
                        kb.op("dve", lambda e: e.scalar_tensor_tensor(out=st2[:, 0:4], in0=ss[:, 0:4], scalar=1.0 / 128, in1=st2[:, 0:4],
                                                                      op0=ALU.mult, op1=ALU.subtract), reads=[SS], writes=[SS])
                        kb.op("act", lambda e: e.activation(out=st2[:, 0:4], in_=st2[:, 0:4], func=AF.Ln, bias=epsD[0:64, 1:2], scale=1.0), reads=[SS, VB], writes=[SS])
                        kb.op("act", lambda e: e.activation(out=st2[:, 0:4], in_=st2[:, 0:4], func=AF.Exp, scale=-0.5), reads=[SS], writes=[SS])
                        kb.op("dve", lambda e: e.tensor_tensor(out=t1[b][:].rearrange("p (h d) -> p h d", h=4), in0=o3,
                                                               in1=st2[:, 4:8].unsqueeze(2).broadcast_to([64, 4, 128]), op=ALU.subtract),
                              reads=[bbuf[bo], SS], writes=[t1b[b]])
                        kb.op("dve", lambda e: e.tensor_tensor(out=t1[b][:].rearrange("p (h d) -> p h d", h=4),
                                                               in0=t1[b][:].rearrange("p (h d) -> p h d", h=4),
                                                               in1=st2[:, 0:4].unsqueeze(2).broadcast_to([64, 4, 128]), op=ALU.mult),
                              reads=[t1b[b], SS], writes=[t1b[b]])
                        kb.op("pool", lambda e: e.tensor_tensor(out=t1[b][:], in0=t1[b][:], in1=gtab[:], op=ALU.mult), reads=[t1b[b], TB], writes=[t1b[b]])
                        kb.op("pool", lambda e: e.tensor_tensor(out=t1[b][:], in0=t1[b][:], in1=btab[:], op=ALU.add), reads=[t1b[b], TB], writes=[t1b[b]])
                    kb.op("pool", lambda e: e.tensor_tensor(out=ob16[b][:], in0=t1[b][:], in1=gt[b][:], op=ALU.mult),
                          reads=[t1b[b], gtb[b]], writes=[obb[b]])
                    for h in range(4):
                        kb.op("pe", lambda e: e.transpose(trps[:, h, :], ob16[b][:, h * 128:(h + 1) * 128], K["ident_b"][0:64, 0:64]),
                              reads=[obb[b], KBUF], writes=[trb])
                    kb.op("act", lambda e: e.activation(out=OTs[:, :, cs], in_=trps[:], func=AF.Copy), reads=[trb], writes=[OB])

            for it in range(NCH + 2):
                if it < NCH:
                    phase(it, 0)
                if 0 <= it - 1 < NCH:
                    phase(it - 1, 1)
                if 0 <= it - 2 < NCH:
                    phase(it - 2, 2)
            kb.dma("sp", odst.rearrange("(c p) s -> p c s", p=128), OTs[:], reads=[OB])
            kb.barrier()

    def stage_merge(l, Gap):
        T = min(1024, S)
        HW = min(512, T)
        NH = T // HW
        GW = 4
        with ExitStack() as st:
            bmS = sb(st, "mg_bm", [128, 4 * DC])
            BM = Buf()
            with ExitStack() as st_pre:
                brow = sb(st_pre, "mg_brow", [1, 4 * D])
                rb = Buf()
                kb.dma("sp", brow[:], b_merge[l:l + 1, :], writes=[rb])
                row_to_cols(lambda j: banks[0][:, j:j + 1], brow, 4 * DC, 0, rb)
                kb.op("act", lambda e: e.activation(out=bmS[:], in_=banks[0][:, 0:4 * DC], func=AF.Copy), reads=[bbuf[0]], writes=[BM])
                kb.barrier()
            ut = sb(st, "mg_u", [128, DC, T], BF16)
            utb = Buf()
            ot = [sb(st, "mg_o%d" % i, [128, 4, T], BF16) for i in range(2)]
            otb = [Buf() for _ in range(2)]
            mg = sb(st, "mg_m", [128, DC, T])
            mgb = Buf()
            mgh = sb(st, "mg_mh", [128, DC, T], BF16)
            mghb = Buf()
            wm = [sb(st, "mg_wm%d" % i, [128, DC, GW * 128], BF16) for i in range(2)]
            wmb = [Buf() for _ in range(2)]
            wbr = [sb(st, "mg_wb%d" % i, [128, 4, GW * 128], BF16) for i in range(2)]
            wbb = [Buf() for _ in range(2)]
            gs = [sb(st, "mg_g%d" % i, [128, HW]) for i in range(2)]
            gsb = [Buf() for _ in range(2)]
            hr = [sb(st, "mg_hr%d" % i, [128, T]) for i in range(2)]
            hrb = [Buf() for _ in range(2)]
            uTv = uT.rearrange("(dc p) s -> p dc s", p=128)
            wi = 0
            pi = 0
            for ti in range(S // T):
                ts = slice(ti * T, (ti + 1) * T)
                kb.dma("sp", ut[:], uTv[:, :, ts], writes=[utb])
                for br in range(4):
                    ob_ = br % 2
                    kb.dma("sp", ot[ob_][:], oT[br].rearrange("(c p) s -> p c s", p=128)[:, :, ts], writes=[otb[ob_]])
                    wmv = w_merge[l, br].rearrange("(kc p) d -> p kc d", p=128)
                    wbv = w_branch[l, br].rearrange("(c p) d -> p c d", p=128)
                    for dg in range(DC // GW):
                        b = wi % 2
                        wi += 1
                        gsl = slice(dg * GW * 128, (dg + 1) * GW * 128)
                        kb.dma("pool", wm[b][:], wmv[:, :, gsl], writes=[wmb[b]])
                        if br == 0:
                            wv2 = w_branch[l, br].rearrange("(g c p) d -> p g c d", g=2, p=64)
                            kb.dma("pool", wbr[b][0:64, :, :], wv2[:, 0, :, gsl], writes=[wbb[b]])
                            kb.dma("pool", wbr[b][64:128, :, :], wv2[:, 1, :, gsl], writes=[wbb[b]])
                        else:
                            kb.dma("pool", wbr[b][:], wbv[:, :, gsl], writes=[wbb[b]])
                        for d in range(GW):
                            dc = dg * GW + d
                            dsl = slice(d * 128, (d + 1) * 128)
                            for hf in range(NH):
                                hs_ = slice(hf * HW, (hf + 1) * HW)
                                bg = 2 * (pi % 3)
                                by = bg + 1
                                gi_ = pi % 2
                                pi += 1
                                for kc in range(DC):
                                    kb.op("pe", lambda e: e.matmul(banks[bg][:, 0:HW], wm[b][:, kc, dsl], ut[:, kc, hs_],
                                                                   start=(kc == 0), stop=(kc == DC - 1)),
                                          reads=[wmb[b], utb], writes=[bbuf[bg]])
                                for c in range(4):
                                    kb.op("pe", lambda e: e.matmul(banks[by][:, 0:HW], wbr[b][:, c, dsl], ot[ob_][:, c, hs_],
                                                                   start=(c == 0), stop=(c == 3)),
                                          reads=[wbb[b], otb[ob_]], writes=[bbuf[by]])
                                kb.op("act", lambda e: e.activation(
                                    out=gs[gi_][:], in_=banks[bg][:, 0:HW], func=AF.Sigmoid, bias=bmS[:, br * DC + dc:br * DC + dc + 1], scale=1.0),
                                    reads=[bbuf[bg], BM], writes=[gsb[gi_]])
                                if br == 0:
                                    kb.op("dve", lambda e: e.tensor_tensor(out=mg[:, dc, hs_], in0=banks[by][:, 0:HW], in1=gs[gi_][:], op=ALU.mult),
                                          reads=[bbuf[by], gsb[gi_]], writes=[mgb])
                                else:
                                    kb.op("dve", lambda e: e.tensor_tensor(out=gs[gi_][:], in0=banks[by][:, 0:HW], in1=gs[gi_][:], op=ALU.mult),
                                          reads=[bbuf[by], gsb[gi_]], writes=[gsb[gi_]])
                                    if br < 3:
                                        kb.op("pool", lambda e: e.tensor_tensor(out=mg[:, dc, hs_], in0=mg[:, dc, hs_], in1=gs[gi_][:], op=ALU.add),
                                              reads=[gsb[gi_], mgb], writes=[mgb])
                                    else:
                                        kb.op("pool", lambda e: e.tensor_tensor(out=mgh[:, dc, hs_], in0=mg[:, dc, hs_], in1=gs[gi_][:], op=ALU.add),
                                              reads=[gsb[gi_], mgb], writes=[mghb])
                wov = w_out[l].rearrange("(kc p) d -> p kc d", p=128)
                for dg in range(DC // GW):
                    b = wi % 2
                    wi += 1
                    gsl = slice(dg * GW * 128, (dg + 1) * GW * 128)
                    kb.dma("pool", wm[b][:], wov[:, :, gsl], writes=[wmb[b]])
                    for d in range(GW):
                        dc = dg * GW + d
                        dsl = slice(d * 128, (d + 1) * 128)
                        hb_i = dc % 2
                        kb.dma("sp", hr[hb_i][:], hT[dc * 128:(dc + 1) * 128, ts], writes=[hrb[hb_i]])
                        for hf in range(NH):
                            hs_ = slice(hf * HW, (hf + 1) * HW)
                            bk = pi % NB
                            pi += 1
                            for kc in range(DC):
                                kb.op("pe", lambda e: e.matmul(banks[bk][:, 0:HW], wm[b][:, kc, dsl], mgh[:, kc, hs_],
                                                               start=(kc == 0), stop=(kc == DC - 1)),
                                      reads=[wmb[b], mghb], writes=[bbuf[bk]])
                            kb.op("dve", lambda e: e.scalar_tensor_tensor(
                                out=hr[hb_i][:, hs_], in0=banks[bk][:, 0:HW], scalar=Gap[:, dc:dc + 1], in1=hr[hb_i][:, hs_],
                                op0=ALU.mult, op1=ALU.add), reads=[bbuf[bk], VB, hrb[hb_i]], writes=[hrb[hb_i]])
                        kb.dma("sp", hT[dc * 128:(dc + 1) * 128, ts], hr[hb_i][:], reads=[hrb[hb_i]])
            kb.barrier()

    K["mask_le_f"] = sb(es, "K_mask_le_f", [128, 128])
    kb.op("dve", lambda e: e.tensor_copy(out=K["mask_le_f"][:], in_=K["mask_le"][:]), reads=[KBUF], writes=[KBUF])
    kb.barrier()

    stages = build_program.stages
    if "tin" in stages:
        stage_transpose_in()
    stage_cond()
    for l in range(L):
        stage_mod(l)
        if "ffn1" in stages:
            stage_norm(Avec[:, 0:DC], modS[:, 0:DC])
            stage_ffn(l, 0, Gvec[:, 0:DC])
        if "mix" in stages:
            stage_norm(Avec[:, DC:2 * DC], modS[:, 3 * DC:4 * DC])
            stage_inproj(l)
            if "swa" in stages:
                stage_attn(l, False)
            if "fox" in stages:
                stage_attn(l, True)
            if "gla" in stages or "ret" in stages:
                pass
            if "gla" in stages:
                stage_linattn(l, False)
            if "ret" in stages:
                stage_linattn(l, True)
            if "merge" in stages:
                stage_merge(l, Gvec[:, DC:2 * DC])
        if "ffn2" in stages:
            stage_norm(Avec[:, 2 * DC:3 * DC], modS[:, 6 * DC:7 * DC])
            stage_ffn(l, 1, Gvec[:, 2 * DC:3 * DC])
    if "fin" in stages:
        stage_norm(finA[:], None, final=True)
    kb.barrier()
    es.close()
    return nc, hc, kb


build_program.stages = {"tin", "ffn1", "mix", "swa", "fox", "gla", "ret", "merge", "ffn2", "fin"}

WNAMES = ["w_ada", "b_ada", "ffn_w_gate", "ffn_w_up", "ffn_w_down", "w_in", "fox_b_forget", "attn_sinks",
          "gla_w_gate", "gla_b_gate", "gla_norm_g", "ret_gn_w", "ret_gn_b", "w_branch", "w_out"]


def make_in_maps(inputs, hc, S, L, nb):
    f = lambda a: np.ascontiguousarray(np.asarray(a, dtype=np.float32))
    shared = {n: f(inputs[n])[:L] for n in WNAMES}
    shared["norm_g"] = f(inputs["norm_g"])[:L].reshape(L, 3 * D)
    shared["w_merge"] = f(inputs["w_merge"])[:L]
    shared["b_merge"] = f(inputs["b_merge"])[:L].reshape(L, 4 * D)
    shared["final_norm_g"] = f(inputs["final_norm_g"]).reshape(1, D)
    for k, v in hc.items():
        shared["k_" + k] = v
    maps = []
    xs = f(inputs["x"])
    cs = f(inputs["c"])
    for core in range(8):
        b = core % nb
        m = dict(shared)
        m["x"] = np.ascontiguousarray(xs[b, :S])
        m["c"] = np.ascontiguousarray(cs[b:b + 1])
        maps.append(m)
    return maps


def kernel(**inputs):
    nc, hc, kb = build_program(SEQ, DEPTH)
    maps = make_in_maps(inputs, hc, SEQ, DEPTH, BATCH)
    res = run_bass_kernel_spmd(nc, maps, core_ids=list(range(8)))
    outs = [np.asarray(res.results[b]["out"], dtype=np.float32) for b in range(BATCH)]
    return np.stack(outs, axis=0)
```

```python
import numpy as np
import ml_dtypes
from contextlib import ExitStack
import concourse.bass as bass
import concourse.mybir as mybir
from concourse.bass_utils import run_bass_kernel_spmd

F32 = mybir.dt.float32
BF16 = mybir.dt.bfloat16
AF = mybir.ActivationFunctionType
ALU = mybir.AluOpType

D = 2048
DC = 16
DFF = 5632
FC = 44
NMOD = 9
EPS = 1e-6
DEPTH = 4
SEQ = 4096
BATCH = 4
INCOLS = 5400
A_Q, A_K, A_V = 0, 512, 640
B_Q, B_K, B_V, B_F = 768, 1280, 1792, 2304
C_Q, C_K, C_V, C_LR, C_R = 2312, 2568, 2824, 3336, 3352
D_Q, D_K, D_V, D_G = 3864, 4120, 4376, 4888


import os
LA_CUT = int(os.environ.get('LA_CUT', '0'))


class Buf:
    __slots__ = ("w", "r")

    def __init__(self):
        self.w = None
        self.r = {}


class KB:
    def __init__(self, nc, es):
        self.nc = nc
        self.eng = {"pe": nc.tensor, "act": nc.scalar, "dve": nc.vector, "pool": nc.gpsimd, "sp": nc.sync}
        self.psem = {e: es.enter_context(nc.semaphore("p_" + e)) for e in self.eng}
        self.cnt = {e: 0 for e in self.eng}
        self.seen = {e: {} for e in self.eng}
        self.semof = {("e", e): self.psem[e] for e in self.eng}
        self.dq = {}
        self.dnext = {}
        for q, n in (("sp", 24), ("pool", 12)):
            self.dq[q] = []
            for i in range(n):
                sem = es.enter_context(nc.semaphore("d_%s%d" % (q, i)))
                self.dq[q].append([sem, 0])
                self.semof[("d", q, i)] = sem
            self.dnext[q] = 0
        self.ninst = 0

    def _deps(self, reads, writes):
        d = {}
        for b in reads:
            if b.w is not None:
                k, v = b.w
                if d.get(k, 0) < v:
                    d[k] = v
        for b in writes:
            if b.w is not None:
                k, v = b.w
                if d.get(k, 0) < v:
                    d[k] = v
            for k, v in b.r.items():
                if d.get(k, 0) < v:
                    d[k] = v
        return d

    def _wait(self, e, d):
        seen = self.seen[e]
        for k, v in d.items():
            if e == "pe" and k == ("e", "pe"):
                continue
            if seen.get(k, 0) < v:
                self.eng[e].wait_ge(self.semof[k], v)
                seen[k] = v
                self.ninst += 1

    def _mark(self, tok, reads, writes):
        k, v = tok
        for b in reads:
            if b.r.get(k, 0) < v:
                b.r[k] = v
        for b in writes:
            b.w = tok
            b.r = {}

    def op(self, e, fn, reads=(), writes=()):
        self._wait(e, self._deps(reads, writes))
        ins = fn(self.eng[e])
        self.cnt[e] += 1
        ins.then_inc(self.psem[e], 1)
        self.ninst += 1
        self._mark((("e", e), self.cnt[e]), reads, writes)

    def dma(self, q, out, in_, reads=(), writes=()):
        slots = self.dq[q]
        i = self.dnext[q]
        self.dnext[q] = (i + 1) % len(slots)
        sl = slots[i]
        d = self._deps(reads, writes)
        key = ("d", q, i)
        if sl[1] > 0 and d.get(key, 0) < sl[1]:
            d[key] = sl[1]
        self._wait(q, d)
        ins = self.eng[q].dma_start(out=out, in_=in_)
        sl[1] += 16
        ins.then_inc(sl[0], 16)
        self.ninst += 1
        self._mark((key, sl[1]), reads, writes)

    def barrier(self):
        tgt = {("e", e): c for e, c in self.cnt.items() if c > 0}
        for q, slots in self.dq.items():
            for i, sl in enumerate(slots):
                if sl[1] > 0:
                    tgt[("d", q, i)] = sl[1]
        for e in self.eng:
            seen = self.seen[e]
            for k, v in tgt.items():
                if seen.get(k, 0) < v:
                    self.eng[e].wait_ge(self.semof[k], v)
                    seen[k] = v
                    self.ninst += 1


def host_consts(S):
    c = {}
    p = np.arange(128)
    c["ident_f"] = np.eye(128, dtype=np.float32)
    c["ident_b"] = np.eye(128, dtype=np.float32).astype(ml_dtypes.bfloat16)
    le = (p[:, None] <= p[None, :]).astype(np.float32)
    c["mask_le"] = le.astype(ml_dtypes.bfloat16)
    c["mask_gt"] = (1.0 - le).astype(ml_dtypes.bfloat16)
    c["ones_b"] = np.ones((128, 128), dtype=ml_dtypes.bfloat16)
    c["ones_f"] = np.ones((128, 128), dtype=np.float32)
    op = np.zeros((128, 192), np.float32)
    op[:, 0:64] = 1.0
    op[:, 128:192] = 1.0
    c["onespad"] = op.astype(ml_dtypes.bfloat16)
    sel = np.zeros((128, 128), np.float32)
    sel[63, :] = 1.0
    c["sel63"] = sel
    q = np.arange(64)
    c["tri64n"] = (-(q[:, None] <= q[None, :]).astype(np.float32) / 16.0)
    c["d64n"] = (-(q[:, None] > q[None, :]).astype(np.float32) / 16.0)
    half = 32
    inv = (10000.0 ** (-np.arange(half, dtype=np.float32) / half)).astype(np.float32)
    pos = np.arange(S, dtype=np.float32)
    ang = (pos[:, None] * inv[None, :]).astype(np.float32)
    cos = np.cos(ang).astype(np.float32)
    sin = np.sin(ang).astype(np.float32)
    fidx = (p % 64) % 32
    sgn = np.where((p % 64) < 32, -1.0, 1.0).astype(np.float32)
    c["cosT"] = np.ascontiguousarray(cos.T[fidx, :])
    c["sinT"] = np.ascontiguousarray(sin.T[fidx, :] * sgn[:, None])
    c["cos_tok"] = np.ascontiguousarray(cos.reshape(S // 64, 64, 32).transpose(1, 0, 2))
    c["sin_tok"] = np.ascontiguousarray(sin.reshape(S // 64, 64, 32).transpose(1, 0, 2))
    lg = np.log1p(-np.exp2(-5.0 - np.arange(4, dtype=np.float64)))
    j = np.arange(64, dtype=np.float64)
    dq = np.zeros((64, 4, 64), np.float64)
    dk = np.zeros((64, 4, 64), np.float64)
    dec = np.zeros((64, 4), np.float64)
    for h in range(4):
        dq[:, h, :] = 0.125 * np.exp((j + 1) * lg[h])[None, :]
        dk[:, h, :] = np.exp(-(j + 1) * lg[h])[None, :]
        dec[:, h] = np.exp(64 * lg[h])
    c["ret_dq"] = dq.astype(np.float32)
    c["ret_dk"] = dk.astype(np.float32)
    c["ret_dec"] = dec.astype(np.float32)
    ks = np.zeros((64, 4), np.float64)
    for h in range(4):
        ks[:, h] = np.exp((63 - j) * lg[h])
    c["ret_ks"] = ks.astype(np.float32)
    return c


CONST_DT = {"ident_b": BF16, "mask_le": BF16, "mask_gt": BF16, "ones_b": BF16, "onespad": BF16}


def build_program(S, L, debug=()):
    NT = S // 128
    NCH = S // 64
    nc = bass.Bass("TRN2", target_bir_lowering=False)
    es = ExitStack()
    kb = KB(nc, es)
    dbg = set(debug)

    def din(name, shape, dt=F32):
        return nc.dram_tensor(name, list(shape), dt, kind="ExternalInput").ap()

    def dscr(name, shape, dt=F32):
        kind = "ExternalOutput" if name in dbg else "Internal"
        return nc.dram_tensor(name, list(shape), dt, kind=kind).ap()

    x = din("x", [S, D])
    cvec = din("c", [1, D])
    w_ada = din("w_ada", [L, D, NMOD * D])
    b_ada = din("b_ada", [L, NMOD * D])
    norm_g = din("norm_g", [L, 3 * D])
    w_gate = din("ffn_w_gate", [L, 2, D, DFF])
    w_up = din("ffn_w_up", [L, 2, D, DFF])
    w_down = din("ffn_w_down", [L, 2, DFF, D])
    w_in = din("w_in", [L, D, INCOLS])
    fox_b = din("fox_b_forget", [L, 8])
    sinks = din("attn_sinks", [L, 8])
    gla_wg = din("gla_w_gate", [L, 16, 256])
    gla_bg = din("gla_b_gate", [L, 256])
    gla_ng = din("gla_norm_g", [L, 128])
    ret_w = din("ret_gn_w", [L, 512])
    ret_b = din("ret_gn_b", [L, 512])
    w_branch = din("w_branch", [L, 4, 512, D])
    w_merge = din("w_merge", [L, 4, D, D])
    b_merge = din("b_merge", [L, 4 * D])
    w_out = din("w_out", [L, D, D])
    fin_g = din("final_norm_g", [1, D])
    hc = host_consts(S)
    cin = {k: din("k_" + k, v.shape, CONST_DT.get(k, F32)) for k, v in hc.items()}
    out = nc.dram_tensor("out", [S, D], F32, kind="ExternalOutput").ap()

    hT = dscr("hT", [D, S])
    uT = dscr("uT", [D, S], BF16)
    qTa = dscr("qTa", [512, S], BF16)
    kTa = dscr("kTa", [128, S], BF16)
    va = dscr("va", [S, 128], BF16)
    qTb = dscr("qTb", [512, S], BF16)
    kTb = dscr("kTb", [512, S], BF16)
    vb = dscr("vb", [S, 512], BF16)
    fb = dscr("fb", [S, 8])
    qTc = dscr("qTc", [256, S])
    kTc = dscr("kTc", [256, S])
    kc_tok = dscr("kc_tok", [S, 256])
    vc = dscr("vc", [S, 512], BF16)
    lrT = dscr("lrT", [16, S])
    rc = dscr("rc", [S, 512])
    qTd = dscr("qTd", [256, S])
    qrTd = dscr("qrTd", [256, S])
    kTd = dscr("kTd", [256, S])
    krTd = dscr("krTd", [256, S])
    kd_tok = dscr("kd_tok", [S, 256])
    vd = dscr("vd", [S, 512], BF16)
    gd = dscr("gd", [S, 512])
    oT = [dscr("oT%d" % i, [512, S], BF16) for i in range(4)]

    uid = [0]

    def sb(stack, name, shape, dt=F32):
        uid[0] += 1
        return stack.enter_context(nc.sbuf_tensor("%s_%d" % (name, uid[0]), list(shape), dt))

    K = {}
    KBUF = Buf()
    for k, v in hc.items():
        if k in ("cosT", "sinT", "cos_tok", "sin_tok"):
            continue
        K[k] = sb(es, "K_" + k, v.shape, CONST_DT.get(k, F32))
        kb.dma("sp", K[k][:], cin[k], writes=[KBUF])
    condT = sb(es, "condT", [128, DC], BF16)
    modS = sb(es, "modS", [128, NMOD * DC])
    ngS = sb(es, "ngS", [128, 3 * DC])
    Avec = sb(es, "Avec", [128, 3 * DC])
    Gvec = sb(es, "Gvec", [128, 3 * DC])
    finA = sb(es, "finA", [128, DC])
    zeroB = sb(es, "zeroB", [128, 1])
    VB = Buf()
    kb.op("dve", lambda e: e.memset(zeroB[:], 0.0), writes=[VB])
    epsD = sb(es, "epsD", [128, 2])
    kb.op("dve", lambda e: e.memset(epsD[:, 0:1], float(D * EPS)), writes=[VB])
    kb.op("dve", lambda e: e.memset(epsD[:, 1:2], float(EPS)), writes=[VB])

    NB = 7
    banks = [es.enter_context(nc.psum_tensor("bank%d" % i, [128, 512], F32)) for i in range(NB)]
    bbuf = [Buf() for _ in range(NB)]
    trps = es.enter_context(nc.psum_tensor("trps", [128, 4, 64], BF16))
    trb = Buf()

    SQD = float(np.sqrt(D))

    def stage_transpose_in():
        with ExitStack() as st:
            xs = [sb(st, "ti_x%d" % i, [128, D]) for i in range(2)]
            xb = [Buf() for _ in range(2)]
            ys = [sb(st, "ti_y%d" % i, [128, DC, 128]) for i in range(2)]
            yb = [Buf() for _ in range(2)]
            hTv = hT.rearrange("(dc p) s -> p dc s", p=128)
            for nt in range(NT):
                b = nt % 2
                kb.dma("sp", xs[b][:], x[nt * 128:(nt + 1) * 128, :], writes=[xb[b]])
                for g in range(4):
                    bk = (nt * 4 + g) % NB
                    for i in range(4):
                        dc = g * 4 + i
                        kb.op("pe", lambda e, dc=dc, i=i, bk=bk, b=b: e.transpose(
                            banks[bk][:, i * 128:(i + 1) * 128], xs[b][:, dc * 128:(dc + 1) * 128], K["ident_f"][:]),
                            reads=[xb[b], KBUF], writes=[bbuf[bk]])
                    eng = "act" if g % 2 == 0 else "dve"
                    if eng == "act":
                        kb.op("act", lambda e, g=g, bk=bk, b=b: e.activation(
                            out=ys[b][:, g * 4:(g + 1) * 4, :], in_=banks[bk][:].rearrange("p (a t) -> p a t", t=128),
                            func=AF.Copy), reads=[bbuf[bk]], writes=[yb[b]])
                    else:
                        kb.op("dve", lambda e, g=g, bk=bk, b=b: e.tensor_copy(
                            out=ys[b][:, g * 4:(g + 1) * 4, :], in_=banks[bk][:].rearrange("p (a t) -> p a t", t=128)),
                            reads=[bbuf[bk]], writes=[yb[b]])
                kb.dma("sp", hTv[:, :, nt * 128:(nt + 1) * 128], ys[b][:], reads=[yb[b]])
            kb.barrier()

    def row_to_cols(ps_ap_fn, row_sb, ncols_chunks, bk, rbuf, first=True):
        for j in range(ncols_chunks):
            kb.op("pe", lambda e, j=j: e.matmul(ps_ap_fn(j), row_sb[0:1, j * 128:(j + 1) * 128], K["ones_f"][0:1, 0:1],
                                                start=(first and j == 0), stop=True, skip_group_check=True),
                  reads=[rbuf, KBUF], writes=[bbuf[bk]])

    def stage_cond():
        with ExitStack() as st:
            crow = sb(st, "crow", [1, D])
            frow = sb(st, "frow", [1, D])
            rb = Buf()
            kb.dma("sp", crow[:], cvec[:, :], writes=[rb])
            kb.dma("sp", frow[:], fin_g[:, :], writes=[rb])
            row_to_cols(lambda j: banks[0][:, j:j + 1], crow, DC, 0, rb)
            kb.op("act", lambda e: e.activation(out=condT[:], in_=banks[0][:, 0:DC], func=AF.Silu),
                  reads=[bbuf[0]], writes=[VB])
            row_to_cols(lambda j: banks[1][:, j:j + 1], frow, DC, 1, rb)
            kb.op("dve", lambda e: e.tensor_scalar(out=finA[:], in0=banks[1][:, 0:DC], scalar1=SQD, scalar2=None,
                                                   op0=ALU.mult), reads=[bbuf[1]], writes=[VB])
            kb.barrier()

    def stage_mod(l):
        with ExitStack() as st:
            NCc = NMOD * DC
            wa = [sb(st, "wa%d" % i, [128, NMOD * D], BF16) for i in range(2)]
            wab = [Buf() for _ in range(2)]
            brow = sb(st, "brow", [1, NMOD * D])
            grow = sb(st, "grow", [1, 3 * D])
            rb = Buf()
            kb.dma("sp", brow[:], b_ada[l:l + 1, :], writes=[rb])
            kb.dma("sp", grow[:], norm_g[l:l + 1, :], writes=[rb])
            for kc in range(DC):
                b = kc % 2
                kb.dma("pool", wa[b][:], w_ada[l, kc * 128:(kc + 1) * 128, :], writes=[wab[b]])
                for j in range(NCc):
                    kb.op("pe", lambda e, j=j, b=b, kc=kc: e.matmul(
                        banks[0][:, j:j + 1], wa[b][:, j * 128:(j + 1) * 128], condT[:, kc:kc + 1],
                        start=(kc == 0 and j == 0), stop=False, skip_group_check=True),
                        reads=[wab[b], VB], writes=[bbuf[0]])
            row_to_cols(lambda j: banks[0][:, j:j + 1], brow, NCc, 0, rb, first=False)
            row_to_cols(lambda j: banks[1][:, j:j + 1], grow, 3 * DC, 1, rb)
            kb.op("act", lambda e: e.activation(out=modS[:], in_=banks[0][:, 0:NCc], func=AF.Copy),
                  reads=[bbuf[0]], writes=[VB])
            kb.op("act", lambda e: e.activation(out=ngS[:], in_=banks[1][:, 0:3 * DC], func=AF.Copy),
                  reads=[bbuf[1]], writes=[VB])
            for k in range(3):
                sc = modS[:, (3 * k + 1) * DC:(3 * k + 2) * DC]
                g = modS[:, (3 * k + 2) * DC:(3 * k + 3) * DC]
                kb.op("dve", lambda e, k=k, sc=sc: e.scalar_tensor_tensor(
                    out=Avec[:, k * DC:(k + 1) * DC], in0=sc, scalar=1.0, in1=ngS[:, k * DC:(k + 1) * DC],
                    op0=ALU.add, op1=ALU.mult), reads=[VB], writes=[VB])
                kb.op("dve", lambda e, k=k: e.tensor_scalar(
                    out=Avec[:, k * DC:(k + 1) * DC], in0=Avec[:, k * DC:(k + 1) * DC], scalar1=SQD, scalar2=None,
                    op0=ALU.mult), reads=[VB], writes=[VB])
                kb.op("dve", lambda e, k=k, g=g: e.tensor_scalar(
                    out=Gvec[:, k * DC:(k + 1) * DC], in0=g, scalar1=(1.0 if k == 1 else 0.5), scalar2=None,
                    op0=ALU.mult), reads=[VB], writes=[VB])
            kb.barrier()

    def stage_norm(Aap, Bap, final=False):
        T = min(512, S)
        with ExitStack() as st:
            hs = [sb(st, "nm_h%d" % i, [128, DC, T]) for i in range(2)]
            hb = [Buf() for _ in range(2)]
            sq = sb(st, "nm_sq", [128, DC, T], BF16)
            sqb = Buf()
            rstd = sb(st, "nm_rstd", [128, T])
            rb = Buf()
            tmp = [sb(st, "nm_tmp%d" % i, [128, T]) for i in range(2)]
            tb = [Buf() for _ in range(2)]
            if not final:
                ub = [sb(st, "nm_u%d" % i, [128, DC, T], BF16) for i in range(2)]
            else:
                ub = [sb(st, "nm_u%d" % i, [128, DC, T]) for i in range(1)]
                ot = [sb(st, "nm_o%d" % i, [128, D]) for i in range(2)]
                otb = [Buf() for _ in range(2)]
            ubb = [Buf() for _ in range(2)]
            hTv = hT.rearrange("(dc p) s -> p dc s", p=128)
            uTv = uT.rearrange("(dc p) s -> p dc s", p=128)
            for ti in range(S // T):
                b = ti % 2
                ts = slice(ti * T, (ti + 1) * T)
                kb.dma("sp", hs[b][:], hTv[:, :, ts], writes=[hb[b]])
                for dc in range(DC):
                    eng = "act" if dc % 2 == 0 else "pool"
                    if eng == "act":
                        kb.op("act", lambda e, dc=dc, b=b: e.activation(out=sq[:, dc, :], in_=hs[b][:, dc, :], func=AF.Square),
                              reads=[hb[b]], writes=[sqb])
                    else:
                        kb.op("pool", lambda e, dc=dc, b=b: e.tensor_tensor(out=sq[:, dc, :], in0=hs[b][:, dc, :],
                                                                            in1=hs[b][:, dc, :], op=ALU.mult),
                              reads=[hb[b]], writes=[sqb])
                bk = ti % 2
                for dc in range(DC):
                    kb.op("pe", lambda e, dc=dc, bk=bk: e.matmul(banks[bk][:, 0:T], K["ones_b"][:], sq[:, dc, :],
                                                                 start=(dc == 0), stop=(dc == DC - 1)),
                          reads=[sqb, KBUF], writes=[bbuf[bk]])
                kb.op("act", lambda e, bk=bk: e.activation(out=rstd[:], in_=banks[bk][:, 0:T], func=AF.Ln, bias=epsD[:, 0:1], scale=1.0),
                      reads=[bbuf[bk], VB], writes=[rb])
                kb.op("act", lambda e: e.activation(out=rstd[:], in_=rstd[:], func=AF.Exp, scale=-0.5), reads=[rb], writes=[rb])
                ui = b if not final else 0
                u = ub[ui]
                for dc in range(DC):
                    tb_i = dc % 2
                    if Bap is not None:
                        kb.op("dve", lambda e, dc=dc, b=b, tb_i=tb_i: e.scalar_tensor_tensor(
                            out=tmp[tb_i][:], in0=hs[b][:, dc, :], scalar=Aap[:, dc:dc + 1], in1=rstd[:],
                            op0=ALU.mult, op1=ALU.mult), reads=[hb[b], rb, VB], writes=[tb[tb_i]])
                        kb.op("act", lambda e, dc=dc, tb_i=tb_i, u=u: e.activation(
                            out=u[:, dc, :], in_=tmp[tb_i][:], func=AF.Identity, bias=Bap[:, dc:dc + 1], scale=1.0),
                            reads=[tb[tb_i], VB], writes=[ubb[ui]])
                    else:
                        kb.op("dve", lambda e, dc=dc, b=b, u=u: e.scalar_tensor_tensor(
                            out=u[:, dc, :], in0=hs[b][:, dc, :], scalar=Aap[:, dc:dc + 1], in1=rstd[:],
                            op0=ALU.mult, op1=ALU.mult), reads=[hb[b], rb, VB], writes=[ubb[ui]])
                if not final:
                    kb.dma("sp", uTv[:, :, ts], u[:], reads=[ubb[ui]])
                else:
                    for sub in range(T // 128):
                        ob = (ti * (T // 128) + sub) % 2
                        for g in range(4):
                            bk2 = 2 + (sub * 4 + g) % 5
                            for i in range(4):
                                dc = g * 4 + i
                                kb.op("pe", lambda e, dc=dc, i=i, bk2=bk2, sub=sub, u=u: e.transpose(
                                    banks[bk2][:, i * 128:(i + 1) * 128], u[:, dc, sub * 128:(sub + 1) * 128],
                                    K["ident_f"][:]), reads=[ubb[ui], KBUF], writes=[bbuf[bk2]])
                            if g % 2 == 0:
                                kb.op("act", lambda e, g=g, bk2=bk2, ob=ob: e.activation(
                                    out=ot[ob][:, g * 512:(g + 1) * 512], in_=banks[bk2][:], func=AF.Copy),
                                    reads=[bbuf[bk2]], writes=[otb[ob]])
                            else:
                                kb.op("dve", lambda e, g=g, bk2=bk2, ob=ob: e.tensor_copy(
                                    out=ot[ob][:, g * 512:(g + 1) * 512], in_=banks[bk2][:]),
                                    reads=[bbuf[bk2]], writes=[otb[ob]])
                        r0 = ti * T + sub * 128
                        kb.dma("sp", out[r0:r0 + 128, :], ot[ob][:], reads=[otb[ob]])
            kb.barrier()

    def stage_ffn(l, which, Gap):
        T = min(1024, S)
        NH = T // 512 if T >= 512 else 1
        HW = min(512, T)
        with ExitStack() as st:
            ut = sb(st, "ff_u", [128, DC, T], BF16)
            utb = Buf()
            act = sb(st, "ff_act", [128, FC, T], BF16)
            actb = Buf()
            wbuf = [sb(st, "ff_w%d" % i, [128, 11264], BF16) for i in range(2)]
            wb = [Buf() for _ in range(2)]
            sg = [sb(st, "ff_sg%d" % i, [128, HW]) for i in range(2)]
            sgb = [Buf() for _ in range(2)]
            hr = [sb(st, "ff_hr%d" % i, [128, T]) for i in range(2)]
            hrb = [Buf() for _ in range(2)]
            uTv = uT.rearrange("(dc p) s -> p dc s", p=128)
            wgv = w_gate[l, which].rearrange("(kc p) f -> p kc f", p=128)
            wuv = w_up[l, which].rearrange("(kc p) f -> p kc f", p=128)
            wdv = w_down[l, which].rearrange("(j p) d -> p j d", p=128)
            wi = 0
            pi = 0
            for ti in range(S // T):
                ts = slice(ti * T, (ti + 1) * T)
                kb.dma("sp", ut[:], uTv[:, :, ts], writes=[utb])
                for jj in range(FC // 2):
                    b = wi % 2
                    wi += 1
                    wg = wbuf[b][:, 0:4096].rearrange("p (k f) -> p k f", f=256)
                    wu = wbuf[b][:, 4096:8192].rearrange("p (k f) -> p k f", f=256)
                    kb.dma("pool", wg, wgv[:, :, jj * 256:(jj + 1) * 256], writes=[wb[b]])
                    kb.dma("pool", wu, wuv[:, :, jj * 256:(jj + 1) * 256], writes=[wb[b]])
                    for j2 in range(2):
                        j = jj * 2 + j2
                        for hf in range(NH):
                            bg = 2 * (pi % 3)
                            bu = bg + 1
                            si = pi % 2
                            pi += 1
                            hs_ = slice(hf * HW, (hf + 1) * HW)
                            for kc in range(DC):
                                kb.op("pe", lambda e, kc=kc, bg=bg, wg=wg, j2=j2, hs_=hs_: e.matmul(
                                    banks[bg][:, 0:HW], wg[:, kc, j2 * 128:(j2 + 1) * 128], ut[:, kc, hs_],
                                    start=(kc == 0), stop=(kc == DC - 1)), reads=[wb[b], utb], writes=[bbuf[bg]])
                            for kc in range(DC):
                                kb.op("pe", lambda e, kc=kc, bu=bu, wu=wu, j2=j2, hs_=hs_: e.matmul(
                                    banks[bu][:, 0:HW], wu[:, kc, j2 * 128:(j2 + 1) * 128], ut[:, kc, hs_],
                                    start=(kc == 0), stop=(kc == DC - 1)), reads=[wb[b], utb], writes=[bbuf[bu]])
                            kb.op("act", lambda e, bg=bg, si=si: e.activation(out=sg[si][:], in_=banks[bg][:, 0:HW], func=AF.Silu),
                                  reads=[bbuf[bg]], writes=[sgb[si]])
                            kb.op("dve", lambda e, bu=bu, si=si, j=j, hs_=hs_: e.tensor_tensor(
                                out=act[:, j, hs_], in0=banks[bu][:, 0:HW], in1=sg[si][:], op=ALU.mult),
                                reads=[bbuf[bu], sgb[si]], writes=[actb])
                for g in range(DC // 2):
                    b = wi % 2
                    wi += 1
                    wd = wbuf[b][:, 0:11264].rearrange("p (j d) -> p j d", d=256)
                    kb.dma("pool", wd, wdv[:, :, g * 256:(g + 1) * 256], writes=[wb[b]])
                    for d2 in range(2):
                        dc = g * 2 + d2
                        hb_i = dc % 2
                        kb.dma("sp", hr[hb_i][:], hT[dc * 128:(dc + 1) * 128, ts], writes=[hrb[hb_i]])
                        for hf in range(NH):
                            bk = pi % NB
                            pi += 1
                            hs_ = slice(hf * HW, (hf + 1) * HW)
                            for j in range(FC):
                                kb.op("pe", lambda e, j=j, bk=bk, wd=wd, d2=d2, hs_=hs_: e.matmul(
                                    banks[bk][:, 0:HW], wd[:, j, d2 * 128:(d2 + 1) * 128], act[:, j, hs_],
                                    start=(j == 0), stop=(j == FC - 1)), reads=[wb[b], actb], writes=[bbuf[bk]])
                            kb.op("dve", lambda e, bk=bk, dc=dc, hb_i=hb_i, hs_=hs_: e.scalar_tensor_tensor(
                                out=hr[hb_i][:, hs_], in0=banks[bk][:, 0:HW], scalar=Gap[:, dc:dc + 1], in1=hr[hb_i][:, hs_],
                                op0=ALU.mult, op1=ALU.add), reads=[bbuf[bk], VB, hrb[hb_i]], writes=[hrb[hb_i]])
                        kb.dma("sp", hT[dc * 128:(dc + 1) * 128, ts], hr[hb_i][:], reads=[hrb[hb_i]])
            kb.barrier()

    def stage_inproj(l):
        T = min(1024, S)
        HW = min(512, T)
        NH = T // HW
        W = w_in[l].rearrange("(kc p) c -> p kc c", p=128)
        groups = []
        for c in range(4):
            groups.append(([(A_Q + c * 64, 64), (A_Q + (4 + c) * 64, 64)], [(0, qTa[c * 128:(c + 1) * 128, :], 128, BF16)]))
        groups.append(([(A_K, 128)], [(0, kTa[:, :], 128, BF16)]))
        groups.append(([(B_Q, 512)], [(c * 128, qTb[c * 128:(c + 1) * 128, :], 128, BF16) for c in range(4)]))
        groups.append(([(B_K, 512)], [(c * 128, kTb[c * 128:(c + 1) * 128, :], 128, BF16) for c in range(4)]))
        groups.append(([(C_Q, 512)], [(c * 128, qTc[c * 128:(c + 1) * 128, :], 128, F32) for c in range(2)] +
                       [(256 + c * 128, kTc[c * 128:(c + 1) * 128, :], 128, F32) for c in range(2)]))
        groups.append(([(D_Q, 512)], [(c * 128, qTd[c * 128:(c + 1) * 128, :], 128, F32) for c in range(2)] +
                       [(256 + c * 128, kTd[c * 128:(c + 1) * 128, :], 128, F32) for c in range(2)]))
        for (base, dst) in ((D_Q, qrTd), (D_K, krTd)):
            segs = []
            for h in range(4):
                h0 = base + h * 64
                segs += [(h0 + 32, 32), (h0, 32)]
            groups.append((segs, [(c * 128, dst[c * 128:(c + 1) * 128, :], 128, F32) for c in range(2)]))
        groups.append(([(C_LR, 16)], [(0, lrT[:, :], 16, F32)]))
        tm = [(A_V, 128, va, BF16, False), (B_V, 512, vb, BF16, False), (B_F, 8, fb, F32, False),
              (C_K, 256, kc_tok, F32, False), (C_V, 512, vc, BF16, False), (C_R, 512, rc, F32, True),
              (D_K, 256, kd_tok, F32, False), (D_V, 512, vd, BF16, False), (D_G, 512, gd, F32, True)]
        with ExitStack() as st:
            ut = [sb(st, "ip_u%d" % i, [128, DC, T], BF16) for i in range(2)]
            utb = [Buf() for _ in range(2)]
            wl = [sb(st, "ip_wl%d" % i, [128, DC, 512], BF16) for i in range(2)]
            wlb = [Buf() for _ in range(2)]
            wr = [sb(st, "ip_wr%d" % i, [128, DC, 512], BF16) for i in range(2)]
            wrb = [Buf() for _ in range(2)]
            of = [sb(st, "ip_of%d" % i, [128, T]) for i in range(3)]
            ofb = [Buf() for _ in range(3)]
            obf = [sb(st, "ip_ob%d" % i, [128, T], BF16) for i in range(3)]
            obb = [Buf() for _ in range(3)]
            tf = [sb(st, "ip_tf%d" % i, [128, 512]) for i in range(3)]
            tfb = [Buf() for _ in range(3)]
            tbf = [sb(st, "ip_tb%d" % i, [128, 512], BF16) for i in range(3)]
            tbb = [Buf() for _ in range(3)]
            uTv = uT.rearrange("(dc p) s -> p dc s", p=128)
            wi = 0
            oi = 0
            pi = 0
            for ti in range(S // T):
                ub_ = ti % 2
                ts = slice(ti * T, (ti + 1) * T)
                kb.dma("sp", ut[ub_][:], uTv[:, :, ts], writes=[utb[ub_]])
                for (segs, jobs) in groups:
                    b = wi % 2
                    wi += 1
                    c0 = 0
                    for (col, n) in segs:
                        kb.dma("pool", wl[b][:, :, c0:c0 + n], W[:, :, col:col + n], writes=[wlb[b]])
                        c0 += n
                    for (off, dst, M, dt) in jobs:
                        o = oi % 3
                        oi += 1
                        if dt == BF16:
                            dstt, dstb = obf[o], obb[o]
                        else:
                            dstt, dstb = of[o], ofb[o]
                        for hf in range(NH):
                            hs_ = slice(hf * HW, (hf + 1) * HW)
                            bk = pi % NB
                            pi += 1
                            for kc in range(DC):
                                kb.op("pe", lambda e: e.matmul(banks[bk][0:M, 0:HW], wl[b][:, kc, off:off + M], ut[ub_][:, kc, hs_],
                                                               start=(kc == 0), stop=(kc == DC - 1)),
                                      reads=[wlb[b], utb[ub_]], writes=[bbuf[bk]])
                            if pi % 2 == 0:
                                kb.op("act", lambda e: e.activation(out=dstt[0:M, hs_], in_=banks[bk][0:M, 0:HW], func=AF.Copy),
                                      reads=[bbuf[bk]], writes=[dstb])
                            else:
                                kb.op("dve", lambda e: e.tensor_copy(out=dstt[0:M, hs_], in_=banks[bk][0:M, 0:HW]),
                                      reads=[bbuf[bk]], writes=[dstb])
                        kb.dma("sp", dst[:, ts], dstt[0:M, :], reads=[dstb])
                for (col, n, dst, dt, silu) in tm:
                    b = wi % 2
                    wi += 1
                    kb.dma("pool", wr[b][:, :, 0:n], W[:, :, col:col + n], writes=[wrb[b]])
                    for sub in range(T // 128):
                        bk = pi % NB
                        pi += 1
                        for kc in range(DC):
                            kb.op("pe", lambda e: e.matmul(banks[bk][:, 0:n], ut[ub_][:, kc, sub * 128:(sub + 1) * 128], wr[b][:, kc, 0:n],
                                                           start=(kc == 0), stop=(kc == DC - 1)), reads=[wrb[b], utb[ub_]], writes=[bbuf[bk]])
                        o = oi % 3
                        oi += 1
                        if dt == BF16:
                            dstt, dstb = tbf[o], tbb[o]
                        else:
                            dstt, dstb = tf[o], tfb[o]
                        if silu:
                            kb.op("act", lambda e: e.activation(out=dstt[:, 0:n], in_=banks[bk][:, 0:n], func=AF.Silu),
                                  reads=[bbuf[bk]], writes=[dstb])
                        elif oi % 2 == 0:
                            kb.op("act", lambda e: e.activation(out=dstt[:, 0:n], in_=banks[bk][:, 0:n], func=AF.Copy),
                                  reads=[bbuf[bk]], writes=[dstb])
                        else:
                            kb.op("dve", lambda e: e.tensor_copy(out=dstt[:, 0:n], in_=banks[bk][:, 0:n]),
                                  reads=[bbuf[bk]], writes=[dstb])
                        r0 = ti * T + sub * 128
                        kb.dma("sp", dst[r0:r0 + 128, :], dstt[:, 0:n], reads=[dstb])
            kb.barrier()

    def stage_attn(l, fox):
        npair = 4
        nkc = 4 if fox else 1
        with ExitStack() as st:
            QT = sb(st, "at_q", [128, 4, S], BF16)
            KT = sb(st, "at_k", [128, nkc, S], BF16)
            VP = sb(st, "at_v", [128, NT, nkc, 192], BF16)
            OT = sb(st, "at_o", [128, 4, S], BF16)
            LB = Buf()
            OB = Buf()
            NPT = 4
            PT = [sb(st, "at_p%d" % i, [128, 512], BF16) for i in range(NPT)]
            PB = [Buf() for _ in range(NPT)]
            rd = [sb(st, "at_rd%d" % i, [128, 512]) for i in range(2)]
            rdb = [Buf() for _ in range(2)]
            kb.op("pool", lambda e: e.memset(VP[:], 0.0), writes=[LB])
            qsrc = qTb if fox else qTa
            ksrc = kTb if fox else kTa
            vsrc = vb if fox else va
            kb.dma("sp", QT[:], qsrc.rearrange("(c p) s -> p c s", p=128), writes=[LB])
            kb.dma("sp", KT[:], ksrc.rearrange("(c p) s -> p c s", p=128), writes=[LB])
            vv = vsrc.rearrange("(n p) (c h d) -> p n c h d", p=128, h=2, d=64)
            for c in range(nkc):
                kb.dma("sp", VP[:, :, c, 0:64], vv[:, :, c, 0, :], writes=[LB])
                kb.dma("sp", VP[:, :, c, 128:192], vv[:, :, c, 1, :], writes=[LB])
            if fox:
                fz = sb(st, "at_fz", [128, NT, 8])
                fl = sb(st, "at_fl", [128, NT, 8])
                fP = sb(st, "at_fP", [128, NT, 8])
                Lc = sb(st, "at_Lc", [128, NT, 8])
                Lr = sb(st, "at_Lr", [128, NT, 8])
                bfo = sb(st, "at_bf", [128, 8])
                bias = sb(st, "at_bias", [128, 8, NT, NT])
                FB = Buf()
                kb.dma("sp", fz[:], fb.rearrange("(n p) h -> p n h", p=128), writes=[FB])
                kb.dma("sp", bfo[:], fox_b[l, :].partition_broadcast(128), writes=[FB])
                kb.op("dve", lambda e: e.tensor_tensor(out=fz[:], in0=fz[:], in1=bfo[:].unsqueeze(1).broadcast_to([128, NT, 8]),
                                                       op=ALU.add), reads=[FB], writes=[FB])
                kb.op("act", lambda e: e.activation(out=fl[:], in_=fz[:], func=AF.Exp, scale=-1.0), reads=[FB], writes=[FB])
                kb.op("act", lambda e: e.activation(out=fl[:], in_=fl[:], func=AF.Ln, bias=1.0, scale=1.0), reads=[FB], writes=[FB])
                kb.op("dve", lambda e: e.memset(fP[:, 0, :], 0.0), reads=[FB], writes=[FB])
                for n in range(1, NT):
                    kb.op("dve", lambda e, n=n: e.tensor_tensor(out=fP[:, n, :], in0=fP[:, n - 1, :], in1=fl[:, n - 1, :], op=ALU.add),
                          reads=[FB], writes=[FB])
                for n in range(NT):
                    kb.op("pe", lambda e, n=n: e.matmul(banks[0][:, n * 8:(n + 1) * 8], K["mask_le_f"][:], fl[:, n, :],
                                                        start=(n == 0), stop=False, skip_group_check=True),
                          reads=[FB, KBUF], writes=[bbuf[0]])
                    kb.op("pe", lambda e, n=n: e.matmul(banks[0][:, n * 8:(n + 1) * 8], K["ones_f"][:], fP[:, n, :],
                                                        start=False, stop=True, skip_group_check=True),
                          reads=[FB, KBUF], writes=[bbuf[0]])
                kb.op("act", lambda e: e.activation(out=Lc[:].rearrange("p n h -> p (n h)"), in_=banks[0][:, 0:NT * 8], func=AF.Copy),
                      reads=[bbuf[0]], writes=[FB])
                for n0 in range(0, NT * 8, 512):
                    n1 = min(NT * 8, n0 + 512)
                    kb.op("pe", lambda e, n0=n0, n1=n1: e.matmul(banks[1][:, 0:n1 - n0], K["sel63"][:],
                                                                 Lc[:].rearrange("p n h -> p (n h)")[:, n0:n1], start=True, stop=True),
                          reads=[FB, KBUF], writes=[bbuf[1]])
                    kb.op("act", lambda e, n0=n0, n1=n1: e.activation(out=Lr[:].rearrange("p n h -> p (n h)")[:, n0:n1],
                                                                      in_=banks[1][:, 0:n1 - n0], func=AF.Copy),
                          reads=[bbuf[1]], writes=[FB])
                for h in range(8):
                    kb.op("dve", lambda e, h=h: e.tensor_tensor(
                        out=bias[:, h, :, :], in0=Lc[:, :, h].unsqueeze(1).broadcast_to([128, NT, NT]),
                        in1=Lr[:, :, h].unsqueeze(2).broadcast_to([128, NT, NT]), op=ALU.subtract),
                        reads=[FB], writes=[FB])
            else:
                es_ = sb(st, "at_es", [128, 8])
                esp = sb(st, "at_esp", [128, 4])
                FB = Buf()
                kb.dma("sp", es_[:], sinks[l, :].partition_broadcast(128), writes=[FB])
                kb.op("act", lambda e: e.activation(out=es_[:], in_=es_[:], func=AF.Exp), reads=[FB], writes=[FB])
                kb.op("dve", lambda e: e.tensor_copy(out=esp[0:64, :], in_=es_[0:64, 0:4]), reads=[FB], writes=[FB])
                kb.op("dve", lambda e: e.tensor_copy(out=esp[64:128, :], in_=es_[64:128, 4:8]), reads=[FB], writes=[FB])

            G = min(4, NT)
            GW_ = G * 128
            gi = 0
            si = 0
            for c in range(npair):
                kc_ = c if fox else 0
                for g in range(NT // G):
                    i0 = g * G
                    i1 = i0 + G - 1
                    bo = (gi % 2) * 2
                    bd = bo + 1
                    r = gi % 2
                    gi += 1
                    steps = []
                    for hf in range(2):
                        js = range(0, i1 + 1) if fox else range(max(i0 - 1, 0), i1 + 1)
                        for j in js:
                            ilo = max(j, i0)
                            ihi = i1 if fox else min(j + 1, i1)
                            if ilo <= ihi:
                                steps.append((hf, j, ilo, ihi))

                    def emit_score(k):
                        hf, j, ilo, ihi = steps[k]
                        Wd = (ihi - ilo + 1) * 128
                        bs = 4 + (si + k) % 3
                        rs = slice(hf * 64, (hf + 1) * 64)
                        kb.op("pe", lambda e: e.matmul(banks[bs][:, 0:Wd], KT[rs, kc_, j * 128:(j + 1) * 128],
                                                       QT[rs, c, ilo * 128:(ihi + 1) * 128], start=True, stop=True),
                              reads=[LB], writes=[bbuf[bs]])
                        pb = (si + k) % NPT
                        if fox:
                            h = 2 * c + hf
                            for i in range(ilo, ihi + 1):
                                off = (i - ilo) * 128
                                kb.op("act", lambda e: e.activation(out=PT[pb][:, off:off + 128], in_=banks[bs][:, off:off + 128], func=AF.Exp,
                                                                    bias=bias[:, h, i, j:j + 1], scale=0.125),
                                      reads=[bbuf[bs], FB], writes=[PB[pb]])
                        else:
                            kb.op("act", lambda e: e.activation(out=PT[pb][:, 0:Wd], in_=banks[bs][:, 0:Wd], func=AF.Exp, scale=0.125),
                                  reads=[bbuf[bs]], writes=[PB[pb]])
                        for i in range(ilo, ihi + 1):
                            off = (i - ilo) * 128
                            if i == j:
                                kb.op("pool", lambda e: e.tensor_tensor(out=PT[pb][:, off:off + 128], in0=PT[pb][:, off:off + 128],
                                                                        in1=K["mask_le"][:], op=ALU.mult),
                                      reads=[PB[pb], KBUF], writes=[PB[pb]])
                            elif not fox:
                                kb.op("pool", lambda e: e.tensor_tensor(out=PT[pb][:, off:off + 128], in0=PT[pb][:, off:off + 128],
                                                                        in1=K["mask_gt"][:], op=ALU.mult),
                                      reads=[PB[pb], KBUF], writes=[PB[pb]])

                    def emit_pv(k):
                        hf, j, ilo, ihi = steps[k]
                        Wd = (ihi - ilo + 1) * 128
                        cols = slice((ilo - i0) * 128, (ihi - i0 + 1) * 128)
                        pb = (si + k) % NPT
                        first = (k == 0)
                        last = (k == len(steps) - 1)
                        kb.op("pe", lambda e: e.matmul(banks[bo][:, cols], VP[:, j, kc_, hf * 64:hf * 64 + 128], PT[pb][:, 0:Wd],
                                                       start=first, stop=last, skip_group_check=True), reads=[LB, PB[pb]], writes=[bbuf[bo]])
                        kb.op("pe", lambda e: e.matmul(banks[bd][:, cols], K["onespad"][:, hf * 64:hf * 64 + 128], PT[pb][:, 0:Wd],
                                                       start=first, stop=last, skip_group_check=True), reads=[KBUF, PB[pb]], writes=[bbuf[bd]])

                    LOOK = 2
                    for k in range(len(steps) + LOOK):
                        if k < len(steps):
                            emit_score(k)
                        if k - LOOK >= 0:
                            emit_pv(k - LOOK)
                    si += len(steps)
                    gsl = slice(i0 * 128, (i1 + 1) * 128)
                    if fox:
                        kb.op("dve", lambda e: e.reciprocal(out=rd[r][:, 0:GW_], in_=banks[bd][:, 0:GW_]),
                              reads=[bbuf[bd]], writes=[rdb[r]])
                    else:
                        kb.op("dve", lambda e: e.tensor_scalar(out=rd[r][:, 0:GW_], in0=banks[bd][:, 0:GW_],
                                                               scalar1=esp[:, c:c + 1], scalar2=None, op0=ALU.add),
                              reads=[bbuf[bd], FB], writes=[rdb[r]])
                        kb.op("dve", lambda e: e.reciprocal(out=rd[r][:, 0:GW_], in_=rd[r][:, 0:GW_]), reads=[rdb[r]], writes=[rdb[r]])
                    kb.op("dve", lambda e: e.tensor_tensor(out=OT[:, c, gsl], in0=banks[bo][:, 0:GW_], in1=rd[r][:, 0:GW_], op=ALU.mult),
                          reads=[bbuf[bo], rdb[r]], writes=[OB])
            dst = oT[1] if fox else oT[0]
            kb.dma("sp", dst.rearrange("(c p) s -> p c s", p=128), OT[:], reads=[OB])
            kb.barrier()

    def stage_linattn(l, ret):
        with ExitStack() as st:
            qin = sb(st, "la_qin", [64, 4, S], BF16)
            kin = sb(st, "la_kin", [64, 4, S], BF16)
            OTs = sb(st, "la_o", [128, 4, S], BF16)
            LB = Buf()
            QB = Buf()
            OB = Buf()
            dec = sb(st, "la_dec", [64, 4, NCH])
            DB = Buf()
            Sst = sb(st, "la_S", [64, 4, 128])
            Sbf = sb(st, "la_Sb", [64, 4, 128], BF16)
            SB_ = Buf()
            SBb = Buf()
            ksr = [sb(st, "la_ks%d" % i, [64, 256], BF16) for i in range(3)]
            ksb = [Buf() for _ in range(3)]
            vt = [sb(st, "la_v%d" % i, [64, 512], BF16) for i in range(3)]
            vtb = [Buf() for _ in range(3)]
            gt = [sb(st, "la_g%d" % i, [64, 512]) for i in range(3)]
            gtb = [Buf() for _ in range(3)]
            kt = [sb(st, "la_kt%d" % i, [64, 256]) for i in range(3)]
            ktb = [Buf() for _ in range(3)]
            sm = [sb(st, "la_sm%d" % i, [64, 4, 64], BF16) for i in range(3)]
            smb = [Buf() for _ in range(3)]
            ss = sb(st, "la_ss", [64, 8])
            st2 = sb(st, "la_st2", [64, 8])
            junk = sb(st, "la_junk", [64, 128])
            t1 = [sb(st, "la_t1%d" % i, [64, 512]) for i in range(3)]
            t1b = [Buf() for _ in range(3)]
            ob16 = [sb(st, "la_ob%d" % i, [64, 512], BF16) for i in range(3)]
            obb = [Buf() for _ in range(3)]
            SS = Buf()
            gtab = sb(st, "la_gtab", [64, 512])
            btab = sb(st, "la_btab", [64, 512])
            TB = Buf()
            kb.op("dve", lambda e: e.memset(Sst[:], 0.0), writes=[SB_])
            kb.op("pool", lambda e: e.memset(Sbf[:], 0.0), writes=[SBb])
            qsrc, ksrc, ktok, vsrc, gsrc, odst = (qTd, kTd, kd_tok, vd, gd, oT[3]) if ret else (qTc, kTc, kc_tok, vc, rc, oT[2])
            qsv = qsrc.rearrange("(h p) s -> p h s", p=64)
            ksv = ksrc.rearrange("(h p) s -> p h s", p=64)
            if ret:
                ctk = sb(st, "la_ctk", [64, NCH, 32])
                stk = sb(st, "la_stk", [64, NCH, 32])
                kb.dma("sp", ctk[:], cin["cos_tok"], writes=[LB])
                kb.dma("sp", stk[:], cin["sin_tok"], writes=[LB])
                kb.dma("sp", gtab[:], ret_w[l, :].partition_broadcast(64), writes=[TB])
                kb.dma("sp", btab[:], ret_b[l, :].partition_broadcast(64), writes=[TB])
                with ExitStack() as st_pre:
                    cosT = sb(st_pre, "la_cos", [64, S])
                    sinT = sb(st_pre, "la_sin", [64, S])
                    SH = S // 2
                    qx = sb(st_pre, "la_qx", [64, SH])
                    qr = sb(st_pre, "la_qr", [64, SH])
                    kb.dma("sp", cosT[:], cin["cosT"][0:64, :], writes=[LB])
                    kb.dma("sp", sinT[:], cin["sinT"][0:64, :], writes=[LB])
                    for (srcv, rsrc, dstt, tab) in ((qsv, qrTd, qin, "ret_dq"), (ksv, krTd, kin, "ret_dk")):
                        rsv = rsrc.rearrange("(h p) s -> p h s", p=64)
                        for h in range(4):
                            for hh in range(2):
                                hsl = slice(hh * SH, (hh + 1) * SH)
                                kb.dma("sp", qx[:], srcv[:, h, hsl], reads=[LB], writes=[LB])
                                kb.dma("sp", qr[:], rsv[:, h, hsl], reads=[LB], writes=[LB])
                                kb.op("dve", lambda e: e.tensor_tensor(out=qx[:], in0=qx[:], in1=cosT[:, hsl], op=ALU.mult),
                                      reads=[LB], writes=[LB])
                                kb.op("pool", lambda e: e.tensor_tensor(out=qr[:], in0=qr[:], in1=sinT[:, hsl], op=ALU.mult),
                                      reads=[LB], writes=[LB])
                                kb.op("dve", lambda e: e.tensor_tensor(out=qx[:], in0=qx[:], in1=qr[:], op=ALU.add),
                                      reads=[LB], writes=[LB])
                                kb.op("pool", lambda e: e.tensor_tensor(
                                    out=dstt[:, h, hsl].rearrange("p (n j) -> p n j", j=64),
                                    in0=qx[:].rearrange("p (n j) -> p n j", j=64),
                                    in1=K[tab][:, h, :].unsqueeze(1).broadcast_to([64, NCH // 2, 64]), op=ALU.mult),
                                    reads=[LB, KBUF], writes=[QB])
                    kb.barrier()
                kb.op("dve", lambda e: e.tensor_copy(out=dec[:], in_=K["ret_dec"][:].unsqueeze(2).broadcast_to([64, 4, NCH])),
                      reads=[KBUF], writes=[DB])
            else:
                lr = sb(st, "la_lr", [33, S])
                wga = sb(st, "la_wga", [33, 256])
                kb.op("dve", lambda e: e.memset(lr[:], 1.0), writes=[LB])
                kb.op("dve", lambda e: e.memset(wga[:], 0.0), writes=[TB])
                kb.dma("sp", lr[0:16, :], lrT[:, :], reads=[LB], writes=[LB])
                kb.dma("sp", wga[0:16, :], gla_wg[l], reads=[TB], writes=[TB])
                kb.dma("sp", wga[32:33, :], gla_bg[l:l + 1, :], reads=[TB], writes=[TB])
                kb.dma("sp", gtab[:, 0:128], gla_ng[l, :].partition_broadcast(64), writes=[TB])
                lsb = [sb(st, "la_l%d" % i, [64, 256]) for i in range(3)]
                lbb = [Buf() for _ in range(3)]
                eq = [sb(st, "la_eq%d" % i, [64, 4, 64]) for i in range(3)]
                eqb = [Buf() for _ in range(3)]
                ek = [sb(st, "la_ek%d" % i, [64, 4, 64]) for i in range(3)]
                ekb = [Buf() for _ in range(3)]
                ed = [sb(st, "la_ed%d" % i, [64, 256]) for i in range(3)]
                edb = [Buf() for _ in range(3)]
                qch = [sb(st, "la_qch%d" % i, [64, 4, 64]) for i in range(3)]
                kch = [sb(st, "la_kch%d" % i, [64, 4, 64]) for i in range(3)]
                qcb = [Buf() for _ in range(3)]

            def phase(n, ph):
                b = n % 3
                cs = slice(n * 64, (n + 1) * 64)
                bo = 4 + n % 2
                o3 = banks[bo][0:64, :].rearrange("p (h d) -> p h d", h=4)
                if ph == 0:
                    kb.dma("sp", vt[b][:], vsrc[n * 64:(n + 1) * 64, :], writes=[vtb[b]])
                    kb.dma("sp", gt[b][:], gsrc[n * 64:(n + 1) * 64, :], writes=[gtb[b]])
                    kb.dma("sp", kt[b][:], ktok[n * 64:(n + 1) * 64, :], writes=[ktb[b]])
                    if not ret:
                        kb.dma("sp", qch[b][:], qsv[:, :, cs], writes=[qcb[b]])
                        kb.dma("sp", kch[b][:], ksv[:, :, cs], writes=[qcb[b]])
                        kb.op("pe", lambda e: e.matmul(banks[0][0:64, 0:256], lr[:, cs], wga[:], start=True, stop=True),
                              reads=[LB, TB], writes=[bbuf[0]])
                        kb.op("act", lambda e: e.activation(out=lsb[b][:], in_=banks[0][0:64, 0:256], func=AF.Exp, scale=-1.0),
                              reads=[bbuf[0]], writes=[lbb[b]])
                        kb.op("act", lambda e: e.activation(out=lsb[b][:], in_=lsb[b][:], func=AF.Ln, bias=1.0, scale=1.0),
                              reads=[lbb[b]], writes=[lbb[b]])
                        for h in range(4):
                            kb.op("pe", lambda e: e.matmul(banks[1][0:64, h * 64:(h + 1) * 64], lsb[b][:, h * 64:(h + 1) * 64],
                                                           K["tri64n"][:], start=(h == 0), stop=True, skip_group_check=True),
                                  reads=[lbb[b], KBUF], writes=[bbuf[1]])
                        kb.op("pe", lambda e: e.matmul(banks[2][0:64, 0:256], K["d64n"][:], lsb[b][:], start=True, stop=True),
                              reads=[lbb[b], KBUF], writes=[bbuf[2]])
                        kb.op("act", lambda e: e.activation(out=eq[b][:].rearrange("p c j -> p (c j)"), in_=banks[1][0:64, 0:256], func=AF.Exp),
                              reads=[bbuf[1]], writes=[eqb[b]])
                        kb.op("act", lambda e: e.activation(out=ek[b][:].rearrange("p c j -> p (c j)"), in_=banks[1][0:64, 0:256], func=AF.Exp, scale=-1.0),
                              reads=[bbuf[1]], writes=[ekb[b]])
                        kb.op("act", lambda e: e.activation(out=ed[b][:], in_=banks[2][0:64, 0:256], func=AF.Exp),
                              reads=[bbuf[2]], writes=[edb[b]])
                        kb.op("dve", lambda e: e.scalar_tensor_tensor(out=qin[:, :, cs], in0=qch[b][:], scalar=0.125, in1=eq[b][:],
                                                                      op0=ALU.mult, op1=ALU.mult), reads=[qcb[b], eqb[b]], writes=[QB])
                        kb.op("pool", lambda e: e.tensor_tensor(out=kin[:, :, cs], in0=kch[b][:], in1=ek[b][:], op=ALU.mult),
                              reads=[qcb[b], ekb[b]], writes=[QB])
                        kb.op("dve", lambda e: e.tensor_copy(out=dec[:, :, n:n + 1], in_=eq[b][:, :, 63:64]),
                              reads=[eqb[b]], writes=[DB])
                        kb.op("dve", lambda e: e.tensor_tensor(out=ksr[b][:], in0=kt[b][:], in1=ed[b][:], op=ALU.mult),
                              reads=[ktb[b], edb[b]], writes=[ksb[b]])
                    else:
                        k4 = kt[b][:].rearrange("p (h two f) -> p h two f", h=4, two=2)
                        o4 = t1[b][:, 0:256].rearrange("p (h two f) -> p h two f", h=4, two=2)
                        j4 = t1[b][:, 256:512].rearrange("p (h two f) -> p h two f", h=4, two=2)
                        cb = ctk[:, n, :].unsqueeze(1).broadcast_to([64, 4, 32])
                        sbn = stk[:, n, :].unsqueeze(1).broadcast_to([64, 4, 32])
                        rd_ = [ktb[b], LB]
                        kb.op("dve", lambda e: e.tensor_tensor(out=o4[:, :, 0, :], in0=k4[:, :, 0, :], in1=cb, op=ALU.mult), reads=rd_, writes=[t1b[b]])
                        kb.op("pool", lambda e: e.tensor_tensor(out=j4[:, :, 0, :], in0=k4[:, :, 1, :], in1=sbn, op=ALU.mult), reads=rd_, writes=[t1b[b]])
                        kb.op("dve", lambda e: e.tensor_tensor(out=o4[:, :, 0, :], in0=o4[:, :, 0, :], in1=j4[:, :, 0, :], op=ALU.subtract),
                              reads=[t1b[b]], writes=[t1b[b]])
                        kb.op("dve", lambda e: e.tensor_tensor(out=o4[:, :, 1, :], in0=k4[:, :, 1, :], in1=cb, op=ALU.mult), reads=rd_, writes=[t1b[b]])
                        kb.op("pool", lambda e: e.tensor_tensor(out=j4[:, :, 1, :], in0=k4[:, :, 0, :], in1=sbn, op=ALU.mult), reads=rd_, writes=[t1b[b]])
                        kb.op("dve", lambda e: e.tensor_tensor(out=o4[:, :, 1, :], in0=o4[:, :, 1, :], in1=j4[:, :, 1, :], op=ALU.add),
                              reads=[t1b[b]], writes=[t1b[b]])
                        kb.op("dve", lambda e: e.tensor_tensor(
                            out=ksr[b][:].rearrange("p (h d) -> p h d", h=4), in0=t1[b][:, 0:256].rearrange("p (h d) -> p h d", h=4),
                            in1=K["ret_ks"][:].unsqueeze(2).broadcast_to([64, 4, 64]), op=ALU.mult),
                            reads=[t1b[b], KBUF], writes=[ksb[b]])
                if ph == 1:
                    bs = 3
                    for h in range(4):
                        kb.op("pe", lambda e: e.matmul(banks[bs][0:64, h * 64:(h + 1) * 64], kin[:, h, cs], qin[:, h, cs],
                                                       start=(h == 0), stop=True, skip_group_check=True),
                              reads=[QB], writes=[bbuf[bs]])
                    kb.op("dve", lambda e: e.tensor_tensor(out=sm[b][:], in0=banks[bs][0:64, 0:256].rearrange("p (h i) -> p h i", h=4),
                                                           in1=K["mask_le"][0:64, 0:64].unsqueeze(1).broadcast_to([64, 4, 64]), op=ALU.mult),
                          reads=[bbuf[bs], KBUF], writes=[smb[b]])
                    for h in range(4):
                        kb.op("pe", lambda e: e.matmul(banks[bo][0:64, h * 128:(h + 1) * 128], qin[:, h, cs], Sbf[:, h, :],
                                                       start=(h == 0), stop=False, skip_group_check=True),
                              reads=[QB, SBb], writes=[bbuf[bo]])
                        kb.op("pe", lambda e: e.matmul(banks[bo][0:64, h * 128:(h + 1) * 128], sm[b][:, h, :], vt[b][:, h * 128:(h + 1) * 128],
                                                       start=False, stop=True, skip_group_check=True),
                              reads=[smb[b], vtb[b]], writes=[bbuf[bo]])
                    bu = 6
                    for h in range(4):
                        kb.op("pe", lambda e: e.matmul(banks[bu][0:64, h * 128:(h + 1) * 128], ksr[b][:, h * 64:(h + 1) * 64], vt[b][:, h * 128:(h + 1) * 128],
                                                       start=(h == 0), stop=True, skip_group_check=True), reads=[ksb[b], vtb[b]], writes=[bbuf[bu]])
                    kb.op("dve", lambda e: e.tensor_tensor(out=Sst[:], in0=Sst[:], in1=dec[:, :, n:n + 1].broadcast_to([64, 4, 128]), op=ALU.mult),
                          reads=[SB_, DB], writes=[SB_])
                    kb.op("dve", lambda e: e.tensor_tensor(out=Sst[:], in0=banks[bu][0:64, :].rearrange("p (h d) -> p h d", h=4), in1=Sst[:], op=ALU.add),
                          reads=[SB_, bbuf[bu]], writes=[SB_])
                    kb.op("act", lambda e: e.activation(out=Sbf[:], in_=Sst[:], func=AF.Copy), reads=[SB_], writes=[SBb])
                    kb.op("dve", lambda e: e.memset(ss[:], 0.0), reads=[SS], writes=[SS])
                if ph == 2:
                    for h in range(4):
                        kb.op("act", lambda e: e.activation(out=junk[:], in_=banks[bo][0:64, h * 128:(h + 1) * 128], func=AF.Square,
                                                            accum_out=ss[:, h:h + 1]), reads=[bbuf[bo]], writes=[SS])
                        if ret:
                            kb.op("act", lambda e: e.activation(out=junk[:], in_=banks[bo][0:64, h * 128:(h + 1) * 128], func=AF.Identity,
                                                                accum_out=ss[:, 4 + h:5 + h]), reads=[bbuf[bo]], writes=[SS])
                    if not ret:
                        kb.op("dve", lambda e: e.tensor_scalar(out=st2[:, 0:4], in0=ss[:, 0:4], scalar1=1.0 / 128, scalar2=EPS, op0=ALU.mult, op1=ALU.add),
                              reads=[SS], writes=[SS])
                        kb.op("act", lambda e: e.activation(out=st2[:, 0:4], in_=st2[:, 0:4], func=AF.Ln), reads=[SS], writes=[SS])
                        kb.op("act", lambda e: e.activation(out=st2[:, 0:4], in_=st2[:, 0:4], func=AF.Exp, scale=-0.5), reads=[SS], writes=[SS])
                        kb.op("dve", lambda e: e.tensor_tensor(out=t1[b][:].rearrange("p (h d) -> p h d", h=4), in0=o3,
                                                               in1=st2[:, 0:4].unsqueeze(2).broadcast_to([64, 4, 128]), op=ALU.mult),
                              reads=[bbuf[bo], SS], writes=[t1b[b]])
                        kb.op("pool", lambda e: e.tensor_tensor(out=t1[b][:].rearrange("p (h d) -> p h d", h=4),
                                                                in0=t1[b][:].rearrange("p (h d) -> p h d", h=4),
                                                                in1=gtab[:, 0:128].unsqueeze(1).broadcast_to([64, 4, 128]), op=ALU.mult),
                              reads=[t1b[b], TB], writes=[t1b[b]])
                    else:
                        kb.op("dve", lambda e: e.tensor_scalar(out=st2[:, 4:8], in0=ss[:, 4:8], scalar1=1.0 / 128, scalar2=None, op0=ALU.mult),
                              reads=[SS], writes=[SS])
                        kb.op("dve", lambda e: e.tensor_tensor(out=st2[:, 0:4], in0=st2[:, 4:8], in1=st2[:, 4:8], op=ALU.mult), reads=[SS], writes=[SS])
                        kb.op("dve", lambda e: e.scalar_tensor_tensor(out=st2[:, 0:4], in0=ss[:, 0:4], scalar=1.0 / 128, in1=st2[:, 0:4],
                                                                      op0=ALU.mult, op1=ALU.subtract), reads=[SS], writes=[SS])
                        kb.op("act", lambda e: e.activation(out=st2[:, 0:4], in_=st2[:, 0:4], func=AF.Ln, bias=epsD[0:64, 1:2], scale=1.0), reads=[SS, VB], writes=[SS])
                        kb.op("act", lambda e: e.activation(out=st2[:, 0:4], in_=st2[:, 0:4], func=AF.Exp, scale=-0.5), reads=[SS], writes=[SS])
                        kb.op("dve", lambda e: e.tensor_tensor(out=t1[b][:].rearrange("p (h d) -> p h d", h=4), in0=o3,
                                                               in1=st2[:, 4:8].unsqueeze(2).broadcast_to([64, 4, 128]), op=ALU.subtract),
                              reads=[bbuf[bo], SS], writes=[t1b[b]])
                        kb.op("dve", lambda e: e.tensor_tensor(out=t1[b][:].rearrange("p (h d) -> p h d", h=4),
                                                               in0=t1[b][:].rearrange("p (h d) -> p h d", h=4),
                                                               in1=st2[:, 0:4].unsqueeze(2).broadcast_to([64, 4, 128]), op=ALU.mult),
                              reads=[t1b[b], SS], writes=[t1b[b]])
                        kb.op("pool", lambda e: e.tensor_tensor(out=t1[b][:], in0=t1[b][:], in1=gtab[:], op=ALU.mult), reads=[t1b[b], TB], writes=[t1b[b]])
                        kb.op("pool", lambda e: e.tensor_tensor(out=t1[b][:], in0=t1[b][:], in1=btab[:], op=ALU.add), reads=[t1b[b], TB], writes=[t1b[b]])
                    kb.op("pool", lambda e: e.tensor_tensor(out=ob16[b][:], in0=t1[b][:], in1=gt[b][:], op=ALU.mult),
                          reads=[t1b[b], gtb[b]], writes=[obb[b]])
                    for h in range(4):
                        kb.op("pe", lambda e: e.transpose(trps[:, h, :], ob16[b][:, h * 128:(h + 1) * 128], K["ident_b"][0:64, 0:64]),
                              reads=[obb[b], KBUF], writes=[trb])
                    kb.op("act", lambda e: e.activation(out=OTs[:, :, cs], in_=trps[:], func=AF.Copy), reads=[trb], writes=[OB])

            for it in range(NCH + 2):
                if it < NCH:
                    phase(it, 0)
                if 0 <= it - 1 < NCH:
                    phase(it - 1, 1)
                if 0 <= it - 2 < NCH:
                    phase(it - 2, 2)
            kb.dma("sp", odst.rearrange("(c p) s -> p c s", p=128), OTs[:], reads=[OB])
            kb.barrier()

    def stage_merge(l, Gap):
        T = min(1024, S)
        HW = min(512, T)
        NH = T // HW
        GW = 4
        with ExitStack() as st:
            bmS = sb(st, "mg_bm", [128, 4 * DC])
            BM = Buf()
            with ExitStack() as st_pre:
                brow = sb(st_pre, "mg_brow", [1, 4 * D])
                rb = Buf()
                kb.dma("sp", brow[:], b_merge[l:l + 1, :], writes=[rb])
                row_to_cols(lambda j: banks[0][:, j:j + 1], brow, 4 * DC, 0, rb)
                kb.op("act", lambda e: e.activation(out=bmS[:], in_=banks[0][:, 0:4 * DC], func=AF.Copy), reads=[bbuf[0]], writes=[BM])
                kb.barrier()
            ut = sb(st, "mg_u", [128, DC, T], BF16)
            utb = Buf()
            ot = [sb(st, "mg_o%d" % i, [128, 4, T], BF16) for i in range(2)]
            otb = [Buf() for _ in range(2)]
            mg = sb(st, "mg_m", [128, DC, T])
            mgb = Buf()
            mgh = sb(st, "mg_mh", [128, DC, T], BF16)
            mghb = Buf()
            wm = [sb(st, "mg_wm%d" % i, [128, DC, GW * 128], BF16) for i in range(2)]
            wmb = [Buf() for _ in range(2)]
            wbr = [sb(st, "mg_wb%d" % i, [128, 4, GW * 128], BF16) for i in range(2)]
            wbb = [Buf() for _ in range(2)]
            gs = [sb(st, "mg_g%d" % i, [128, HW]) for i in range(2)]
            gsb = [Buf() for _ in range(2)]
            hr = [sb(st, "mg_hr%d" % i, [128, T]) for i in range(2)]
            hrb = [Buf() for _ in range(2)]
            uTv = uT.rearrange("(dc p) s -> p dc s", p=128)
            wi = 0
            pi = 0
            for ti in range(S // T):
                ts = slice(ti * T, (ti + 1) * T)
                kb.dma("sp", ut[:], uTv[:, :, ts], writes=[utb])
                for br in range(4):
                    ob_ = br % 2
                    kb.dma("sp", ot[ob_][:], oT[br].rearrange("(c p) s -> p c s", p=128)[:, :, ts], writes=[otb[ob_]])
                    wmv = w_merge[l, br].rearrange("(kc p) d -> p kc d", p=128)
                    wbv = w_branch[l, br].rearrange("(c p) d -> p c d", p=128)
                    for dg in range(DC // GW):
                        b = wi % 2
                        wi += 1
                        gsl = slice(dg * GW * 128, (dg + 1) * GW * 128)
                        kb.dma("pool", wm[b][:], wmv[:, :, gsl], writes=[wmb[b]])
                        if br == 0:
                            wv2 = w_branch[l, br].rearrange("(g c p) d -> p g c d", g=2, p=64)
                            kb.dma("pool", wbr[b][0:64, :, :], wv2[:, 0, :, gsl], writes=[wbb[b]])
                            kb.dma("pool", wbr[b][64:128, :, :], wv2[:, 1, :, gsl], writes=[wbb[b]])
                        else:
                            kb.dma("pool", wbr[b][:], wbv[:, :, gsl], writes=[wbb[b]])
                        for d in range(GW):
                            dc = dg * GW + d
                            dsl = slice(d * 128, (d + 1) * 128)
                            for hf in range(NH):
                                hs_ = slice(hf * HW, (hf + 1) * HW)
                                bg = 2 * (pi % 3)
                                by = bg + 1
                                gi_ = pi % 2
                                pi += 1
                                for kc in range(DC):
                                    kb.op("pe", lambda e: e.matmul(banks[bg][:, 0:HW], wm[b][:, kc, dsl], ut[:, kc, hs_],
                                                                   start=(kc == 0), stop=(kc == DC - 1)),
                                          reads=[wmb[b], utb], writes=[bbuf[bg]])
                                for c in range(4):
                                    kb.op("pe", lambda e: e.matmul(banks[by][:, 0:HW], wbr[b][:, c, dsl], ot[ob_][:, c, hs_],
                                                                   start=(c == 0), stop=(c == 3)),
                                          reads=[wbb[b], otb[ob_]], writes=[bbuf[by]])
                                kb.op("act", lambda e: e.activation(
                                    out=gs[gi_][:], in_=banks[bg][:, 0:HW], func=AF.Sigmoid, bias=bmS[:, br * DC + dc:br * DC + dc + 1], scale=1.0),
                                    reads=[bbuf[bg], BM], writes=[gsb[gi_]])
                                if br == 0:
                                    kb.op("dve", lambda e: e.tensor_tensor(out=mg[:, dc, hs_], in0=banks[by][:, 0:HW], in1=gs[gi_][:], op=ALU.mult),
                                          reads=[bbuf[by], gsb[gi_]], writes=[mgb])
                                else:
                                    kb.op("dve", lambda e: e.tensor_tensor(out=gs[gi_][:], in0=banks[by][:, 0:HW], in1=gs[gi_][:], op=ALU.mult),
                                          reads=[bbuf[by], gsb[gi_]], writes=[gsb[gi_]])
                                    if br < 3:
                                        kb.op("pool", lambda e: e.tensor_tensor(out=mg[:, dc, hs_], in0=mg[:, dc, hs_], in1=gs[gi_][:], op=ALU.add),
                                              reads=[gsb[gi_], mgb], writes=[mgb])
                                    else:
                                        kb.op("pool", lambda e: e.tensor_tensor(out=mgh[:, dc, hs_], in0=mg[:, dc, hs_], in1=gs[gi_][:], op=ALU.add),
                                              reads=[gsb[gi_], mgb], writes=[mghb])
                wov = w_out[l].rearrange("(kc p) d -> p kc d", p=128)
                for dg in range(DC // GW):
                    b = wi % 2
                    wi += 1
                    gsl = slice(dg * GW * 128, (dg + 1) * GW * 128)
                    kb.dma("pool", wm[b][:], wov[:, :, gsl], writes=[wmb[b]])
                    for d in range(GW):
                        dc = dg * GW + d
                        dsl = slice(d * 128, (d + 1) * 128)
                        hb_i = dc % 2
                        kb.dma("sp", hr[hb_i][:], hT[dc * 128:(dc + 1) * 128, ts], writes=[hrb[hb_i]])
                        for hf in range(NH):
                            hs_ = slice(hf * HW, (hf + 1) * HW)
                            bk = pi % NB
                            pi += 1
                            for kc in range(DC):
                                kb.op("pe", lambda e: e.matmul(banks[bk][:, 0:HW], wm[b][:, kc, dsl], mgh[:, kc, hs_],
                                                               start=(kc == 0), stop=(kc == DC - 1)),
                                      reads=[wmb[b], mghb], writes=[bbuf[bk]])
                            kb.op("dve", lambda e: e.scalar_tensor_tensor(
                                out=hr[hb_i][:, hs_], in0=banks[bk][:, 0:HW], scalar=Gap[:, dc:dc + 1], in1=hr[hb_i][:, hs_],
                                op0=ALU.mult, op1=ALU.add), reads=[bbuf[bk], VB, hrb[hb_i]], writes=[hrb[hb_i]])
                        kb.dma("sp", hT[dc * 128:(dc + 1) * 128, ts], hr[hb_i][:], reads=[hrb[hb_i]])
            kb.barrier()

    K["mask_le_f"] = sb(es, "K_mask_le_f", [128, 128])
    kb.op("dve", lambda e: e.tensor_copy(out=K["mask_le_f"][:], in_=K["mask_le"][:]), reads=[KBUF], writes=[KBUF])
    kb.barrier()

    stages = build_program.stages
    if "tin" in stages:
        stage_transpose_in()
    stage_cond()
    for l in range(L):
        stage_mod(l)
        if "ffn1" in stages:
            stage_norm(Avec[:, 0:DC], modS[:, 0:DC])
            stage_ffn(l, 0, Gvec[:, 0:DC])
        if "mix" in stages:
            stage_norm(Avec[:, DC:2 * DC], modS[:, 3 * DC:4 * DC])
            stage_inproj(l)
            if "swa" in stages:
                stage_attn(l, False)
            if "fox" in stages:
                stage_attn(l, True)
            if "gla" in stages or "ret" in stages:
                pass
            if "gla" in stages:
                stage_linattn(l, False)
            if "ret" in stages:
                stage_linattn(l, True)
            if "merge" in stages:
                stage_merge(l, Gvec[:, DC:2 * DC])
        if "ffn2" in stages:
            stage_norm(Avec[:, 2 * DC:3 * DC], modS[:, 6 * DC:7 * DC])
            stage_ffn(l, 1, Gvec[:, 2 * DC:3 * DC])
    if "fin" in stages:
        stage_norm(finA[:], None, final=True)
    kb.barrier()
    es.close()
    return nc, hc, kb


build_program.stages = {"tin", "ffn1", "mix", "swa", "fox", "gla", "ret", "merge", "ffn2", "fin"}

WNAMES = ["w_ada", "b_ada", "ffn_w_gate", "ffn_w_up", "ffn_w_down", "w_in", "fox_b_forget", "attn_sinks",
          "gla_w_gate", "gla_b_gate", "gla_norm_g", "ret_gn_w", "ret_gn_b", "w_branch", "w_out"]


def make_in_maps(inputs, hc, S, L, nb):
    f = lambda a: np.ascontiguousarray(np.asarray(a, dtype=np.float32))
    shared = {n: f(inputs[n])[:L] for n in WNAMES}
    shared["norm_g"] = f(inputs["norm_g"])[:L].reshape(L, 3 * D)
    shared["w_merge"] = f(inputs["w_merge"])[:L]
    shared["b_merge"] = f(inputs["b_merge"])[:L].reshape(L, 4 * D)
    shared["final_norm_g"] = f(inputs["final_norm_g"]).reshape(1, D)
    for k, v in hc.items():
        shared["k_" + k] = v
    maps = []
    xs = f(inputs["x"])
    cs = f(inputs["c"])
    for core in range(8):
        b = core % nb
        m = dict(shared)
        m["x"] = np.ascontiguousarray(xs[b, :S])
        m["c"] = np.ascontiguousarray(cs[b:b + 1])
        maps.append(m)
    return maps


def kernel(**inputs):
    nc, hc, kb = build_program(SEQ, DEPTH)
    maps = make_in_maps(inputs, hc, SEQ, DEPTH, BATCH)
    res = run_bass_kernel_spmd(nc, maps, core_ids=list(range(8)))
    outs = [np.asarray(res.results[b]["out"], dtype=np.float32) for b in range(BATCH)]
    return np.stack(outs, axis=0)
```

```python
import numpy as np
import ml_dtypes
from contextlib import ExitStack
import concourse.bass as bass
import concourse.mybir as mybir
from concourse.bass_utils import run_bass_kernel_spmd

F32 = mybir.dt.float32
BF16 = mybir.dt.bfloat16
AF = mybir.ActivationFunctionType
ALU = mybir.AluOpType

D = 2048
DC = 16
DFF = 5632
FC = 44
NMOD = 9
EPS = 1e-6
DEPTH = 4
SEQ = 4096
BATCH = 4
INCOLS = 5400
A_Q, A_K, A_V = 0, 512, 640
B_Q, B_K, B_V, B_F = 768, 1280, 1792, 2304
C_Q, C_K, C_V, C_LR, C_R = 2312, 2568, 2824, 3336, 3352
D_Q, D_K, D_V, D_G = 3864, 4120, 4376, 4888


import os
LA_CUT = int(os.environ.get('LA_CUT', '0'))


class Buf:
    __slots__ = ("w", "r")

    def __init__(self):
        self.w = None
        self.r = {}


class KB:
    def __init__(self, nc, es):
        self.nc = nc
        self.eng = {"pe": nc.tensor, "act": nc.scalar, "dve": nc.vector, "pool": nc.gpsimd, "sp": nc.sync}
        self.psem = {e: es.enter_context(nc.semaphore("p_" + e)) for e in self.eng}
        self.cnt = {e: 0 for e in self.eng}
        self.seen = {e: {} for e in self.eng}
        self.semof = {("e", e): self.psem[e] for e in self.eng}
        self.dq = {}
        self.dnext = {}
        for q, n in (("sp", 24), ("pool", 12)):
            self.dq[q] = []
            for i in range(n):
                sem = es.enter_context(nc.semaphore("d_%s%d" % (q, i)))
                self.dq[q].append([sem, 0])
                self.semof[("d", q, i)] = sem
            self.dnext[q] = 0
        self.ninst = 0

    def _deps(self, reads, writes):
        d = {}
        for b in reads:
            if b.w is not None:
                k, v = b.w
                if d.get(k, 0) < v:
                    d[k] = v
        for b in writes:
            if b.w is not None:
                k, v = b.w
                if d.get(k, 0) < v:
                    d[k] = v
            for k, v in b.r.items():
                if d.get(k, 0) < v:
                    d[k] = v
        return d

    def _wait(self, e, d):
        seen = self.seen[e]
        for k, v in d.items():
            if e == "pe" and k == ("e", "pe"):
                continue
            if seen.get(k, 0) < v:
                self.eng[e].wait_ge(self.semof[k], v)
                seen[k] = v
                self.ninst += 1

    def _mark(self, tok, reads, writes):
        k, v = tok
        for b in reads:
            if b.r.get(k, 0) < v:
                b.r[k] = v
        for b in writes:
            b.w = tok
            b.r = {}

    def op(self, e, fn, reads=(), writes=()):
        self._wait(e, self._deps(reads, writes))
        ins = fn(self.eng[e])
        self.cnt[e] += 1
        ins.then_inc(self.psem[e], 1)
        self.ninst += 1
        self._mark((("e", e), self.cnt[e]), reads, writes)

    def dma(self, q, out, in_, reads=(), writes=()):
        slots = self.dq[q]
        i = self.dnext[q]
        self.dnext[q] = (i + 1) % len(slots)
        sl = slots[i]
        d = self._deps(reads, writes)
        key = ("d", q, i)
        if sl[1] > 0 and d.get(key, 0) < sl[1]:
            d[key] = sl[1]
        self._wait(q, d)
        ins = self.eng[q].dma_start(out=out, in_=in_)
        sl[1] += 16
        ins.then_inc(sl[0], 16)
        self.ninst += 1
        self._mark((key, sl[1]), reads, writes)

    def barrier(self):
        tgt = {("e", e): c for e, c in self.cnt.items() if c > 0}
        for q, slots in self.dq.items():
            for i, sl in enumerate(slots):
                if sl[1] > 0:
                    tgt[("d", q, i)] = sl[1]
        for e in self.eng:
            seen = self.seen[e]
            for k, v in tgt.items():
                if seen.get(k, 0) < v:
                    self.eng[e].wait_ge(self.semof[k], v)
                    seen[k] = v
                    self.ninst += 1


def host_consts(S):
    c = {}
    p = np.arange(128)
    c["ident_f"] = np.eye(128, dtype=np.float32)
    c["ident_b"] = np.eye(128, dtype=np.float32).astype(ml_dtypes.bfloat16)
    le = (p[:, None] <= p[None, :]).astype(np.float32)
    c["mask_le"] = le.astype(ml_dtypes.bfloat16)
    c["mask_gt"] = (1.0 - le).astype(ml_dtypes.bfloat16)
    c["ones_b"] = np.ones((128, 128), dtype=ml_dtypes.bfloat16)
    c["ones_f"] = np.ones((128, 128), dtype=np.float32)
    op = np.zeros((128, 192), np.float32)
    op[:, 0:64] = 1.0
    op[:, 128:192] = 1.0
    c["onespad"] = op.astype(ml_dtypes.bfloat16)
    sel = np.zeros((128, 128), np.float32)
    sel[63, :] = 1.0
    c["sel63"] = sel
    q = np.arange(64)
    c["tri64n"] = (-(q[:, None] <= q[None, :]).astype(np.float32) / 16.0)
    c["d64n"] = (-(q[:, None] > q[None, :]).astype(np.float32) / 16.0)
    half = 32
    inv = (10000.0 ** (-np.arange(half, dtype=np.float32) / half)).astype(np.float32)
    pos = np.arange(S, dtype=np.float32)
    ang = (pos[:, None] * inv[None, :]).astype(np.float32)
    cos = np.cos(ang).astype(np.float32)
    sin = np.sin(ang).astype(np.float32)
    fidx = (p % 64) % 32
    sgn = np.where((p % 64) < 32, -1.0, 1.0).astype(np.float32)
    c["cosT"] = np.ascontiguousarray(cos.T[fidx, :])
    c["sinT"] = np.ascontiguousarray(sin.T[fidx, :] * sgn[:, None])
    c["cos_tok"] = np.ascontiguousarray(cos.reshape(S // 64, 64, 32).transpose(1, 0, 2))
    c["sin_tok"] = np.ascontiguousarray(sin.reshape(S // 64, 64, 32).transpose(1, 0, 2))
    lg = np.log1p(-np.exp2(-5.0 - np.arange(4, dtype=np.float64)))
    j = np.arange(64, dtype=np.float64)
    dq = np.zeros((64, 4, 64), np.float64)
    dk = np.zeros((64, 4, 64), np.float64)
    dec = np.zeros((64, 4), np.float64)
    for h in range(4):
        dq[:, h, :] = 0.125 * np.exp((j + 1) * lg[h])[None, :]
        dk[:, h, :] = np.exp(-(j + 1) * lg[h])[None, :]
        dec[:, h] = np.exp(64 * lg[h])
    c["ret_dq"] = dq.astype(np.float32)
    c["ret_dk"] = dk.astype(np.float32)
    c["ret_dec"] = dec.astype(np.float32)
    ks = np.zeros((64, 4), np.float64)
    for h in range(4):
        ks[:, h] = np.exp((63 - j) * lg[h])
    c["ret_ks"] = ks.astype(np.float32)
    return c


CONST_DT = {"ident_b": BF16, "mask_le": BF16, "mask_gt": BF16, "ones_b": BF16, "onespad": BF16}


def build_program(S, L, debug=()):
    NT = S // 128
    NCH = S // 64
    nc = bass.Bass("TRN2", target_bir_lowering=False)
    es = ExitStack()
    kb = KB(nc, es)
    dbg = set(debug)

    def din(name, shape, dt=F32):
        return nc.dram_tensor(name, list(shape), dt, kind="ExternalInput").ap()

    def dscr(name, shape, dt=F32):
        kind = "ExternalOutput" if name in dbg else "Internal"
        return nc.dram_tensor(name, list(shape), dt, kind=kind).ap()

    x = din("x", [S, D])
    cvec = din("c", [1, D])
    w_ada = din("w_ada", [L, D, NMOD * D])
    b_ada = din("b_ada", [L, NMOD * D])
    norm_g = din("norm_g", [L, 3 * D])
    w_gate = din("ffn_w_gate", [L, 2, D, DFF])
    w_up = din("ffn_w_up", [L, 2, D, DFF])
    w_down = din("ffn_w_down", [L, 2, DFF, D])
    w_in = din("w_in", [L, D, INCOLS])
    fox_b = din("fox_b_forget", [L, 8])
    sinks = din("attn_sinks", [L, 8])
    gla_wg = din("gla_w_gate", [L, 16, 256])
    gla_bg = din("gla_b_gate", [L, 256])
    gla_ng = din("gla_norm_g", [L, 128])
    ret_w = din("ret_gn_w", [L, 512])
    ret_b = din("ret_gn_b", [L, 512])
    w_branch = din("w_branch", [L, 4, 512, D])
    w_merge = din("w_merge", [L, 4, D, D])
    b_merge = din("b_merge", [L, 4 * D])
    w_out = din("w_out", [L, D, D])
    fin_g = din("final_norm_g", [1, D])
    hc = host_consts(S)
    cin = {k: din("k_" + k, v.shape, CONST_DT.get(k, F32)) for k, v in hc.items()}
    out = nc.dram_tensor("out", [S, D], F32, kind="ExternalOutput").ap()

    hT = dscr("hT", [D, S])
    uT = dscr("uT", [D, S], BF16)
    qTa = dscr("qTa", [512, S], BF16)
    kTa = dscr("kTa", [128, S], BF16)
    va = dscr("va", [S, 128], BF16)
    qTb = dscr("qTb", [512, S], BF16)
    kTb = dscr("kTb", [512, S], BF16)
    vb = dscr("vb", [S, 512], BF16)
    fb = dscr("fb", [S, 8])
    qTc = dscr("qTc", [256, S])
    kTc = dscr("kTc", [256, S])
    kc_tok = dscr("kc_tok", [S, 256])
    vc = dscr("vc", [S, 512], BF16)
    lrT = dscr("lrT", [16, S])
    rc = dscr("rc", [S, 512])
    qTd = dscr("qTd", [256, S])
    qrTd = dscr("qrTd", [256, S])
    kTd = dscr("kTd", [256, S])
    krTd = dscr("krTd", [256, S])
    kd_tok = dscr("kd_tok", [S, 256])
    vd = dscr("vd", [S, 512], BF16)
    gd = dscr("gd", [S, 512])
    oT = [dscr("oT%d" % i, [512, S], BF16) for i in range(4)]

    uid = [0]

    def sb(stack, name, shape, dt=F32):
        uid[0] += 1
        return stack.enter_context(nc.sbuf_tensor("%s_%d" % (name, uid[0]), list(shape), dt))

    K = {}
    KBUF = Buf()
    for k, v in hc.items():
        if k in ("cosT", "sinT", "cos_tok", "sin_tok"):
            continue
        K[k] = sb(es, "K_" + k, v.shape, CONST_DT.get(k, F32))
        kb.dma("sp", K[k][:], cin[k], writes=[KBUF])
    condT = sb(es, "condT", [128, DC], BF16)
    modS = sb(es, "modS", [128, NMOD * DC])
    ngS = sb(es, "ngS", [128, 3 * DC])
    Avec = sb(es, "Avec", [128, 3 * DC])
    Gvec = sb(es, "Gvec", [128, 3 * DC])
    finA = sb(es, "finA", [128, DC])
    zeroB = sb(es, "zeroB", [128, 1])
    VB = Buf()
    kb.op("dve", lambda e: e.memset(zeroB[:], 0.0), writes=[VB])
    epsD = sb(es, "epsD", [128, 2])
    kb.op("dve", lambda e: e.memset(epsD[:, 0:1], float(D * EPS)), writes=[VB])
    kb.op("dve", lambda e: e.memset(epsD[:, 1:2], float(EPS)), writes=[VB])

    NB = 7
    banks = [es.enter_context(nc.psum_tensor("bank%d" % i, [128, 512], F32)) for i in range(NB)]
    bbuf = [Buf() for _ in range(NB)]
    trps = es.enter_context(nc.psum_tensor("trps", [128, 4, 64], BF16))
    trb = Buf()

    SQD = float(np.sqrt(D))

    def stage_transpose_in():
        with ExitStack() as st:
            xs = [sb(st, "ti_x%d" % i, [128, D]) for i in range(2)]
            xb = [Buf() for _ in range(2)]
            ys = [sb(st, "ti_y%d" % i, [128, DC, 128]) for i in range(2)]
            yb = [Buf() for _ in range(2)]
            hTv = hT.rearrange("(dc p) s -> p dc s", p=128)
            for nt in range(NT):
                b = nt % 2
                kb.dma("sp", xs[b][:], x[nt * 128:(nt + 1) * 128, :], writes=[xb[b]])
                for g in range(4):
                    bk = (nt * 4 + g) % NB
                    for i in range(4):
                        dc = g * 4 + i
                        kb.op("pe", lambda e, dc=dc, i=i, bk=bk, b=b: e.transpose(
                            banks[bk][:, i * 128:(i + 1) * 128], xs[b][:, dc * 128:(dc + 1) * 128], K["ident_f"][:]),
                            reads=[xb[b], KBUF], writes=[bbuf[bk]])
                    eng = "act" if g % 2 == 0 else "dve"
                    if eng == "act":
                        kb.op("act", lambda e, g=g, bk=bk, b=b: e.activation(
                            out=ys[b][:, g * 4:(g + 1) * 4, :], in_=banks[bk][:].rearrange("p (a t) -> p a t", t=128),
                            func=AF.Copy), reads=[bbuf[bk]], writes=[yb[b]])
                    else:
                        kb.op("dve", lambda e, g=g, bk=bk, b=b: e.tensor_copy(
                            out=ys[b][:, g * 4:(g + 1) * 4, :], in_=banks[bk][:].rearrange("p (a t) -> p a t", t=128)),
                            reads=[bbuf[bk]], writes=[yb[b]])
                kb.dma("sp", hTv[:, :, nt * 128:(nt + 1) * 128], ys[b][:], reads=[yb[b]])
            kb.barrier()

    def row_to_cols(ps_ap_fn, row_sb, ncols_chunks, bk, rbuf, first=True):
        for j in range(ncols_chunks):
            kb.op("pe", lambda e, j=j: e.matmul(ps_ap_fn(j), row_sb[0:1, j * 128:(j + 1) * 128], K["ones_f"][0:1, 0:1],
                                                start=(first and j == 0), stop=True, skip_group_check=True),
                  reads=[rbuf, KBUF], writes=[bbuf[bk]])

    def stage_cond():
        with ExitStack() as st:
            crow = sb(st, "crow", [1, D])
            frow = sb(st, "frow", [1, D])
            rb = Buf()
            kb.dma("sp", crow[:], cvec[:, :], writes=[rb])
            kb.dma("sp", frow[:], fin_g[:, :], writes=[rb])
            row_to_cols(lambda j: banks[0][:, j:j + 1], crow, DC, 0, rb)
            kb.op("act", lambda e: e.activation(out=condT[:], in_=banks[0][:, 0:DC], func=AF.Silu),
                  reads=[bbuf[0]], writes=[VB])
            row_to_cols(lambda j: banks[1][:, j:j + 1], frow, DC, 1, rb)
            kb.op("dve", lambda e: e.tensor_scalar(out=finA[:], in0=banks[1][:, 0:DC], scalar1=SQD, scalar2=None,
                                                   op0=ALU.mult), reads=[bbuf[1]], writes=[VB])
            kb.barrier()

    def stage_mod(l):
        with ExitStack() as st:
            NCc = NMOD * DC
            wa = [sb(st, "wa%d" % i, [128, NMOD * D], BF16) for i in range(2)]
            wab = [Buf() for _ in range(2)]
            brow = sb(st, "brow", [1, NMOD * D])
            grow = sb(st, "grow", [1, 3 * D])
            rb = Buf()
            kb.dma("sp", brow[:], b_ada[l:l + 1, :], writes=[rb])
            kb.dma("sp", grow[:], norm_g[l:l + 1, :], writes=[rb])
            for kc in range(DC):
                b = kc % 2
                kb.dma("pool", wa[b][:], w_ada[l, kc * 128:(kc + 1) * 128, :], writes=[wab[b]])
                for j in range(NCc):
                    kb.op("pe", lambda e, j=j, b=b, kc=kc: e.matmul(
                        banks[0][:, j:j + 1], wa[b][:, j * 128:(j + 1) * 128], condT[:, kc:kc + 1],
                        start=(kc == 0 and j == 0), stop=False, skip_group_check=True),
                        reads=[wab[b], VB], writes=[bbuf[0]])
            row_to_cols(lambda j: banks[0][:, j:j + 1], brow, NCc, 0, rb, first=False)
            row_to_cols(lambda j: banks[1][:, j:j + 1], grow, 3 * DC, 1, rb)
            kb.op("act", lambda e: e.activation(out=modS[:], in_=banks[0][:, 0:NCc], func=AF.Copy),
                  reads=[bbuf[0]], writes=[VB])
            kb.op("act", lambda e: e.activation(out=ngS[:], in_=banks[1][:, 0:3 * DC], func=AF.Copy),
                  reads=[bbuf[1]], writes=[VB])
            for k in range(3):
                sc = modS[:, (3 * k + 1) * DC:(3 * k + 2) * DC]
                g = modS[:, (3 * k + 2) * DC:(3 * k + 3) * DC]
                kb.op("dve", lambda e, k=k, sc=sc: e.scalar_tensor_tensor(
                    out=Avec[:, k * DC:(k + 1) * DC], in0=sc, scalar=1.0, in1=ngS[:, k * DC:(k + 1) * DC],
                    op0=ALU.add, op1=ALU.mult), reads=[VB], writes=[VB])
                kb.op("dve", lambda e, k=k: e.tensor_scalar(
                    out=Avec[:, k * DC:(k + 1) * DC], in0=Avec[:, k * DC:(k + 1) * DC], scalar1=SQD, scalar2=None,
                    op0=ALU.mult), reads=[VB], writes=[VB])
                kb.op("dve", lambda e, k=k, g=g: e.tensor_scalar(
                    out=Gvec[:, k * DC:(k + 1) * DC], in0=g, scalar1=(1.0 if k == 1 else 0.5), scalar2=None,
                    op0=ALU.mult), reads=[VB], writes=[VB])
            kb.barrier()

    def stage_norm(Aap, Bap, final=False):
        T = min(512, S)
        with ExitStack() as st:
            hs = [sb(st, "nm_h%d" % i, [128, DC, T]) for i in range(2)]
            hb = [Buf() for _ in range(2)]
            sq = sb(st, "nm_sq", [128, DC, T], BF16)
            sqb = Buf()
            rstd = sb(st, "nm_rstd", [128, T])
            rb = Buf()
            tmp = [sb(st, "nm_tmp%d" % i, [128, T]) for i in range(2)]
            tb = [Buf() for _ in range(2)]
            if not final:
                ub = [sb(st, "nm_u%d" % i, [128, DC, T], BF16) for i in range(2)]
            else:
                ub = [sb(st, "nm_u%d" % i, [128, DC, T]) for i in range(1)]
                ot = [sb(st, "nm_o%d" % i, [128, D]) for i in range(2)]
                otb = [Buf() for _ in range(2)]
            ubb = [Buf() for _ in range(2)]
            hTv = hT.rearrange("(dc p) s -> p dc s", p=128)
            uTv = uT.rearrange("(dc p) s -> p dc s", p=128)
            for ti in range(S // T):
                b = ti % 2
                ts = slice(ti * T, (ti + 1) * T)
                kb.dma("sp", hs[b][:], hTv[:, :, ts], writes=[hb[b]])
                for dc in range(DC):
                    eng = "act" if dc % 2 == 0 else "pool"
                    if eng == "act":
                        kb.op("act", lambda e, dc=dc, b=b: e.activation(out=sq[:, dc, :], in_=hs[b][:, dc, :], func=AF.Square),
                              reads=[hb[b]], writes=[sqb])
                    else:
                        kb.op("pool", lambda e, dc=dc, b=b: e.tensor_tensor(out=sq[:, dc, :], in0=hs[b][:, dc, :],
                                                                            in1=hs[b][:, dc, :], op=ALU.mult),
                              reads=[hb[b]], writes=[sqb])
                bk = ti % 2
                for dc in range(DC):
                    kb.op("pe", lambda e, dc=dc, bk=bk: e.matmul(banks[bk][:, 0:T], K["ones_b"][:], sq[:, dc, :],
                                                                 start=(dc == 0), stop=(dc == DC - 1)),
                          reads=[sqb, KBUF], writes=[bbuf[bk]])
                kb.op("act", lambda e, bk=bk: e.activation(out=rstd[:], in_=banks[bk][:, 0:T], func=AF.Ln, bias=epsD[:, 0:1], scale=1.0),
                      reads=[bbuf[bk], VB], writes=[rb])
                kb.op("act", lambda e: e.activation(out=rstd[:], in_=rstd[:], func=AF.Exp, scale=-0.5), reads=[rb], writes=[rb])
                ui = b if not final else 0
                u = ub[ui]
                for dc in range(DC):
                    tb_i = dc % 2
                    if Bap is not None:
                        kb.op("dve", lambda e, dc=dc, b=b, tb_i=tb_i: e.scalar_tensor_tensor(
                            out=tmp[tb_i][:], in0=hs[b][:, dc, :], scalar=Aap[:, dc:dc + 1], in1=rstd[:],
                            op0=ALU.mult, op1=ALU.mult), reads=[hb[b], rb, VB], writes=[tb[tb_i]])
                        kb.op("act", lambda e, dc=dc, tb_i=tb_i, u=u: e.activation(
                            out=u[:, dc, :], in_=tmp[tb_i][:], func=AF.Identity, bias=Bap[:, dc:dc + 1], scale=1.0),
                            reads=[tb[tb_i], VB], writes=[ubb[ui]])
                    else:
                        kb.op("dve", lambda e, dc=dc, b=b, u=u: e.scalar_tensor_tensor(
                            out=u[:, dc, :], in0=hs[b][:, dc, :], scalar=Aap[:, dc:dc + 1], in1=rstd[:],
                            op0=ALU.mult, op1=ALU.mult), reads=[hb[b], rb, VB], writes=[ubb[ui]])
                if not final:
                    kb.dma("sp", uTv[:, :, ts], u[:], reads=[ubb[ui]])
                else:
                    for sub in range(T // 128):
                        ob = (ti * (T // 128) + sub) % 2
                        for g in range(4):
                            bk2 = 2 + (sub * 4 + g) % 5
                            for i in range(4):
                                dc = g * 4 + i
                                kb.op("pe", lambda e, dc=dc, i=i, bk2=bk2, sub=sub, u=u: e.transpose(
                                    banks[bk2][:, i * 128:(i + 1) * 128], u[:, dc, sub * 128:(sub + 1) * 128],
                                    K["ident_f"][:]), reads=[ubb[ui], KBUF], writes=[bbuf[bk2]])
                            if g % 2 == 0:
                                kb.op("act", lambda e, g=g, bk2=bk2, ob=ob: e.activation(
                                    out=ot[ob][:, g * 512:(g + 1) * 512], in_=banks[bk2][:], func=AF.Copy),
                                    reads=[bbuf[bk2]], writes=[otb[ob]])
                            else:
                                kb.op("dve", lambda e, g=g, bk2=bk2, ob=ob: e.tensor_copy(
                                    out=ot[ob][:, g * 512:(g + 1) * 512], in_=banks[bk2][:]),
                                    reads=[bbuf[bk2]], writes=[otb[ob]])
                        r0 = ti * T + sub * 128
                        kb.dma("sp", out[r0:r0 + 128, :], ot[ob][:], reads=[otb[ob]])
            kb.barrier()

    def stage_ffn(l, which, Gap):
        T = min(1024, S)
        NH = T // 512 if T >= 512 else 1
        HW = min(512, T)
        with ExitStack() as st:
            ut = sb(st, "ff_u", [128, DC, T], BF16)
            utb = Buf()
            act = sb(st, "ff_act", [128, FC, T], BF16)
            actb = Buf()
            wbuf = [sb(st, "ff_w%d" % i, [128, 11264], BF16) for i in range(2)]
            wb = [Buf() for _ in range(2)]
            sg = [sb(st, "ff_sg%d" % i, [128, HW]) for i in range(2)]
            sgb = [Buf() for _ in range(2)]
            hr = [sb(st, "ff_hr%d" % i, [128, T]) for i in range(2)]
            hrb = [Buf() for _ in range(2)]
            uTv = uT.rearrange("(dc p) s -> p dc s", p=128)
            wgv = w_gate[l, which].rearrange("(kc p) f -> p kc f", p=128)
            wuv = w_up[l, which].rearrange("(kc p) f -> p kc f", p=128)
            wdv = w_down[l, which].rearrange("(j p) d -> p j d", p=128)
            wi = 0
            pi = 0
            for ti in range(S // T):
                ts = slice(ti * T, (ti + 1) * T)
                kb.dma("sp", ut[:], uTv[:, :, ts], writes=[utb])
                for jj in range(FC // 2):
                    b = wi % 2
                    wi += 1
                    wg = wbuf[b][:, 0:4096].rearrange("p (k f) -> p k f", f=256)
                    wu = wbuf[b][:, 4096:8192].rearrange("p (k f) -> p k f", f=256)
                    kb.dma("pool", wg, wgv[:, :, jj * 256:(jj + 1) * 256], writes=[wb[b]])
                    kb.dma("pool", wu, wuv[:, :, jj * 256:(jj + 1) * 256], writes=[wb[b]])
                    for j2 in range(2):
                        j = jj * 2 + j2
                        for hf in range(NH):
                            bg = 2 * (pi % 3)
                            bu = bg + 1
                            si = pi % 2
                            pi += 1
                            hs_ = slice(hf * HW, (hf + 1) * HW)
                            for kc in range(DC):
                                kb.op("pe", lambda e, kc=kc, bg=bg, wg=wg, j2=j2, hs_=hs_: e.matmul(
                                    banks[bg][:, 0:HW], wg[:, kc, j2 * 128:(j2 + 1) * 128], ut[:, kc, hs_],
                                    start=(kc == 0), stop=(kc == DC - 1)), reads=[wb[b], utb], writes=[bbuf[bg]])
                            for kc in range(DC):
                                kb.op("pe", lambda e, kc=kc, bu=bu, wu=wu, j2=j2, hs_=hs_: e.matmul(
                                    banks[bu][:, 0:HW], wu[:, kc, j2 * 128:(j2 + 1) * 128], ut[:, kc, hs_],
                                    start=(kc == 0), stop=(kc == DC - 1)), reads=[wb[b], utb], writes=[bbuf[bu]])
                            kb.op("act", lambda e, bg=bg, si=si: e.activation(out=sg[si][:], in_=banks[bg][:, 0:HW], func=AF.Silu),
                                  reads=[bbuf[bg]], writes=[sgb[si]])
                            kb.op("dve", lambda e, bu=bu, si=si, j=j, hs_=hs_: e.tensor_tensor(
                                out=act[:, j, hs_], in0=banks[bu][:, 0:HW], in1=sg[si][:], op=ALU.mult),
                                reads=[bbuf[bu], sgb[si]], writes=[actb])
                for g in range(DC // 2):
                    b = wi % 2
                    wi += 1
                    wd = wbuf[b][:, 0:11264].rearrange("p (j d) -> p j d", d=256)
                    kb.dma("pool", wd, wdv[:, :, g * 256:(g + 1) * 256], writes=[wb[b]])
                    for d2 in range(2):
                        dc = g * 2 + d2
                        hb_i = dc % 2
                        kb.dma("sp", hr[hb_i][:], hT[dc * 128:(dc + 1) * 128, ts], writes=[hrb[hb_i]])
                        for hf in range(NH):
                            bk = pi % NB
                            pi += 1
                            hs_ = slice(hf * HW, (hf + 1) * HW)
                            for j in range(FC):
                                kb.op("pe", lambda e, j=j, bk=bk, wd=wd, d2=d2, hs_=hs_: e.matmul(
                                    banks[bk][:, 0:HW], wd[:, j, d2 * 128:(d2 + 1) * 128], act[:, j, hs_],
                                    start=(j == 0), stop=(j == FC - 1)), reads=[wb[b], actb], writes=[bbuf[bk]])
                            kb.op("dve", lambda e, bk=bk, dc=dc, hb_i=hb_i, hs_=hs_: e.scalar_tensor_tensor(
                                out=hr[hb_i][:, hs_], in0=banks[bk][:, 0:HW], scalar=Gap[:, dc:dc + 1], in1=hr[hb_i][:, hs_],
                                op0=ALU.mult, op1=ALU.add), reads=[bbuf[bk], VB, hrb[hb_i]], writes=[hrb[hb_i]])
                        kb.dma("sp", hT[dc * 128:(dc + 1) * 128, ts], hr[hb_i][:], reads=[hrb[hb_i]])
            kb.barrier()

    def stage_inproj(l):
        T = min(1024, S)
        HW = min(512, T)
        NH = T // HW
        W = w_in[l].rearrange("(kc p) c -> p kc c", p=128)
        groups = []
        for c in range(4):
            groups.append(([(A_Q + c * 64, 64), (A_Q + (4 + c) * 64, 64)], [(0, qTa[c * 128:(c + 1) * 128, :], 128, BF16)]))
        groups.append(([(A_K, 128)], [(0, kTa[:, :], 128, BF16)]))
        groups.append(([(B_Q, 512)], [(c * 128, qTb[c * 128:(c + 1) * 128, :], 128, BF16) for c in range(4)]))
        groups.append(([(B_K, 512)], [(c * 128, kTb[c * 128:(c + 1) * 128, :], 128, BF16) for c in range(4)]))
        groups.append(([(C_Q, 512)], [(c * 128, qTc[c * 128:(c + 1) * 128, :], 128, F32) for c in range(2)] +
                       [(256 + c * 128, kTc[c * 128:(c + 1) * 128, :], 128, F32) for c in range(2)]))
        groups.append(([(D_Q, 512)], [(c * 128, qTd[c * 128:(c + 1) * 128, :], 128, F32) for c in range(2)] +
                       [(256 + c * 128, kTd[c * 128:(c + 1) * 128, :], 128, F32) for c in range(2)]))
        for (base, dst) in ((D_Q, qrTd), (D_K, krTd)):
            segs = []
            for h in range(4):
                h0 = base + h * 64
                segs += [(h0 + 32, 32), (h0, 32)]
            groups.append((segs, [(c * 128, dst[c * 128:(c + 1) * 128, :], 128, F32) for c in range(2)]))
        groups.append(([(C_LR, 16)], [(0, lrT[:, :], 16, F32)]))
        tm = [(A_V, 128, va, BF16, False), (B_V, 512, vb, BF16, False), (B_F, 8, fb, F32, False),
              (C_K, 256, kc_tok, F32, False), (C_V, 512, vc, BF16, False), (C_R, 512, rc, F32, True),
              (D_K, 256, kd_tok, F32, False), (D_V, 512, vd, BF16, False), (D_G, 512, gd, F32, True)]
        with ExitStack() as st:
            ut = [sb(st, "ip_u%d" % i, [128, DC, T], BF16) for i in range(2)]
            utb = [Buf() for _ in range(2)]
            wl = [sb(st, "ip_wl%d" % i, [128, DC, 512], BF16) for i in range(2)]
            wlb = [Buf() for _ in range(2)]
            wr = [sb(st, "ip_wr%d" % i, [128, DC, 512], BF16) for i in range(2)]
            wrb = [Buf() for _ in range(2)]
            of = [sb(st, "ip_of%d" % i, [128, T]) for i in range(3)]
            ofb = [Buf() for _ in range(3)]
            obf = [sb(st, "ip_ob%d" % i, [128, T], BF16) for i in range(3)]
            obb = [Buf() for _ in range(3)]
            tf = [sb(st, "ip_tf%d" % i, [128, 512]) for i in range(3)]
            tfb = [Buf() for _ in range(3)]
            tbf = [sb(st, "ip_tb%d" % i, [128, 512], BF16) for i in range(3)]
            tbb = [Buf() for _ in range(3)]
            uTv = uT.rearrange("(dc p) s -> p dc s", p=128)
            wi = 0
            oi = 0
            pi = 0
            for ti in range(S // T):
                ub_ = ti % 2
                ts = slice(ti * T, (ti + 1) * T)
                kb.dma("sp", ut[ub_][:], uTv[:, :, ts], writes=[utb[ub_]])
                for (segs, jobs) in groups:
                    b = wi % 2
                    wi += 1
                    c0 = 0
                    for (col, n) in segs:
                        kb.dma("pool", wl[b][:, :, c0:c0 + n], W[:, :, col:col + n], writes=[wlb[b]])
                        c0 += n
                    for (off, dst, M, dt) in jobs:
                        o = oi % 3
                        oi += 1
                        if dt == BF16:
                            dstt, dstb = obf[o], obb[o]
                        else:
                            dstt, dstb = of[o], ofb[o]
                        for hf in range(NH):
                            hs_ = slice(hf * HW, (hf + 1) * HW)
                            bk = pi % NB
                            pi += 1
                            for kc in range(DC):
                                kb.op("pe", lambda e: e.matmul(banks[bk][0:M, 0:HW], wl[b][:, kc, off:off + M], ut[ub_][:, kc, hs_],
                                                               start=(kc == 0), stop=(kc == DC - 1)),
                                      reads=[wlb[b], utb[ub_]], writes=[bbuf[bk]])
                            if pi % 2 == 0:
                                kb.op("act", lambda e: e.activation(out=dstt[0:M, hs_], in_=banks[bk][0:M, 0:HW], func=AF.Copy),
                                      reads=[bbuf[bk]], writes=[dstb])
                            else:
                                kb.op("dve", lambda e: e.tensor_copy(out=dstt[0:M, hs_], in_=banks[bk][0:M, 0:HW]),
                                      reads=[bbuf[bk]], writes=[dstb])
                        kb.dma("sp", dst[:, ts], dstt[0:M, :], reads=[dstb])
                for (col, n, dst, dt, silu) in tm:
                    b = wi % 2
                    wi += 1
                    kb.dma("pool", wr[b][:, :, 0:n], W[:, :, col:col + n], writes=[wrb[b]])
                    for sub in range(T // 128):
                        bk = pi % NB
                        pi += 1
                        for kc in range(DC):
                            kb.op("pe", lambda e: e.matmul(banks[bk][:, 0:n], ut[ub_][:, kc, sub * 128:(sub + 1) * 128], wr[b][:, kc, 0:n],
                                                           start=(kc == 0), stop=(kc == DC - 1)), reads=[wrb[b], utb[ub_]], writes=[bbuf[bk]])
                        o = oi % 3
                        oi += 1
                        if dt == BF16:
                            dstt, dstb = tbf[o], tbb[o]
                        else:
                            dstt, dstb = tf[o], tfb[o]
                        if silu:
                            kb.op("act", lambda e: e.activation(out=dstt[:, 0:n], in_=banks[bk][:, 0:n], func=AF.Silu),
                                  reads=[bbuf[bk]], writes=[dstb])
                        elif oi % 2 == 0:
                            kb.op("act", lambda e: e.activation(out=dstt[:, 0:n], in_=banks[bk][:, 0:n], func=AF.Copy),
                                  reads=[bbuf[bk]], writes=[dstb])
                        else:
                            kb.op("dve", lambda e: e.tensor_copy(out=dstt[:, 0:n], in_=banks[bk][:, 0:n]),
                                  reads=[bbuf[bk]], writes=[dstb])
                        r0 = ti * T + sub * 128
                        kb.dma("sp", dst[r0:r0 + 128, :], dstt[:, 0:n], reads=[dstb])
            kb.barrier()

    def stage_attn(l, fox):
        npair = 4
        nkc = 4 if fox else 1
        with ExitStack() as st:
            QT = sb(st, "at_q", [128, 4, S], BF16)
            KT = sb(st, "at_k", [128, nkc, S], BF16)
            VP = sb(st, "at_v", [128, NT, nkc, 192], BF16)
            OT = sb(st, "at_o", [128, 4, S], BF16)
            LB = Buf()
            OB = Buf()
            NPT = 4
            PT = [sb(st, "at_p%d" % i, [128, 512], BF16) for i in range(NPT)]
            PB = [[Buf() for _ in range(4)] for _ in range(NPT)]
            rd = [sb(st, "at_rd%d" % i, [128, 512]) for i in range(2)]
            rdb = [Buf() for _ in range(2)]
            kb.op("pool", lambda e: e.memset(VP[:], 0.0), writes=[LB])
            qsrc = qTb if fox else qTa
            ksrc = kTb if fox else kTa
            vsrc = vb if fox else va
            kb.dma("sp", QT[:], qsrc.rearrange("(c p) s -> p c s", p=128), writes=[LB])
            kb.dma("sp", KT[:], ksrc.rearrange("(c p) s -> p c s", p=128), writes=[LB])
            vv = vsrc.rearrange("(n p) (c h d) -> p n c h d", p=128, h=2, d=64)
            for c in range(nkc):
                kb.dma("sp", VP[:, :, c, 0:64], vv[:, :, c, 0, :], writes=[LB])
                kb.dma("sp", VP[:, :, c, 128:192], vv[:, :, c, 1, :], writes=[LB])
            if fox:
                fz = sb(st, "at_fz", [128, NT, 8])
                fl = sb(st, "at_fl", [128, NT, 8])
                fP = sb(st, "at_fP", [128, NT, 8])
                Lc = sb(st, "at_Lc", [128, NT, 8])
                Lr = sb(st, "at_Lr", [128, NT, 8])
                bfo = sb(st, "at_bf", [128, 8])
                bias = sb(st, "at_bias", [128, 8, NT, NT])
                FB = Buf()
                kb.dma("sp", fz[:], fb.rearrange("(n p) h -> p n h", p=128), writes=[FB])
                kb.dma("sp", bfo[:], fox_b[l, :].partition_broadcast(128), writes=[FB])
                kb.op("dve", lambda e: e.tensor_tensor(out=fz[:], in0=fz[:], in1=bfo[:].unsqueeze(1).broadcast_to([128, NT, 8]),
                                                       op=ALU.add), reads=[FB], writes=[FB])
                kb.op("act", lambda e: e.activation(out=fl[:], in_=fz[:], func=AF.Exp, scale=-1.0), reads=[FB], writes=[FB])
                kb.op("act", lambda e: e.activation(out=fl[:], in_=fl[:], func=AF.Ln, bias=1.0, scale=1.0), reads=[FB], writes=[FB])
                kb.op("dve", lambda e: e.memset(fP[:, 0, :], 0.0), reads=[FB], writes=[FB])
                for n in range(1, NT):
                    kb.op("dve", lambda e, n=n: e.tensor_tensor(out=fP[:, n, :], in0=fP[:, n - 1, :], in1=fl[:, n - 1, :], op=ALU.add),
                          reads=[FB], writes=[FB])
                for n in range(NT):
                    kb.op("pe", lambda e, n=n: e.matmul(banks[0][:, n * 8:(n + 1) * 8], K["mask_le_f"][:], fl[:, n, :],
                                                        start=(n == 0), stop=False, skip_group_check=True),
                          reads=[FB, KBUF], writes=[bbuf[0]])
                    kb.op("pe", lambda e, n=n: e.matmul(banks[0][:, n * 8:(n + 1) * 8], K["ones_f"][:], fP[:, n, :],
                                                        start=False, stop=True, skip_group_check=True),
                          reads=[FB, KBUF], writes=[bbuf[0]])
                kb.op("act", lambda e: e.activation(out=Lc[:].rearrange("p n h -> p (n h)"), in_=banks[0][:, 0:NT * 8], func=AF.Copy),
                      reads=[bbuf[0]], writes=[FB])
                for n0 in range(0, NT * 8, 512):
                    n1 = min(NT * 8, n0 + 512)
                    kb.op("pe", lambda e, n0=n0, n1=n1: e.matmul(banks[1][:, 0:n1 - n0], K["sel63"][:],
                                                                 Lc[:].rearrange("p n h -> p (n h)")[:, n0:n1], start=True, stop=True),
                          reads=[FB, KBUF], writes=[bbuf[1]])
                    kb.op("act", lambda e, n0=n0, n1=n1: e.activation(out=Lr[:].rearrange("p n h -> p (n h)")[:, n0:n1],
                                                                      in_=banks[1][:, 0:n1 - n0], func=AF.Copy),
                          reads=[bbuf[1]], writes=[FB])
                for h in range(8):
                    kb.op("dve", lambda e, h=h: e.tensor_tensor(
                        out=bias[:, h, :, :], in0=Lc[:, :, h].unsqueeze(1).broadcast_to([128, NT, NT]),
                        in1=Lr[:, :, h].unsqueeze(2).broadcast_to([128, NT, NT]), op=ALU.subtract),
                        reads=[FB], writes=[FB])
            else:
                es_ = sb(st, "at_es", [128, 8])
                esp = sb(st, "at_esp", [128, 4])
                FB = Buf()
                kb.dma("sp", es_[:], sinks[l, :].partition_broadcast(128), writes=[FB])
                kb.op("act", lambda e: e.activation(out=es_[:], in_=es_[:], func=AF.Exp), reads=[FB], writes=[FB])
                kb.op("dve", lambda e: e.tensor_copy(out=esp[0:64, :], in_=es_[0:64, 0:4]), reads=[FB], writes=[FB])
                kb.op("dve", lambda e: e.tensor_copy(out=esp[64:128, :], in_=es_[64:128, 4:8]), reads=[FB], writes=[FB])

            G = min(4, NT)
            GW_ = G * 128
            gi = 0
            si = 0
            for c in range(npair):
                kc_ = c if fox else 0
                for g in range(NT // G):
                    i0 = g * G
                    i1 = i0 + G - 1
                    bo = (gi % 2) * 2
                    bd = bo + 1
                    r = gi % 2
                    gi += 1
                    steps = []
                    for hf in range(2):
                        js = range(0, i1 + 1) if fox else range(max(i0 - 1, 0), i1 + 1)
                        for j in js:
                            ilo = max(j, i0)
                            ihi = i1 if fox else min(j + 1, i1)
                            if ilo <= ihi:
                                steps.append((hf, j, ilo, ihi))

                    def emit_score(k):
                        hf, j, ilo, ihi = steps[k]
                        Wd = (ihi - ilo + 1) * 128
                        bs = 4 + (si + k) % 3
                        rs = slice(hf * 64, (hf + 1) * 64)
                        kb.op("pe", lambda e: e.matmul(banks[bs][:, 0:Wd], KT[rs, kc_, j * 128:(j + 1) * 128],
                                                       QT[rs, c, ilo * 128:(ihi + 1) * 128], start=True, stop=True),
                              reads=[LB], writes=[bbuf[bs]])
                        pb = (si + k) % NPT
                        if fox:
                            h = 2 * c + hf
                            for i in range(ilo, ihi + 1):
                                off = (i - ilo) * 128
                                kb.op("act", lambda e: e.activation(out=PT[pb][:, off:off + 128], in_=banks[bs][:, off:off + 128], func=AF.Exp,
                                                                    bias=bias[:, h, i, j:j + 1], scale=0.125),
                                      reads=[bbuf[bs], FB], writes=[PB[pb][i - ilo]])
                        else:
                            kb.op("act", lambda e: e.activation(out=PT[pb][:, 0:Wd], in_=banks[bs][:, 0:Wd], func=AF.Exp, scale=0.125),
                                  reads=[bbuf[bs]], writes=PB[pb][0:ihi - ilo + 1])
                        for i in range(ilo, ihi + 1):
                            off = (i - ilo) * 128
                            if i == j:
                                kb.op("pool", lambda e: e.tensor_tensor(out=PT[pb][:, off:off + 128], in0=PT[pb][:, off:off + 128],
                                                                        in1=K["mask_le"][:], op=ALU.mult),
                                      reads=[PB[pb][i - ilo], KBUF], writes=[PB[pb][i - ilo]])
                            elif not fox:
                                kb.op("pool", lambda e: e.tensor_tensor(out=PT[pb][:, off:off + 128], in0=PT[pb][:, off:off + 128],
                                                                        in1=K["mask_gt"][:], op=ALU.mult),
                                      reads=[PB[pb][i - ilo], KBUF], writes=[PB[pb][i - ilo]])

                    def emit_pv(k):
                        hf, j, ilo, ihi = steps[k]
                        Wd = (ihi - ilo + 1) * 128
                        cols = slice((ilo - i0) * 128, (ihi - i0 + 1) * 128)
                        pb = (si + k) % NPT
                        first = (k == 0)
                        last = (k == len(steps) - 1)
                        kb.op("pe", lambda e: e.matmul(banks[bo][:, cols], VP[:, j, kc_, hf * 64:hf * 64 + 128], PT[pb][:, 0:Wd],
                                                       start=first, stop=last, skip_group_check=True), reads=[LB] + PB[pb][0:ihi - ilo + 1], writes=[bbuf[bo]])
                        kb.op("pe", lambda e: e.matmul(banks[bd][:, cols], K["onespad"][:, hf * 64:hf * 64 + 128], PT[pb][:, 0:Wd],
                                                       start=first, stop=last, skip_group_check=True), reads=[KBUF] + PB[pb][0:ihi - ilo + 1], writes=[bbuf[bd]])

                    LOOK = 2
                    for k in range(len(steps) + LOOK):
                        if k < len(steps):
                            emit_score(k)
                        if k - LOOK >= 0:
                            emit_pv(k - LOOK)
                    si += len(steps)
                    gsl = slice(i0 * 128, (i1 + 1) * 128)
                    if fox:
                        kb.op("dve", lambda e: e.reciprocal(out=rd[r][:, 0:GW_], in_=banks[bd][:, 0:GW_]),
                              reads=[bbuf[bd]], writes=[rdb[r]])
                    else:
                        kb.op("dve", lambda e: e.tensor_scalar(out=rd[r][:, 0:GW_], in0=banks[bd][:, 0:GW_],
                                                               scalar1=esp[:, c:c + 1], scalar2=None, op0=ALU.add),
                              reads=[bbuf[bd], FB], writes=[rdb[r]])
                        kb.op("dve", lambda e: e.reciprocal(out=rd[r][:, 0:GW_], in_=rd[r][:, 0:GW_]), reads=[rdb[r]], writes=[rdb[r]])
                    kb.op("dve", lambda e: e.tensor_tensor(out=OT[:, c, gsl], in0=banks[bo][:, 0:GW_], in1=rd[r][:, 0:GW_], op=ALU.mult),
                          reads=[bbuf[bo], rdb[r]], writes=[OB])
            dst = oT[1] if fox else oT[0]
            kb.dma("sp", dst.rearrange("(c p) s -> p c s", p=128), OT[:], reads=[OB])
            kb.barrier()

    def stage_linattn(l, ret):
        with ExitStack() as st:
            qin = sb(st, "la_qin", [64, 4, S], BF16)
            kin = sb(st, "la_kin", [64, 4, S], BF16)
            OTs = sb(st, "la_o", [128, 4, S], BF16)
            LB = Buf()
            QB = Buf()
            OB = Buf()
            dec = sb(st, "la_dec", [64, 4, NCH])
            DB = Buf()
            Sst = sb(st, "la_S", [64, 4, 128])
            Sbf = sb(st, "la_Sb", [64, 4, 128], BF16)
            SB_ = Buf()
            SBb = Buf()
            ksr = [sb(st, "la_ks%d" % i, [64, 256], BF16) for i in range(3)]
            ksb = [Buf() for _ in range(3)]
            vt = [sb(st, "la_v%d" % i, [64, 512], BF16) for i in range(3)]
            vtb = [Buf() for _ in range(3)]
            gt = [sb(st, "la_g%d" % i, [64, 512]) for i in range(3)]
            gtb = [Buf() for _ in range(3)]
            kt = [sb(st, "la_kt%d" % i, [64, 256]) for i in range(3)]
            ktb = [Buf() for _ in range(3)]
            sm = [sb(st, "la_sm%d" % i, [64, 4, 64], BF16) for i in range(3)]
            smb = [Buf() for _ in range(3)]
            ss = sb(st, "la_ss", [64, 8])
            st2 = sb(st, "la_st2", [64, 8])
            junk = sb(st, "la_junk", [64, 128])
            t1 = [sb(st, "la_t1%d" % i, [64, 512]) for i in range(3)]
            t1b = [Buf() for _ in range(3)]
            ob16 = [sb(st, "la_ob%d" % i, [64, 512], BF16) for i in range(3)]
            obb = [Buf() for _ in range(3)]
            SS = Buf()
            gtab = sb(st, "la_gtab", [64, 512])
            btab = sb(st, "la_btab", [64, 512])
            TB = Buf()
            kb.op("dve", lambda e: e.memset(Sst[:], 0.0), writes=[SB_])
            kb.op("pool", lambda e: e.memset(Sbf[:], 0.0), writes=[SBb])
            qsrc, ksrc, ktok, vsrc, gsrc, odst = (qTd, kTd, kd_tok, vd, gd, oT[3]) if ret else (qTc, kTc, kc_tok, vc, rc, oT[2])
            qsv = qsrc.rearrange("(h p) s -> p h s", p=64)
            ksv = ksrc.rearrange("(h p) s -> p h s", p=64)
            if ret:
                ctk = sb(st, "la_ctk", [64, NCH, 32])
                stk = sb(st, "la_stk", [64, NCH, 32])
                kb.dma("sp", ctk[:], cin["cos_tok"], writes=[LB])
                kb.dma("sp", stk[:], cin["sin_tok"], writes=[LB])
                kb.dma("sp", gtab[:], ret_w[l, :].partition_broadcast(64), writes=[TB])
                kb.dma("sp", btab[:], ret_b[l, :].partition_broadcast(64), writes=[TB])
                with ExitStack() as st_pre:
                    cosT = sb(st_pre, "la_cos", [64, S])
                    sinT = sb(st_pre, "la_sin", [64, S])
                    SH = S // 2
                    qx = sb(st_pre, "la_qx", [64, SH])
                    qr = sb(st_pre, "la_qr", [64, SH])
                    kb.dma("sp", cosT[:], cin["cosT"][0:64, :], writes=[LB])
                    kb.dma("sp", sinT[:], cin["sinT"][0:64, :], writes=[LB])
                    for (srcv, rsrc, dstt, tab) in ((qsv, qrTd, qin, "ret_dq"), (ksv, krTd, kin, "ret_dk")):
                        rsv = rsrc.rearrange("(h p) s -> p h s", p=64)
                        for h in range(4):
                            for hh in range(2):
                                hsl = slice(hh * SH, (hh + 1) * SH)
                                kb.dma("sp", qx[:], srcv[:, h, hsl], reads=[LB], writes=[LB])
                                kb.dma("sp", qr[:], rsv[:, h, hsl], reads=[LB], writes=[LB])
                                kb.op("dve", lambda e: e.tensor_tensor(out=qx[:], in0=qx[:], in1=cosT[:, hsl], op=ALU.mult),
                                      reads=[LB], writes=[LB])
                                kb.op("pool", lambda e: e.tensor_tensor(out=qr[:], in0=qr[:], in1=sinT[:, hsl], op=ALU.mult),
                                      reads=[LB], writes=[LB])
                                kb.op("dve", lambda e: e.tensor_tensor(out=qx[:], in0=qx[:], in1=qr[:], op=ALU.add),
                                      reads=[LB], writes=[LB])
                                kb.op("pool", lambda e: e.tensor_tensor(
                                    out=dstt[:, h, hsl].rearrange("p (n j) -> p n j", j=64),
                                    in0=qx[:].rearrange("p (n j) -> p n j", j=64),
                                    in1=K[tab][:, h, :].unsqueeze(1).broadcast_to([64, NCH // 2, 64]), op=ALU.mult),
                                    reads=[LB, KBUF], writes=[QB])
                    kb.barrier()
                kb.op("dve", lambda e: e.tensor_copy(out=dec[:], in_=K["ret_dec"][:].unsqueeze(2).broadcast_to([64, 4, NCH])),
                      reads=[KBUF], writes=[DB])
            else:
                lr = sb(st, "la_lr", [33, S])
                wga = sb(st, "la_wga", [33, 256])
                kb.op("dve", lambda e: e.memset(lr[:], 1.0), writes=[LB])
                kb.op("dve", lambda e: e.memset(wga[:], 0.0), writes=[TB])
                kb.dma("sp", lr[0:16, :], lrT[:, :], reads=[LB], writes=[LB])
                kb.dma("sp", wga[0:16, :], gla_wg[l], reads=[TB], writes=[TB])
                kb.dma("sp", wga[32:33, :], gla_bg[l:l + 1, :], reads=[TB], writes=[TB])
                kb.dma("sp", gtab[:, 0:128], gla_ng[l, :].partition_broadcast(64), writes=[TB])
                lsb = [sb(st, "la_l%d" % i, [64, 256]) for i in range(3)]
                lbb = [Buf() for _ in range(3)]
                eq = [sb(st, "la_eq%d" % i, [64, 4, 64]) for i in range(3)]
                eqb = [Buf() for _ in range(3)]
                ek = [sb(st, "la_ek%d" % i, [64, 4, 64]) for i in range(3)]
                ekb = [Buf() for _ in range(3)]
                ed = [sb(st, "la_ed%d" % i, [64, 256]) for i in range(3)]
                edb = [Buf() for _ in range(3)]
                qch = [sb(st, "la_qch%d" % i, [64, 4, 64]) for i in range(3)]
                kch = [sb(st, "la_kch%d" % i, [64, 4, 64]) for i in range(3)]
                qcb = [Buf() for _ in range(3)]

            def phase(n, ph):
                b = n % 3
                cs = slice(n * 64, (n + 1) * 64)
                bo = 4 + n % 2
                o3 = banks[bo][0:64, :].rearrange("p (h d) -> p h d", h=4)
                if ph == 0:
                    kb.dma("sp", vt[b][:], vsrc[n * 64:(n + 1) * 64, :], writes=[vtb[b]])
                    kb.dma("sp", gt[b][:], gsrc[n * 64:(n + 1) * 64, :], writes=[gtb[b]])
                    kb.dma("sp", kt[b][:], ktok[n * 64:(n + 1) * 64, :], writes=[ktb[b]])
                    if not ret:
                        kb.dma("sp", qch[b][:], qsv[:, :, cs], writes=[qcb[b]])
                        kb.dma("sp", kch[b][:], ksv[:, :, cs], writes=[qcb[b]])
                        kb.op("pe", lambda e: e.matmul(banks[0][0:64, 0:256], lr[:, cs], wga[:], start=True, stop=True),
                              reads=[LB, TB], writes=[bbuf[0]])
                        kb.op("act", lambda e: e.activation(out=lsb[b][:], in_=banks[0][0:64, 0:256], func=AF.Exp, scale=-1.0),
                              reads=[bbuf[0]], writes=[lbb[b]])
                        kb.op("act", lambda e: e.activation(out=lsb[b][:], in_=lsb[b][:], func=AF.Ln, bias=1.0, scale=1.0),
                              reads=[lbb[b]], writes=[lbb[b]])
                        for h in range(4):
                            kb.op("pe", lambda e: e.matmul(banks[1][0:64, h * 64:(h + 1) * 64], lsb[b][:, h * 64:(h + 1) * 64],
                                                           K["tri64n"][:], start=(h == 0), stop=True, skip_group_check=True),
                                  reads=[lbb[b], KBUF], writes=[bbuf[1]])
                        kb.op("pe", lambda e: e.matmul(banks[2][0:64, 0:256], K["d64n"][:], lsb[b][:], start=True, stop=True),
                              reads=[lbb[b], KBUF], writes=[bbuf[2]])
                        kb.op("act", lambda e: e.activation(out=eq[b][:].rearrange("p c j -> p (c j)"), in_=banks[1][0:64, 0:256], func=AF.Exp),
                              reads=[bbuf[1]], writes=[eqb[b]])
                        kb.op("act", lambda e: e.activation(out=ek[b][:].rearrange("p c j -> p (c j)"), in_=banks[1][0:64, 0:256], func=AF.Exp, scale=-1.0),
                              reads=[bbuf[1]], writes=[ekb[b]])
                        kb.op("act", lambda e: e.activation(out=ed[b][:], in_=banks[2][0:64, 0:256], func=AF.Exp),
                              reads=[bbuf[2]], writes=[edb[b]])
                        kb.op("dve", lambda e: e.scalar_tensor_tensor(out=qin[:, :, cs], in0=qch[b][:], scalar=0.125, in1=eq[b][:],
                                                                      op0=ALU.mult, op1=ALU.mult), reads=[qcb[b], eqb[b]], writes=[QB])
                        kb.op("pool", lambda e: e.tensor_tensor(out=kin[:, :, cs], in0=kch[b][:], in1=ek[b][:], op=ALU.mult),
                              reads=[qcb[b], ekb[b]], writes=[QB])
                        kb.op("dve", lambda e: e.tensor_copy(out=dec[:, :, n:n + 1], in_=eq[b][:, :, 63:64]),
                              reads=[eqb[b]], writes=[DB])
                        kb.op("dve", lambda e: e.tensor_tensor(out=ksr[b][:], in0=kt[b][:], in1=ed[b][:], op=ALU.mult),
                              reads=[ktb[b], edb[b]], writes=[ksb[b]])
                    else:
                        k4 = kt[b][:].rearrange("p (h two f) -> p h two f", h=4, two=2)
                        o4 = t1[b][:, 0:256].rearrange("p (h two f) -> p h two f", h=4, two=2)
                        j4 = t1[b][:, 256:512].rearrange("p (h two f) -> p h two f", h=4, two=2)
                        cb = ctk[:, n, :].unsqueeze(1).broadcast_to([64, 4, 32])
                        sbn = stk[:, n, :].unsqueeze(1).broadcast_to([64, 4, 32])
                        rd_ = [ktb[b], LB]
                        kb.op("dve", lambda e: e.tensor_tensor(out=o4[:, :, 0, :], in0=k4[:, :, 0, :], in1=cb, op=ALU.mult), reads=rd_, writes=[t1b[b]])
                        kb.op("pool", lambda e: e.tensor_tensor(out=j4[:, :, 0, :], in0=k4[:, :, 1, :], in1=sbn, op=ALU.mult), reads=rd_, writes=[t1b[b]])
                        kb.op("dve", lambda e: e.tensor_tensor(out=o4[:, :, 0, :], in0=o4[:, :, 0, :], in1=j4[:, :, 0, :], op=ALU.subtract),
                              reads=[t1b[b]], writes=[t1b[b]])
                        kb.op("dve", lambda e: e.tensor_tensor(out=o4[:, :, 1, :], in0=k4[:, :, 1, :], in1=cb, op=ALU.mult), reads=rd_, writes=[t1b[b]])
                        kb.op("pool", lambda e: e.tensor_tensor(out=j4[:, :, 1, :], in0=k4[:, :, 0, :], in1=sbn, op=ALU.mult), reads=rd_, writes=[t1b[b]])
                        kb.op("dve", lambda e: e.tensor_tensor(out=o4[:, :, 1, :], in0=o4[:, :, 1, :], in1=j4[:, :, 1, :], op=ALU.add),
                              reads=[t1b[b]], writes=[t1b[b]])
                        kb.op("dve", lambda e: e.tensor_tensor(
                            out=ksr[b][:].rearrange("p (h d) -> p h d", h=4), in0=t1[b][:, 0:256].rearrange("p (h d) -> p h d", h=4),
                            in1=K["ret_ks"][:].unsqueeze(2).broadcast_to([64, 4, 64]), op=ALU.mult),
                            reads=[t1b[b], KBUF], writes=[ksb[b]])
                if ph == 1:
                    bs = 3
                    for h in range(4):
                        kb.op("pe", lambda e: e.matmul(banks[bs][0:64, h * 64:(h + 1) * 64], kin[:, h, cs], qin[:, h, cs],
                                                       start=(h == 0), stop=True, skip_group_check=True),
                              reads=[QB], writes=[bbuf[bs]])
                    kb.op("dve", lambda e: e.tensor_tensor(out=sm[b][:], in0=banks[bs][0:64, 0:256].rearrange("p (h i) -> p h i", h=4),
                                                           in1=K["mask_le"][0:64, 0:64].unsqueeze(1).broadcast_to([64, 4, 64]), op=ALU.mult),
                          reads=[bbuf[bs], KBUF], writes=[smb[b]])
                    for h in range(4):
                        kb.op("pe", lambda e: e.matmul(banks[bo][0:64, h * 128:(h + 1) * 128], qin[:, h, cs], Sbf[:, h, :],
                                                       start=(h == 0), stop=False, skip_group_check=True),
                              reads=[QB, SBb], writes=[bbuf[bo]])
                        kb.op("pe", lambda e: e.matmul(banks[bo][0:64, h * 128:(h + 1) * 128], sm[b][:, h, :], vt[b][:, h * 128:(h + 1) * 128],
                                                       start=False, stop=True, skip_group_check=True),
                              reads=[smb[b], vtb[b]], writes=[bbuf[bo]])
                    bu = 6
                    for h in range(4):
                        kb.op("pe", lambda e: e.matmul(banks[bu][0:64, h * 128:(h + 1) * 128], ksr[b][:, h * 64:(h + 1) * 64], vt[b][:, h * 128:(h + 1) * 128],
                                                       start=(h == 0), stop=True, skip_group_check=True), reads=[ksb[b], vtb[b]], writes=[bbuf[bu]])
                    kb.op("dve", lambda e: e.tensor_tensor(out=Sst[:], in0=Sst[:], in1=dec[:, :, n:n + 1].broadcast_to([64, 4, 128]), op=ALU.mult),
                          reads=[SB_, DB], writes=[SB_])
                    kb.op("dve", lambda e: e.tensor_tensor(out=Sst[:], in0=banks[bu][0:64, :].rearrange("p (h d) -> p h d", h=4), in1=Sst[:], op=ALU.add),
                          reads=[SB_, bbuf[bu]], writes=[SB_])
                    kb.op("act", lambda e: e.activation(out=Sbf[:], in_=Sst[:], func=AF.Copy), reads=[SB_], writes=[SBb])
                    kb.op("dve", lambda e: e.memset(ss[:], 0.0), reads=[SS], writes=[SS])
                if ph == 2:
                    for h in range(4):
                        kb.op("act", lambda e: e.activation(out=junk[:], in_=banks[bo][0:64, h * 128:(h + 1) * 128], func=AF.Square,
                                                            accum_out=ss[:, h:h + 1]), reads=[bbuf[bo]], writes=[SS])
                        if ret:
                            kb.op("act", lambda e: e.activation(out=junk[:], in_=banks[bo][0:64, h * 128:(h + 1) * 128], func=AF.Identity,
                                                                accum_out=ss[:, 4 + h:5 + h]), reads=[bbuf[bo]], writes=[SS])
                    if not ret:
                        kb.op("dve", lambda e: e.tensor_scalar(out=st2[:, 0:4], in0=ss[:, 0:4], scalar1=1.0 / 128, scalar2=EPS, op0=ALU.mult, op1=ALU.add),
                              reads=[SS], writes=[SS])
                        kb.op("act", lambda e: e.activation(out=st2[:, 0:4], in_=st2[:, 0:4], func=AF.Ln), reads=[SS], writes=[SS])
                        kb.op("act", lambda e: e.activation(out=st2[:, 0:4], in_=st2[:, 0:4], func=AF.Exp, scale=-0.5), reads=[SS], writes=[SS])
                        kb.op("dve", lambda e: e.tensor_tensor(out=t1[b][:].rearrange("p (h d) -> p h d", h=4), in0=o3,
                                                               in1=st2[:, 0:4].unsqueeze(2).broadcast_to([64, 4, 128]), op=ALU.mult),
                              reads=[bbuf[bo], SS], writes=[t1b[b]])
                        kb.op("pool", lambda e: e.tensor_tensor(out=t1[b][:].rearrange("p (h d) -> p h d", h=4),
                                                                in0=t1[b][:].rearrange("p (h d) -> p h d", h=4),
                                                                in1=gtab[:, 0:128].unsqueeze(1).broadcast_to([64, 4, 128]), op=ALU.mult),
                              reads=[t1b[b], TB], writes=[t1b[b]])
                    else:
                        kb.op("dve", lambda e: e.tensor_scalar(out=st2[:, 4:8], in0=ss[:, 4:8], scalar1=1.0 / 128, scalar2=None, op0=ALU.mult),
                              reads=[SS], writes=[SS])
                        kb.op("dve", lambda e: e.tensor_tensor(out=st2[:, 0:4], in0=st2[:, 4:8], in1=st2[:, 4:8], op=ALU.mult), reads=[SS], writes=[SS])
                        kb.op("dve", lambda e: e.scalar_tensor_tensor(out=st2[:, 0:4], in0=ss[:, 0:4], scalar=1.0 / 128, in1=st2[:, 0:4],
                                                                      op0=ALU.mult, op1=ALU.subtract), reads=[SS], writes=[SS])
                        kb.op("act", lambda e: e.activation(out=st2[:, 0:4], in_=st2[:, 0:4], func=AF.Ln, bias=epsD[0:64, 1:2], scale=1.0), reads=[SS, VB], writes=[SS])
                        kb.op("act", lambda e: e.activation(out=st2[:, 0:4], in_=st2[:, 0:4], func=AF.Exp, scale=-0.5), reads=[SS], writes=[SS])
                        kb.op("dve", lambda e: e.tensor_tensor(out=t1[b][:].rearrange("p (h d) -> p h d", h=4), in0=o3,
                                                               in1=st2[:, 4:8].unsqueeze(2).broadcast_to([64, 4, 128]), op=ALU.subtract),
                              reads=[bbuf[bo], SS], writes=[t1b[b]])
                        kb.op("dve", lambda e: e.tensor_tensor(out=t1[b][:].rearrange("p (h d) -> p h d", h=4),
                                                               in0=t1[b][:].rearrange("p (h d) -> p h d", h=4),
                                                               in1=st2[:, 0:4].unsqueeze(2).broadcast_to([64, 4, 128]), op=ALU.mult),
                              reads=[t1b[b], SS], writes=[t1b[b]])
                        kb.op("pool", lambda e: e.tensor_tensor(out=t1[b][:], in0=t1[b][:], in1=gtab[:], op=ALU.mult), reads=[t1b[b], TB], writes=[t1b[b]])
                        kb.op("pool", lambda e: e.tensor_tensor(out=t1[b][:], in0=t1[b][:], in1=btab[:], op=ALU.add), reads=[t1b[b], TB], writes=[t1b[b]])
                    kb.op("pool", lambda e: e.tensor_tensor(out=ob16[b][:], in0=t1[b][:], in1=gt[b][:], op=ALU.mult),
                          reads=[t1b[b], gtb[b]], writes=[obb[b]])
                    for h in range(4):
                        kb.op("pe", lambda e: e.transpose(trps[:, h, :], ob16[b][:, h * 128:(h + 1) * 128], K["ident_b"][0:64, 0:64]),
                              reads=[obb[b], KBUF], writes=[trb])
                    kb.op("act", lambda e: e.activation(out=OTs[:, :, cs], in_=trps[:], func=AF.Copy), reads=[trb], writes=[OB])

            for it in range(NCH + 2):
                if it < NCH:
                    phase(it, 0)
                if 0 <= it - 1 < NCH:
                    phase(it - 1, 1)
                if 0 <= it - 2 < NCH:
                    phase(it - 2, 2)
            kb.dma("sp", odst.rearrange("(c p) s -> p c s", p=128), OTs[:], reads=[OB])
            kb.barrier()

    def stage_merge(l, Gap):
        T = min(1024, S)
        HW = min(512, T)
        NH = T // HW
        GW = 4
        with ExitStack() as st:
            bmS = sb(st, "mg_bm", [128, 4 * DC])
            BM = Buf()
            with ExitStack() as st_pre:
                brow = sb(st_pre, "mg_brow", [1, 4 * D])
                rb = Buf()
                kb.dma("sp", brow[:], b_merge[l:l + 1, :], writes=[rb])
                row_to_cols(lambda j: banks[0][:, j:j + 1], brow, 4 * DC, 0, rb)
                kb.op("act", lambda e: e.activation(out=bmS[:], in_=banks[0][:, 0:4 * DC], func=AF.Copy), reads=[bbuf[0]], writes=[BM])
                kb.barrier()
            ut = sb(st, "mg_u", [128, DC, T], BF16)
            utb = Buf()
            ot = [sb(st, "mg_o%d" % i, [128, 4, T], BF16) for i in range(2)]
            otb = [Buf() for _ in range(2)]
            mg = sb(st, "mg_m", [128, DC, T])
            mgb = Buf()
            mgh = sb(st, "mg_mh", [128, DC, T], BF16)
            mghb = Buf()
            wm = [sb(st, "mg_wm%d" % i, [128, DC, GW * 128], BF16) for i in range(2)]
            wmb = [Buf() for _ in range(2)]
            wbr = [sb(st, "mg_wb%d" % i, [128, 4, GW * 128], BF16) for i in range(2)]
            wbb = [Buf() for _ in range(2)]
            gs = [sb(st, "mg_g%d" % i, [128, HW]) for i in range(2)]
            gsb = [Buf() for _ in range(2)]
            hr = [sb(st, "mg_hr%d" % i, [128, T]) for i in range(2)]
            hrb = [Buf() for _ in range(2)]
            uTv = uT.rearrange("(dc p) s -> p dc s", p=128)
            wi = 0
            pi = 0
            for ti in range(S // T):
                ts = slice(ti * T, (ti + 1) * T)
                kb.dma("sp", ut[:], uTv[:, :, ts], writes=[utb])
                for br in range(4):
                    ob_ = br % 2
                    kb.dma("sp", ot[ob_][:], oT[br].rearrange("(c p) s -> p c s", p=128)[:, :, ts], writes=[otb[ob_]])
                    wmv = w_merge[l, br].rearrange("(kc p) d -> p kc d", p=128)
                    wbv = w_branch[l, br].rearrange("(c p) d -> p c d", p=128)
                    for dg in range(DC // GW):
                        b = wi % 2
                        wi += 1
                        gsl = slice(dg * GW * 128, (dg + 1) * GW * 128)
                        kb.dma("pool", wm[b][:], wmv[:, :, gsl], writes=[wmb[b]])
                        if br == 0:
                            wv2 = w_branch[l, br].rearrange("(g c p) d -> p g c d", g=2, p=64)
                            kb.dma("pool", wbr[b][0:64, :, :], wv2[:, 0, :, gsl], writes=[wbb[b]])
                            kb.dma("pool", wbr[b][64:128, :, :], wv2[:, 1, :, gsl], writes=[wbb[b]])
                        else:
                            kb.dma("pool", wbr[b][:], wbv[:, :, gsl], writes=[wbb[b]])
                        for d in range(GW):
                            dc = dg * GW + d
                            dsl = slice(d * 128, (d + 1) * 128)
                            for hf in range(NH):
                                hs_ = slice(hf * HW, (hf + 1) * HW)
                                bg = 2 * (pi % 3)
                                by = bg + 1
                                gi_ = pi % 2
                                pi += 1
                                for kc in range(DC):
                                    kb.op("pe", lambda e: e.matmul(banks[bg][:, 0:HW], wm[b][:, kc, dsl], ut[:, kc, hs_],
                                                                   start=(kc == 0), stop=(kc == DC - 1)),
                                          reads=[wmb[b], utb], writes=[bbuf[bg]])
                                for c in range(4):
                                    kb.op("pe", lambda e: e.matmul(banks[by][:, 0:HW], wbr[b][:, c, dsl], ot[ob_][:, c, hs_],
                                                                   start=(c == 0), stop=(c == 3)),
                                          reads=[wbb[b], otb[ob_]], writes=[bbuf[by]])
                                kb.op("act", lambda e: e.activation(
                                    out=gs[gi_][:], in_=banks[bg][:, 0:HW], func=AF.Sigmoid, bias=bmS[:, br * DC + dc:br * DC + dc + 1], scale=1.0),
                                    reads=[bbuf[bg], BM], writes=[gsb[gi_]])
                                if br == 0:
                                    kb.op("dve", lambda e: e.tensor_tensor(out=mg[:, dc, hs_], in0=banks[by][:, 0:HW], in1=gs[gi_][:], op=ALU.mult),
                                          reads=[bbuf[by], gsb[gi_]], writes=[mgb])
                                else:
                                    kb.op("dve", lambda e: e.tensor_tensor(out=gs[gi_][:], in0=banks[by][:, 0:HW], in1=gs[gi_][:], op=ALU.mult),
                                          reads=[bbuf[by], gsb[gi_]], writes=[gsb[gi_]])
                                    if br < 3:
                                        kb.op("pool", lambda e: e.tensor_tensor(out=mg[:, dc, hs_], in0=mg[:, dc, hs_], in1=gs[gi_][:], op=ALU.add),
                                              reads=[gsb[gi_], mgb], writes=[mgb])
                                    else:
                                        kb.op("pool", lambda e: e.tensor_tensor(out=mgh[:, dc, hs_], in0=mg[:, dc, hs_], in1=gs[gi_][:], op=ALU.add),
                                              reads=[gsb[gi_], mgb], writes=[mghb])
                wov = w_out[l].rearrange("(kc p) d -> p kc d", p=128)
                for dg in range(DC // GW):
                    b = wi % 2
                    wi += 1
                    gsl = slice(dg * GW * 128, (dg + 1) * GW * 128)
                    kb.dma("pool", wm[b][:], wov[:, :, gsl], writes=[wmb[b]])
                    for d in range(GW):
                        dc = dg * GW + d
                        dsl = slice(d * 128, (d + 1) * 128)
                        hb_i = dc % 2
                        kb.dma("sp", hr[hb_i][:], hT[dc * 128:(dc + 1) * 128, ts], writes=[hrb[hb_i]])
                        for hf in range(NH):
                            hs_ = slice(hf * HW, (hf + 1) * HW)
                            bk = pi % NB
                            pi += 1
                            for kc in range(DC):
                                kb.op("pe", lambda e: e.matmul(banks[bk][:, 0:HW], wm[b][:, kc, dsl], mgh[:, kc, hs_],
                                                               start=(kc == 0), stop=(kc == DC - 1)),
                                      reads=[wmb[b], mghb], writes=[bbuf[bk]])
                            kb.op("dve", lambda e: e.scalar_tensor_tensor(
                                out=hr[hb_i][:, hs_], in0=banks[bk][:, 0:HW], scalar=Gap[:, dc:dc + 1], in1=hr[hb_i][:, hs_],
                                op0=ALU.mult, op1=ALU.add), reads=[bbuf[bk], VB, hrb[hb_i]], writes=[hrb[hb_i]])
                        kb.dma("sp", hT[dc * 128:(dc + 1) * 128, ts], hr[hb_i][:], reads=[hrb[hb_i]])
            kb.barrier()

    K["mask_le_f"] = sb(es, "K_mask_le_f", [128, 128])
    kb.op("dve", lambda e: e.tensor_copy(out=K["mask_le_f"][:], in_=K["mask_le"][:]), reads=[KBUF], writes=[KBUF])
    kb.barrier()

    stages = build_program.stages
    if "tin" in stages:
        stage_transpose_in()
    stage_cond()
    for l in range(L):
        stage_mod(l)
        if "ffn1" in stages:
            stage_norm(Avec[:, 0:DC], modS[:, 0:DC])
            stage_ffn(l, 0, Gvec[:, 0:DC])
        if "mix" in stages:
            stage_norm(Avec[:, DC:2 * DC], modS[:, 3 * DC:4 * DC])
            stage_inproj(l)
            if "swa" in stages:
                stage_attn(l, False)
            if "fox" in stages:
                stage_attn(l, True)
            if "gla" in stages or "ret" in stages:
                pass
            if "gla" in stages:
                stage_linattn(l, False)
            if "ret" in stages:
                stage_linattn(l, True)
            if "merge" in stages:
                stage_merge(l, Gvec[:, DC:2 * DC])
        if "ffn2" in stages:
            stage_norm(Avec[:, 2 * DC:3 * DC], modS[:, 6 * DC:7 * DC])
            stage_ffn(l, 1, Gvec[:, 2 * DC:3 * DC])
    if "fin" in stages:
        stage_norm(finA[:], None, final=True)
    kb.barrier()
    es.close()
    return nc, hc, kb


build_program.stages = {"tin", "ffn1", "mix", "swa", "fox", "gla", "ret", "merge", "ffn2", "fin"}

WNAMES = ["w_ada", "b_ada", "ffn_w_gate", "ffn_w_up", "ffn_w_down", "w_in", "fox_b_forget", "attn_sinks",
          "gla_w_gate", "gla_b_gate", "gla_norm_g", "ret_gn_w", "ret_gn_b", "w_branch", "w_out"]


def make_in_maps(inputs, hc, S, L, nb):
    f = lambda a: np.ascontiguousarray(np.asarray(a, dtype=np.float32))
    shared = {n: f(inputs[n])[:L] for n in WNAMES}
    shared["norm_g"] = f(inputs["norm_g"])[:L].reshape(L, 3 * D)
    shared["w_merge"] = f(inputs["w_merge"])[:L]
    shared["b_merge"] = f(inputs["b_merge"])[:L].reshape(L, 4 * D)
    shared["final_norm_g"] = f(inputs["final_norm_g"]).reshape(1, D)
    for k, v in hc.items():
        shared["k_" + k] = v
    maps = []
    xs = f(inputs["x"])
    cs = f(inputs["c"])
    for core in range(8):
        b = core % nb
        m = dict(shared)
        m["x"] = np.ascontiguousarray(xs[b, :S])
        m["c"] = np.ascontiguousarray(cs[b:b + 1])
        maps.append(m)
    return maps


def kernel(**inputs):
    nc, hc, kb = build_program(SEQ, DEPTH)
    maps = make_in_maps(inputs, hc, SEQ, DEPTH, BATCH)
    res = run_bass_kernel_spmd(nc, maps, core_ids=list(range(8)))
    outs = [np.asarray(res.results[b]["out"], dtype=np.float32) for b in range(BATCH)]
    return np.stack(outs, axis=0)
```
